# Optimizing a Trainium2 kernel written in Bass

```python
import jax, jax.numpy as jnp
from jax import lax
import numpy as np

D_MODEL = 1024
BATCH = 8
SEQ = 4096
DEPTH = 2

MLA_HEADS = 8
QK_NOPE_DIM = 64
QK_ROPE_DIM = 32
QK_HEAD_DIM = QK_NOPE_DIM + QK_ROPE_DIM
V_HEAD_DIM = 64
Q_LORA_RANK = 256
KV_LORA_RANK = 128
ROPE_THETA = 10000.0
Q_BLOCK = 128
MLA_OUT = MLA_HEADS * V_HEAD_DIM
MLA_IN = Q_LORA_RANK + KV_LORA_RANK + QK_ROPE_DIM

RWKV_HEADS = 8
RWKV_HEAD_DIM = 64
RWKV_DIM = RWKV_HEADS * RWKV_HEAD_DIM
DECAY_LORA = 64
ICLR_LORA = 64
VRES_LORA = 32
GATE_LORA = 128
RWKV_GN_EPS = 64e-5
RWKV_IN = 3 * RWKV_DIM + DECAY_LORA + ICLR_LORA + GATE_LORA

N_IN = MLA_IN + RWKV_IN
MIX_DIM = MLA_OUT + RWKV_DIM

N_EXPERTS = 32
TOP_K = 4
D_FF_EXPERT = D_MODEL
SWIGLU_LIMIT = 7.0
SWIGLU_ALPHA = 1.702
EXPERT_BLOCK = 128

DEEPNORM_ALPHA = (2 * DEPTH) ** 0.25
DEEPNORM_BETA = (8 * DEPTH) ** -0.25
LN_EPS = 1e-5
RMS_EPS = 1e-6

kernel_name = 'hybrid_mla_rwkv7_moe_deepnorm_adaln'


def layer_norm(x, g, b):
    xf = x.astype(jnp.float32)
    mu = xf.mean(-1, keepdims=True)
    var = jnp.square(xf - mu).mean(-1, keepdims=True)
    return ((xf - mu) * lax.rsqrt(var + LN_EPS) * g + b).astype(x.dtype)


def rms_norm(x, g):
    xf = x.astype(jnp.float32)
    return (xf * lax.rsqrt(jnp.square(xf).mean(-1, keepdims=True) + RMS_EPS) * g).astype(x.dtype)


def rope(x, cos, sin):
    xp = x.astype(jnp.float32).reshape(x.shape[:-1] + (x.shape[-1] // 2, 2))
    x0, x1 = xp[..., 0], xp[..., 1]
    out = jnp.stack([x0 * cos - x1 * sin, x0 * sin + x1 * cos], axis=-1)
    return out.reshape(x.shape).astype(x.dtype)


def causal_attention(q, k, v):
    b, s, h, dq = q.shape
    nb = s // Q_BLOCK
    scale = QK_HEAD_DIM ** -0.5
    q_blocks = jnp.moveaxis(q.reshape(b, nb, Q_BLOCK, h, dq), 1, 0)
    k_pos = jnp.arange(s)

    def one_block(args):
        qb, bi = args
        scores = jnp.einsum('bqhd,bkhd->bhqk', qb, k, preferred_element_type=jnp.float32) * scale
        q_pos = bi * Q_BLOCK + jnp.arange(Q_BLOCK)
        scores = jnp.where(k_pos[None, :] <= q_pos[:, None], scores, -jnp.inf)
        probs = jax.nn.softmax(scores, axis=-1).astype(v.dtype)
        return jnp.einsum('bhqk,bkhd->bqhd', probs, v)

    out = lax.map(one_block, (q_blocks, jnp.arange(nb)))
    return jnp.moveaxis(out, 0, 1).reshape(b, s, h, v.shape[-1])


def mla_group(p, cos, sin, q_norm_g, w_uq, kv_norm_g, w_uk, w_uv, out_g):
    b, s, _ = p.shape
    q_lat = p[..., :Q_LORA_RANK]
    kv_lat = p[..., Q_LORA_RANK:Q_LORA_RANK + KV_LORA_RANK]
    k_rot = p[..., Q_LORA_RANK + KV_LORA_RANK:]
    q = (rms_norm(q_lat, q_norm_g) @ w_uq).reshape(b, s, MLA_HEADS, QK_HEAD_DIM)
    q = jnp.concatenate([q[..., :QK_NOPE_DIM],
                         rope(q[..., QK_NOPE_DIM:], cos[:, :, None, :], sin[:, :, None, :])], axis=-1)
    c_kv = rms_norm(kv_lat, kv_norm_g)
    k_nope = (c_kv @ w_uk).reshape(b, s, MLA_HEADS, QK_NOPE_DIM)
    v = (c_kv @ w_uv).reshape(b, s, MLA_HEADS, V_HEAD_DIM)
    k_rot = rope(k_rot, cos, sin)
    k = jnp.concatenate([k_nope, jnp.broadcast_to(k_rot[:, :, None, :], (b, s, MLA_HEADS, QK_ROPE_DIM))], axis=-1)
    o = causal_attention(q, k, v)
    return rms_norm(o.reshape(b, s, MLA_OUT), out_g)


def rwkv7_recurrence(r, decay, k, v, kk, a):
    b, s, h, n = r.shape

    def step(state, inp):
        r_t, w_t, k_t, v_t, kk_t, a_t = inp
        sa = jnp.einsum('bhij,bhj->bhi', state, -kk_t)
        state = (state * w_t[:, :, None, :]
                 + sa[..., :, None] * (kk_t * a_t)[..., None, :]
                 + v_t[..., :, None] * k_t[..., None, :])
        return state, jnp.einsum('bhij,bhj->bhi', state, r_t)

    xs = tuple(jnp.moveaxis(z, 1, 0) for z in (r, decay, k, v, kk, a))
    _, out = lax.scan(step, jnp.zeros((b, h, n, n), jnp.float32), xs)
    return jnp.moveaxis(out, 0, 1)


def rwkv7_group(p, mu, w0, w2, a0, a2, g2, k_k, k_a, r_k, gn_g, gn_b, v_first, vres):
    b, s, _ = p.shape
    h_, n_ = RWKV_HEADS, RWKV_HEAD_DIM
    p_prev = jnp.pad(p[:, :-1], ((0, 0), (1, 0), (0, 0)))
    p = p + mu * (p_prev - p)
    c3 = 3 * RWKV_DIM
    r = p[..., :RWKV_DIM]
    k = p[..., RWKV_DIM:2 * RWKV_DIM]
    v = p[..., 2 * RWKV_DIM:c3]
    wd = p[..., c3:c3 + DECAY_LORA]
    ad = p[..., c3 + DECAY_LORA:c3 + DECAY_LORA + ICLR_LORA]
    gd = p[..., c3 + DECAY_LORA + ICLR_LORA:]
    w = -jax.nn.softplus(-(w0 + jnp.tanh(wd) @ w2).astype(jnp.float32)) - 0.5
    decay = jnp.exp(-jnp.exp(w))
    a = jax.nn.sigmoid(a0 + ad @ a2)
    g = jax.nn.sigmoid(gd) @ g2
    if vres is not None:
        v0, v1, v2 = vres
        v = v + (v_first - v) * jax.nn.sigmoid(v0 + (v @ v1) @ v2)
    heads = lambda z: z.reshape(b, s, h_, n_).astype(jnp.float32)
    r_h, k_h, v_h, a_h, d_h = heads(r), heads(k), heads(v), heads(a), heads(decay)
    kk = k_h * k_k.reshape(h_, n_)
    kk = kk / jnp.maximum(jnp.linalg.norm(kk, axis=-1, keepdims=True), 1e-12)
    k_h = k_h * (1.0 + (a_h - 1.0) * k_a.reshape(h_, n_))
    o = rwkv7_recurrence(r_h, d_h, k_h, v_h, kk, a_h)
    mean = o.mean(-1, keepdims=True)
    var = jnp.square(o - mean).mean(-1, keepdims=True)
    o = ((o - mean) * lax.rsqrt(var + RWKV_GN_EPS)).reshape(b, s, RWKV_DIM) * gn_g + gn_b
    bonus = jnp.sum(r_h * k_h * r_k, axis=-1, keepdims=True) * v_h
    o = (o + bonus.reshape(b, s, RWKV_DIM)) * g
    return o.astype(p.dtype), v


def clamped_swiglu(hh):
    x_glu = jnp.minimum(hh[..., ::2], SWIGLU_LIMIT)
    x_lin = jnp.clip(hh[..., 1::2], -SWIGLU_LIMIT, SWIGLU_LIMIT)
    return x_glu * jax.nn.sigmoid(SWIGLU_ALPHA * x_glu) * (x_lin + 1.0)


def moe_ffn(h, router_w, router_b, w1, b1, w2, b2):
    b, s, d = h.shape
    t = b * s
    ht = h.reshape(t, d)
    logits = (ht @ router_w + router_b).astype(jnp.float32)
    top_val, top_idx = lax.top_k(logits, TOP_K)
    gates = jax.nn.softmax(top_val, axis=-1)
    n_assign = t * TOP_K
    e_flat = top_idx.reshape(n_assign)
    tok_flat = jnp.arange(n_assign, dtype=jnp.int32) // TOP_K
    order = jnp.argsort(e_flat)
    e_sorted = e_flat[order]
    counts = jnp.bincount(e_flat, length=N_EXPERTS)
    starts = jnp.cumsum(counts) - counts
    padded = (counts + EXPERT_BLOCK - 1) // EXPERT_BLOCK * EXPERT_BLOCK
    pad_ends = jnp.cumsum(padded)
    pad_starts = pad_ends - padded
    dest = pad_starts[e_sorted] + (jnp.arange(n_assign) - starts[e_sorted])
    n_blocks = -(-n_assign // EXPERT_BLOCK) + N_EXPERTS
    n_slots = n_blocks * EXPERT_BLOCK
    slot_tok = jnp.full((n_slots,), t, jnp.int32).at[dest].set(tok_flat[order])
    slot_gate = jnp.zeros((n_slots,), jnp.float32).at[dest].set(gates.reshape(n_assign)[order])
    block_expert = jnp.minimum(
        jnp.searchsorted(pad_ends, jnp.arange(n_blocks) * EXPERT_BLOCK, side='right'), N_EXPERTS - 1)
    h_pad = jnp.concatenate([ht, jnp.zeros((1, d), ht.dtype)], axis=0)

    def expert_block(args):
        idx, e = args
        u = clamped_swiglu(h_pad[idx] @ w1[e] + b1[e])
        return u @ w2[e] + b2[e]

    y_slots = lax.map(expert_block, (slot_tok.reshape(n_blocks, EXPERT_BLOCK), block_expert))
    y = jnp.zeros((t + 1, d), jnp.float32).at[slot_tok].add(
        y_slots.reshape(n_slots, d).astype(jnp.float32) * slot_gate[:, None])
    return y[:t].reshape(b, s, d).astype(h.dtype)


def setup_inputs(seed: int = 0) -> dict:
    key = jax.random.key(seed)
    keys = iter(jax.random.split(key, 64))

    def nrm(shape, scale):
        return jax.random.normal(next(keys), shape, jnp.float32) * scale

    L, D, C, E, F = DEPTH, D_MODEL, RWKV_DIM, N_EXPERTS, D_FF_EXPERT
    fan = D ** -0.5
    x = nrm((BATCH, SEQ, D), 1.0)
    c = nrm((BATCH, D), 1.0)
    positions = (jnp.arange(SEQ, dtype=jnp.int32)[None, :]
                 + jax.random.randint(next(keys), (BATCH, 1), 0, 2048, dtype=jnp.int32))
    emb_ln_g = 1.0 + nrm((D,), 0.02)
    emb_ln_b = nrm((D,), 0.02)
    ada_w = nrm((L, D, 6 * D), 0.1 * fan)
    ada_b = nrm((L, 6 * D), 0.02)
    w_in = jnp.concatenate([
        nrm((L, D, MLA_IN), fan),
        nrm((L, D, 2 * C), fan),
        nrm((L, D, C), fan * DEEPNORM_BETA),
        nrm((L, D, DECAY_LORA + ICLR_LORA + GATE_LORA), fan)], axis=-1)
    q_norm_g = 1.0 + nrm((L, Q_LORA_RANK), 0.02)
    w_uq = nrm((L, Q_LORA_RANK, MLA_HEADS * QK_HEAD_DIM), Q_LORA_RANK ** -0.5)
    kv_norm_g = 1.0 + nrm((L, KV_LORA_RANK), 0.02)
    w_uk = nrm((L, KV_LORA_RANK, MLA_HEADS * QK_NOPE_DIM), KV_LORA_RANK ** -0.5)
    w_uv = nrm((L, KV_LORA_RANK, MLA_HEADS * V_HEAD_DIM), KV_LORA_RANK ** -0.5 * DEEPNORM_BETA)
    mla_out_g = 1.0 + nrm((L, MLA_OUT), 0.02)
    rwkv_mu = jax.random.uniform(next(keys), (L, RWKV_IN), jnp.float32)
    ramp = jnp.linspace(0.0, 1.0, C) ** 0.85
    rwkv_w0 = -6.5 + 5.0 * ramp[None, :] + nrm((L, C), 0.1)
    rwkv_w2 = nrm((L, DECAY_LORA, C), 0.1 * DECAY_LORA ** -0.5)
    rwkv_a0 = nrm((L, C), 0.1)
    rwkv_a2 = nrm((L, ICLR_LORA, C), 0.1 * ICLR_LORA ** -0.5)
    rwkv_g2 = nrm((L, GATE_LORA, C), GATE_LORA ** -0.5)
    rwkv_k_k = 0.85 + nrm((L, C), 0.02)
    rwkv_k_a = 1.0 + nrm((L, C), 0.02)
    rwkv_r_k = nrm((L, RWKV_HEADS, RWKV_HEAD_DIM), 0.1)
    rwkv_gn_g = 1.0 + nrm((L, C), 0.02)
    rwkv_gn_b = nrm((L, C), 0.02)
    vres_v0 = 1.0 + nrm((L - 1, C), 0.1)
    vres_v1 = nrm((L - 1, C, VRES_LORA), C ** -0.5)
    vres_v2 = nrm((L - 1, VRES_LORA, C), 0.1 * VRES_LORA ** -0.5)
    w_o = nrm((L, MIX_DIM, D), MIX_DIM ** -0.5 * DEEPNORM_BETA)
    ln1_g = 1.0 + nrm((L, D), 0.02)
    ln1_b = nrm((L, D), 0.02)
    router_w = nrm((L, D, E), fan)
    router_b = nrm((L, E), 0.01)
    exp_w1 = nrm((L, E, D, 2 * F), fan)
    exp_b1 = nrm((L, E, 2 * F), 0.02)
    exp_w2 = nrm((L, E, F, D), F ** -0.5 * DEEPNORM_BETA)
    exp_b2 = nrm((L, E, D), 0.02)
    ln2_g = 1.0 + nrm((L, D), 0.02)
    ln2_b = nrm((L, D), 0.02)
    return {'x': x, 'c': c, 'positions': positions, 'emb_ln_g': emb_ln_g, 'emb_ln_b': emb_ln_b,
            'ada_w': ada_w, 'ada_b': ada_b, 'w_in': w_in,
            'q_norm_g': q_norm_g, 'w_uq': w_uq, 'kv_norm_g': kv_norm_g, 'w_uk': w_uk, 'w_uv': w_uv,
            'mla_out_g': mla_out_g, 'rwkv_mu': rwkv_mu, 'rwkv_w0': rwkv_w0, 'rwkv_w2': rwkv_w2,
            'rwkv_a0': rwkv_a0, 'rwkv_a2': rwkv_a2, 'rwkv_g2': rwkv_g2, 'rwkv_k_k': rwkv_k_k,
            'rwkv_k_a': rwkv_k_a, 'rwkv_r_k': rwkv_r_k, 'rwkv_gn_g': rwkv_gn_g, 'rwkv_gn_b': rwkv_gn_b,
            'vres_v0': vres_v0, 'vres_v1': vres_v1, 'vres_v2': vres_v2, 'w_o': w_o,
            'ln1_g': ln1_g, 'ln1_b': ln1_b, 'router_w': router_w, 'router_b': router_b,
            'exp_w1': exp_w1, 'exp_b1': exp_b1, 'exp_w2': exp_w2, 'exp_b2': exp_b2,
            'ln2_g': ln2_g, 'ln2_b': ln2_b}


def reference(x, c, positions, emb_ln_g, emb_ln_b, ada_w, ada_b, w_in,
              q_norm_g, w_uq, kv_norm_g, w_uk, w_uv, mla_out_g,
              rwkv_mu, rwkv_w0, rwkv_w2, rwkv_a0, rwkv_a2, rwkv_g2, rwkv_k_k, rwkv_k_a, rwkv_r_k,
              rwkv_gn_g, rwkv_gn_b, vres_v0, vres_v1, vres_v2, w_o, ln1_g, ln1_b,
              router_w, router_b, exp_w1, exp_b1, exp_w2, exp_b2, ln2_g, ln2_b):
    inv_freq = ROPE_THETA ** (-jnp.arange(0, QK_ROPE_DIM, 2, dtype=jnp.float32) / QK_ROPE_DIM)
    ang = positions.astype(jnp.float32)[..., None] * inv_freq
    cos, sin = jnp.cos(ang), jnp.sin(ang)
    c_act = jax.nn.silu(c)
    x = layer_norm(x, emb_ln_g, emb_ln_b)
    v_first = None
    for i in range(DEPTH):
        mod = c_act @ ada_w[i] + ada_b[i]
        sh1, sc1, gt1, sh2, sc2, gt2 = [m[:, None, :] for m in jnp.split(mod, 6, axis=-1)]
        h = x * (1.0 + sc1) + sh1
        p = h @ w_in[i]
        mla_o = mla_group(p[..., :MLA_IN], cos, sin, q_norm_g[i], w_uq[i], kv_norm_g[i],
                          w_uk[i], w_uv[i], mla_out_g[i])
        vres = None if i == 0 else (vres_v0[i - 1], vres_v1[i - 1], vres_v2[i - 1])
        rwkv_o, v_i = rwkv7_group(p[..., MLA_IN:], rwkv_mu[i], rwkv_w0[i], rwkv_w2[i], rwkv_a0[i],
                                  rwkv_a2[i], rwkv_g2[i], rwkv_k_k[i], rwkv_k_a[i], rwkv_r_k[i],
                                  rwkv_gn_g[i], rwkv_gn_b[i], v_first, vres)
        if i == 0:
            v_first = v_i
        mix = jnp.concatenate([mla_o, rwkv_o], axis=-1) @ w_o[i]
        x = layer_norm(DEEPNORM_ALPHA * x + (1.0 + gt1) * mix, ln1_g[i], ln1_b[i])
        h = x * (1.0 + sc2) + sh2
        ffn = moe_ffn(h, router_w[i], router_b[i], exp_w1[i], exp_b1[i], exp_w2[i], exp_b2[i])
        x = layer_norm(DEEPNORM_ALPHA * x + (1.0 + gt2) * ffn, ln2_g[i], ln2_b[i])
    return x
```

```python
import numpy as np
from contextlib import ExitStack
import concourse.bass as bass
import concourse.mybir as mybir
from concourse.bass_utils import run_bass_kernel_spmd

F32 = mybir.dt.float32; BF16 = mybir.dt.bfloat16; I32 = mybir.dt.int32; U32 = mybir.dt.uint32
AF = mybir.ActivationFunctionType; ALU = mybir.AluOpType; AX = mybir.AxisListType

D = 1024; L = 2; NE = 32; TOPK = 4
ALPHA = (2 * L) ** 0.25
LN_EPS = 1e-5; RMS_EPS = 1e-6; GN_EPS = 64e-5
CAP = 1024
NCH = 18

class Trk:
    __slots__ = ("w", "r", "excl")
    def __init__(self, excl=False): self.w = None; self.r = {}; self.excl = excl

ENGS = ("pe", "act", "dve", "pool", "sp")
NDS = 8
class Sched:
    def __init__(self):
        self.ops = {e: [] for e in ENGS}
        self.cnt = {e: 0 for e in ENGS}
        self.dcnt = {e: 0 for e in ENGS}
        self.waited = {e: {} for e in ENGS}
        self.lastdma = {}
    def op(self, eng, fn, r=(), w=(), dma=False, serial=False):
        deps = []
        if serial and self.lastdma.get(eng) is not None: deps.append((self.lastdma[eng], True))
        for t in r:
            if t.w is not None: deps.append((t.w, True))
            if t.excl:
                for ev in t.r.values(): deps.append((ev, False))
        for t in w:
            if t.w is not None: deps.append((t.w, False))
            for ev in t.r.values(): deps.append((ev, False))
        waits = {}
        for (semkey, val, src, isdma), raw in deps:
            if src == eng and not isdma:
                if eng == "pe": continue
                if not raw: continue
            if self.waited[eng].get(semkey, 0) >= val: continue
            if waits.get(semkey, 0) < val: waits[semkey] = val
        for k, v in waits.items(): self.waited[eng][k] = v
        if dma:
            i = self.dcnt[eng]; self.dcnt[eng] += 1
            semkey = ("d", eng, i % NDS); val = 16 * (i // NDS + 1); inc = 16
        else:
            self.cnt[eng] += 1; semkey = ("e", eng); val = self.cnt[eng]; inc = 1
        ev = (semkey, val, eng, dma)
        if dma: self.lastdma[eng] = ev
        self.ops[eng].append((list(waits.items()), fn, semkey, inc))
        for t in r:
            old = t.r.get(semkey)
            if old is None or old[1] < val: t.r[semkey] = ev
        for t in w:
            t.w = ev; t.r = {}
    def barrier(self):
        snap = []
        for en in ENGS:
            if self.cnt[en] > 0: snap.append((("e", en), self.cnt[en]))
            n = self.dcnt[en]
            for k in range(NDS):
                c = (n - k + NDS - 1) // NDS if n > k else 0
                if c > 0: snap.append((("d", en, k), 16 * c))
        for eng in ENGS:
            waits = [(k, v) for k, v in snap if self.waited[eng].get(k, 0) < v and k != ("e", eng)]
            for k, v in waits: self.waited[eng][k] = v
            self.ops[eng].append((waits, None, None, 0))
    def emit(self, nc):
        with ExitStack() as st:
            sems = {}
            for e in ENGS:
                sems[("e", e)] = st.enter_context(nc.semaphore("se_" + e))
                for k in range(NDS):
                    sems[("d", e, k)] = st.enter_context(nc.semaphore(f"sd_{e}_{k}"))
            block = st.enter_context(nc.Block())
            def mk(ename):
                ops = self.ops[ename]
                def body(e):
                    for waits, fn, semkey, inc in ops:
                        for k, v in waits: e.wait_ge(sems[k], v)
                        if fn is not None: fn(e).then_inc(sems[semkey], inc)
                    if ename == "sp":
                        for en in ENGS:
                            n = self.dcnt[en]
                            for k in range(NDS):
                                c = (n - k + NDS - 1) // NDS if n > k else 0
                                if c > 0: e.wait_ge(sems[("d", en, k)], 16 * c)
                            if self.cnt[en] > 0 and en != ename: e.wait_ge(sems[("e", en)], self.cnt[en])
                return body
            block.tensor(mk("pe")); block.scalar(mk("act")); block.vector(mk("dve"))
            block.gpsimd(mk("pool")); block.sync(mk("sp"))

class B:
    def __init__(self, S, dbg=()):
        self.S = S; self.NT = S // 128; self.NB = S // 512
        self.nc = bass.Bass("TRN2", target_bir_lowering=False)
        self.K = Sched()
        self.stacks = [ExitStack()]
        self.dbg = dbg
        self.ins = {}; self.outs = {}
        self.rr = 0
    def din(self, name, shape, dt=F32):
        a = self.nc.dram_tensor(name, list(shape), dt, kind="ExternalInput").ap(); self.ins[name] = a; return a
    def dout(self, name, shape, dt=F32):
        a = self.nc.dram_tensor(name, list(shape), dt, kind="ExternalOutput").ap(); self.outs[name] = a; return a
    def dscr(self, name, shape, dt=F32):
        return self.nc.dram_tensor("i_" + name, list(shape), dt, kind="Internal").ap()
    def sb(self, name, shape, dt=F32):
        self.uid = getattr(self, "uid", 0) + 1
        return self.stacks[-1].enter_context(self.nc.sbuf_tensor(f"s{self.uid}_{name}", list(shape), dt))
    def ps(self, name, shape, dt=F32):
        return self.stacks[0].enter_context(self.nc.psum_tensor("p_" + name, list(shape), dt))
    def dma(self, out, in_, r=(), w=(), q="sp"):
        self.K.op(q, lambda e: e.dma_start(out=out, in_=in_), r, w, dma=True)
    def mm(self, out, lhsT, rhs, start, stop, r=(), w=()):
        self.K.op("pe", lambda e: e.matmul(out, lhsT, rhs, start=start, stop=stop), r, w)
    def tr(self, out, in_, ident, r=(), w=()):
        self.K.op("pe", lambda e: e.transpose(out, in_, ident), r, w)
    def act(self, out, in_, func, r=(), w=(), bias=None, scale=None, accum=None):
        kw = {}
        if bias is not None: kw["bias"] = bias
        if scale is not None: kw["scale"] = scale
        if accum is not None: kw["accum_out"] = accum
        self.K.op("act", lambda e: e.activation(out=out, in_=in_, func=func, **kw), r, w)
    def tt(self, out, in0, in1, op, r=(), w=(), eng="dve"):
        self.K.op(eng, lambda e: e.tensor_tensor(out, in0, in1, op), r, w)
    def ts(self, out, in0, s1, s2, op0, op1=None, r=(), w=(), eng="dve"):
        if op1 is None:
            self.K.op(eng, lambda e: e.tensor_scalar(out, in0, s1, None, op0), r, w)
        else:
            self.K.op(eng, lambda e: e.tensor_scalar(out, in0, s1, s2, op0, op1), r, w)
    def stt(self, out, in0, scalar, in1, op0, op1, r=(), w=()):
        self.K.op("dve", lambda e: e.scalar_tensor_tensor(out, in0, scalar, in1, op0, op1), r, w)
    def cp(self, out, in_, r=(), w=(), eng="dve"):
        self.K.op(eng, lambda e: e.tensor_copy(out, in_), r, w)
    def recip(self, out, in_, r=(), w=()):
        self.K.op("dve", lambda e: e.reciprocal(out, in_), r, w)
    def breg(self, e, v):
        if not hasattr(self, "_bregs"): self._bregs = {}
        if v not in self._bregs: self._bregs[v] = e.to_reg(v)
        return self._bregs[v]
    def memset(self, ap, v, w=(), eng="dve"):
        self.K.op(eng, lambda e: e.memset(ap, v), (), w)

    def push(self):
        self.stacks.append(ExitStack())
    def pop(self):
        self.K.barrier()
        self.stacks.pop().close()

    def build(self, stop_after=None, layers=L, l0=0):
        S, NT, NB = self.S, self.NT, self.NB
        nc = self.nc
        dbg = self.dbg
        x_in = self.din("x", [S, D])
        cT_in = self.din("cT", [128, 8])
        pos_in = self.din("pos", [1, S], I32)
        consts_in = self.din("consts", [128, 1280])
        embrow_in = self.din("embrow", [2, D])
        ada_w_in = self.din("ada_w", [L, 128, 8, 6 * D])
        vecs_in = self.din("vecs", [L, 128, 128])
        w_in_in = self.din("w_in", [L, 128, 8, NCH * 128])
        w_uq_in = self.din("w_uq", [L, 128, 2, 1536])
        w_ukv_in = self.din("w_ukv", [L, 128, 1024])
        w_lora_in = self.din("w_lora", [L, 128, 1536])
        vres1_in = self.din("vres1", [128, 4, 32])
        vres2_in = self.din("vres2", [32, 512])
        w_o_in = self.din("w_o", [L, 128, 8, D])
        rows_in = self.din("rows", [L, 4 * D + NE])
        router_w_in = self.din("router_w", [L, 128, 8, NE])
        b1v_in = self.din("b1v", [L * NE * 128, 16])
        exp_b2_in = self.din("exp_b2", [L, NE, D])
        b1tab_in = self.din("b1tab", [L * NE, 2 * D])
        y_out = self.dout("y", [S, D])
        xres = self.dscr("xres", [S, D]); t_xres = [Trk() for _ in range(NT)]
        xmid = self.dscr("xmid", [S, D]); t_xmid = [Trk() for _ in range(NT)]
        vfirst = self.dscr("vfirst", [4, 128, S]); t_vfirst = [Trk() for _ in range(NB)]
        d_qnT = self.dscr("qnT", [3, 128, S], BF16); t_dqn = [Trk() for _ in range(NB)]
        d_krot = self.dscr("krot", [128, S], BF16); t_dkrot = [Trk() for _ in range(NB)]
        d_rw = self.dscr("rw", [5, 4, 128, S], BF16); t_drw = [Trk() for _ in range(NB)]
        d_gC = self.dscr("gC", [4, 128, NT]); t_dgC = [Trk() for _ in range(NB)]
        d_bonus = self.dscr("bonus", [4, 128, S]); t_dbonus = [Trk() for _ in range(NB)]
        d_gT = self.dscr("gT", [4, 128, S], BF16); t_dgT = [Trk() for _ in range(NB)]
        d_ro = self.dscr("ro", [4, 128, S], BF16); t_dro = [Trk() for _ in range(NT)]
        self.scr_hs = self.dscr("hslots", [(4 * NT + NE) * 128, D], BF16); self.scr_ys = self.dscr("yslots", [(4 * NT + NE) * 128, D])
        d_rope = self.dscr("ropetab", [2, 32, S]); t_drope = Trk()
        cst = self.sb("cst", [128, 1280]); t_cst = Trk()
        cstb = self.sb("cstb", [128, 1024], BF16); t_cstb = Trk()
        self.dma(cst[:], consts_in[:, :], w=[t_cst])
        self.dma(cstb[:], consts_in[:, 0:1024], w=[t_cstb], q="pool")
        ident_f = cst[:, 0:128]; ident_b = cstb[:, 0:128]; ones_f = cst[:, 640:768]
        m_su_b = cstb[:, 128:256]; m_iu_b = cstb[:, 256:384]; m_sl_b = cstb[:, 384:512]
        bones_b = cstb[:, 512:640]; ones_b = cstb[:, 640:768]
        c_rmseps = cst[:, 770:771]; c_one = cst[:, 771:772]; c_mhalf = cst[:, 772:773]; c_gneps = cst[:, 773:774]
        c_zero = cst[:, 774:775]
        PS = [self.ps(f"ps{i}", [128, 512]) for i in range(8)]
        t_PS = [Trk(excl=True) for _ in range(8)]
        def bank():
            i = self.rr % 7; self.rr += 1; return PS[i], t_PS[i]
        vecs = self.sb("vecs", [128, L, 128]); t_vecs = Trk()
        for l in range(L):
            self.dma(vecs[:, l, :], vecs_in[l], w=[t_vecs])
        modT = self.sb("modT", [128, L, 48]); t_mod = Trk()
        dvec = self.sb("dvec", [128, L, 32]); t_dvec = Trk()
        stat = self.sb("stat", [128, 16]); t_stat = Trk()
        xt = [self.sb(f"xt{i}", [128, D]) for i in range(2)]; t_xt = [Trk(), Trk()]

        def layernorm(xa, t_x, gB, bB, t_g, t_b):
            self.K.op("dve", lambda e: e.bn_stats(stat[:, 0:6], xa[:, 0:512]), [t_x], [t_stat])
            self.K.op("dve", lambda e: e.bn_stats(stat[:, 6:12], xa[:, 512:1024]), [t_x], [t_stat])
            self.K.op("dve", lambda e: e.bn_aggr(stat[:, 12:14], stat[:, 0:12]), [t_stat], [t_stat])
            self.ts(stat[:, 14:15], stat[:, 13:14], LN_EPS, None, ALU.add, r=[t_stat], w=[t_stat])
            self.act(stat[:, 14:15], stat[:, 14:15], AF.Sqrt, r=[t_stat], w=[t_stat])
            self.recip(stat[:, 15:16], stat[:, 14:15], r=[t_stat], w=[t_stat])
            self.ts(xa, xa, stat[:, 12:13], stat[:, 15:16], ALU.subtract, ALU.mult, r=[t_x, t_stat], w=[t_x])
            self.tt(xa, xa, gB, ALU.mult, r=[t_x, t_g], w=[t_x])
            self.tt(xa, xa, bB, ALU.add, r=[t_x, t_b], w=[t_x])

        self.push()
        cT = self.sb("cTf", [128, 8]); t_cT = Trk()
        self.dma(cT[:], cT_in[:, :], w=[t_cT])
        cTb = self.sb("cTb", [128, 8], BF16); t_cTb = Trk()
        self.act(cTb[:], cT[:], AF.Silu, r=[t_cT], w=[t_cTb])
        adaw = [self.sb(f"adaw{i}", [128, 8, 384], BF16) for i in range(2)]; t_adaw = [Trk(), Trk()]
        for l in range(L):
            pb, tpb = bank()
            for cg in range(16):
                bi = cg % 2
                self.dma(adaw[bi][:], ada_w_in[l, :, :, cg * 384:(cg + 1) * 384], w=[t_adaw[bi]], q="pool")
                for cc in range(3):
                    col = cg * 3 + cc
                    for kc in range(8):
                        self.mm(pb[:, col:col + 1], adaw[bi][:, kc, cc * 128:(cc + 1) * 128], cTb[:, kc:kc + 1],
                                kc == 0, kc == 7, r=[t_adaw[bi], t_cTb], w=[tpb])
            self.tt(modT[:, l, :], pb[:, 0:48], vecs[:, l, 0:48], ALU.add, r=[tpb, t_vecs], w=[t_mod])
            for v in (1, 2, 4, 5):
                self.ts(modT[:, l, v * 8:(v + 1) * 8], modT[:, l, v * 8:(v + 1) * 8], 1.0, None, ALU.add, r=[t_mod], w=[t_mod])
            self.ts(dvec[:, l, 0:18], vecs[:, l, 48:66], -1.0, 1.0, ALU.mult, ALU.add, r=[t_vecs], w=[t_dvec])
            self.ts(dvec[:, l, 18:22], vecs[:, l, 73:77], -1.0, None, ALU.mult, r=[t_vecs], w=[t_dvec])
        if "mod" in dbg:
            o = self.dout("dbg_mod", [128, L, 48]); self.dma(o[:, :, :], modT[:], r=[t_mod])
        cosF = self.sb("cosF", [128, S]); sinF = self.sb("sinF", [128, S]); t_rope = Trk()
        posi = self.sb("posi", [128, 512], I32); t_posi = Trk()
        rt1 = self.sb("rt1", [128, 512]); t_rt1 = Trk()
        rt2 = self.sb("rt2", [128, 512]); t_rt2 = Trk()
        for blk in range(NB):
            cs = slice(blk * 512, (blk + 1) * 512)
            self.dma(posi[:], pos_in[:, cs].broadcast_to([128, 512]), w=[t_posi])
            self.cp(rt1[:], posi[:], r=[t_posi], w=[t_rt1])
            self.ts(rt1[:], rt1[:], cst[:, 768:769], None, ALU.mult, r=[t_rt1, t_cst], w=[t_rt1])
            self.cp(posi[:], rt1[:], r=[t_rt1], w=[t_posi])
            self.cp(rt2[:], posi[:], r=[t_posi], w=[t_rt2])
            self.tt(rt2[:], rt1[:], rt2[:], ALU.subtract, r=[t_rt1, t_rt2], w=[t_rt2])
            self.act(sinF[:, cs], rt2[:], AF.Sin, r=[t_rt2], w=[t_rope], scale=float(2 * np.pi))
            self.ts(sinF[:, cs], sinF[:, cs], cst[:, 769:770], None, ALU.mult, r=[t_rope, t_cst], w=[t_rope])
            self.ts(rt1[:], rt1[:], 0.25, None, ALU.add, r=[t_rt1], w=[t_rt1])
            self.cp(posi[:], rt1[:], r=[t_rt1], w=[t_posi])
            self.cp(rt2[:], posi[:], r=[t_posi], w=[t_rt2])
            self.tt(rt2[:], rt1[:], rt2[:], ALU.subtract, r=[t_rt1, t_rt2], w=[t_rt2])
            self.act(cosF[:, cs], rt2[:], AF.Sin, r=[t_rt2], w=[t_rope], scale=float(2 * np.pi))
        self.dma(d_rope[0], cosF[64:96, :], r=[t_rope], w=[t_drope])
        self.dma(d_rope[1], sinF[64:96, :], r=[t_rope], w=[t_drope])
        rowB = [self.sb(f"rowB{i}", [128, D]) for i in range(2)]; t_rowB = [Trk() for _ in range(2)]
        self.dma(rowB[0][:], embrow_in[0:1, :].broadcast_to([128, D]), w=[t_rowB[0]])
        self.dma(rowB[1][:], embrow_in[1:2, :].broadcast_to([128, D]), w=[t_rowB[1]])
        for tt_ in range(NT):
            bi = tt_ % 2
            self.dma(xt[bi][:], x_in[tt_ * 128:(tt_ + 1) * 128, :], w=[t_xt[bi]])
            layernorm(xt[bi][:], t_xt[bi], rowB[0][:], rowB[1][:], t_rowB[0], t_rowB[1])
            self.dma(xres[tt_ * 128:(tt_ + 1) * 128, :], xt[bi][:], r=[t_xt[bi]], w=[t_xres[tt_]])
        self.pop()

        def dbg_out(name, shape, src_ap, r, dt=F32):
            o = self.dout(name, shape, dt)
            self.dma(o, src_ap, r=r)

        for l in range(l0, layers):
            V = lambda a, b: vecs[:, l, a:b]
            self.push()
            cosB = [self.sb(f"cosB{i}", [128, 512]) for i in range(2)]; sinB = [self.sb(f"sinB{i}", [128, 512]) for i in range(2)]
            t_ropeB = [Trk(), Trk()]
            hT = self.sb("hT", [128, 8, 512], BF16); t_hT = Trk()
            winb = self.sb("winb", [128, 8, NCH * 128], BF16); t_win = Trk()
            self.dma(winb[:], w_in_in[l], w=[t_win], q="pool")
            wlora = self.sb("wlora", [128, 1536], BF16); t_wl = Trk()
            self.dma(wlora[:], w_lora_in[l], w=[t_wl], q="pool")
            vr1 = self.sb("vr1", [128, 4, 32], BF16); vr2 = self.sb("vr2", [32, 512], BF16); t_vr = Trk()
            self.dma(vr1[:], vres1_in[:, :, :], w=[t_vr], q="pool")
            self.dma(vr2[:], vres2_in[:, :], w=[t_vr], q="pool")
            praw = self.sb("praw", [128, 15, 513]); t_praw = [Trk() for _ in range(15)]
            pq = self.sb("pq", [128, 3, 512]); t_pq = Trk()
            sqb = self.sb("sqb", [128, 3, 512], BF16); t_sqb = Trk()
            rs = self.sb("rs", [128, 2, 512]); t_rs = Trk()
            qst = self.sb("qst", [128, 3, 512], BF16); t_qst = Trk()
            krst = self.sb("krst", [128, 512], BF16); t_krst = Trk()
            E3 = self.sb("E3", [128, 512]); t_E3 = Trk()
            E4 = self.sb("E4", [128, 512]); t_E4 = Trk()
            wdT = self.sb("wdT", [128, 512], BF16); adT = self.sb("adT", [128, 512], BF16); sgT = self.sb("sgT", [128, 512], BF16)
            t_wdT = Trk(); t_adT = Trk(); t_sgT = Trk()
            Ev = self.sb("Ev", [128, 4, 512]); t_Ev = [Trk() for _ in range(4)]
            Evb = self.sb("Evb", [128, 4, 512], BF16); t_Evb = Trk()
            vv1 = self.sb("vv1", [32, 512], BF16); t_vv1 = Trk()
            vf = self.sb("vf", [128, 512]); t_vf = Trk()
            names = ["Er", "Ek", "Ea", "Eld", "Ekap", "Ekm", "Eb", "EL", "Ex1", "Ex2"]
            Ets = [{n: self.sb(f"{n}_{i}", [128, 512]) for n in names} for i in range(2)]; tEs = [{n: Trk() for n in names} for i in range(2)]
            Et, tE = Ets[0], tEs[0]
            stg = self.sb("stg", [128, 2, 6, 512], BF16); t_stg = [Trk(), Trk()]
            gcs = self.sb("gcs", [128, 2, 4]); t_gcs = [Trk(), Trk()]
            for blk in range(NB):
                cs = slice(blk * 512, (blk + 1) * 512)
                cosF_, sinF_, t_rope = cosB[blk % 2], sinB[blk % 2], t_ropeB[blk % 2]
                self.dma(cosF_[64:96, :], d_rope[0][:, cs], r=[t_drope], w=[t_rope])
                self.dma(sinF_[64:96, :], d_rope[1][:, cs], r=[t_drope], w=[t_rope])
                for j in range(4):
                    tt_ = blk * 4 + j; bi = tt_ % 2
                    self.dma(xt[bi][:], xres[tt_ * 128:(tt_ + 1) * 128, :], r=[t_xres[tt_]], w=[t_xt[bi]])
                    for half in range(2):
                        pb, tpb = bank()
                        for q in range(4):
                            c = half * 4 + q
                            self.tr(pb[:, q * 128:(q + 1) * 128], xt[bi][:, c * 128:(c + 1) * 128], ident_f,
                                    r=[t_xt[bi], t_cst], w=[tpb])
                        for q in range(4):
                            c = half * 4 + q
                            self.act(hT[:, c, j * 128:(j + 1) * 128], pb[:, q * 128:(q + 1) * 128], AF.Identity,
                                     r=[tpb, t_mod], w=[t_hT], scale=modT[:, l, 8 + c:9 + c], bias=modT[:, l, c:c + 1])
                def pchunk(c):
                    pb, tpb = bank()
                    for kc in range(8):
                        self.mm(pb[:], winb[:, kc, c * 128:(c + 1) * 128], hT[:, kc, :], kc == 0, kc == 7,
                                r=[t_win, t_hT], w=[tpb])
                    return pb, tpb
                def shift(c, out_ap, t_out):
                    pb, tpb = pchunk(c)
                    ci = c - 3
                    if blk == 0:
                        self.memset(praw[:, ci, 0:1], 0.0, w=[t_praw[ci]])
                    else:
                        self.cp(praw[:, ci, 0:1], praw[:, ci, 512:513], r=[t_praw[ci]], w=[t_praw[ci]])
                    self.act(praw[:, ci, 1:513], pb[:], AF.Copy, r=[tpb], w=[t_praw[ci]])
                    self.ts(out_ap, praw[:, ci, 1:513], dvec[:, l, c:c + 1], None, ALU.mult, r=[t_praw[ci], t_dvec], w=[t_out])
                    self.stt(out_ap, praw[:, ci, 0:512], vecs[:, l, 48 + c:49 + c], out_ap, ALU.mult, ALU.add,
                             r=[t_praw[ci], t_vecs, t_out], w=[t_out])
                for c in range(3):
                    pb, tpb = pchunk(c)
                    self.act(pq[:, c, :], pb[:], AF.Copy, r=[tpb], w=[t_pq])
                    self.act(sqb[:, c, :], pb[:], AF.Square, r=[tpb], w=[t_sqb])
                pb, tpb = bank()
                self.mm(pb[:], ones_b, sqb[:, 0, :], True, False, r=[t_cstb, t_sqb], w=[tpb])
                self.mm(pb[:], ones_b, sqb[:, 1, :], False, True, r=[t_cstb, t_sqb], w=[tpb])
                self.act(rs[:, 0, :], pb[:], AF.Sqrt, r=[tpb, t_cst], w=[t_rs], scale=1.0 / 256, bias=c_rmseps)
                pb, tpb = bank()
                self.mm(pb[:], ones_b, sqb[:, 2, :], True, True, r=[t_cstb, t_sqb], w=[tpb])
                self.act(rs[:, 1, :], pb[:], AF.Sqrt, r=[tpb, t_cst], w=[t_rs], scale=1.0 / 128, bias=c_rmseps)
                self.recip(rs[:], rs[:], r=[t_rs], w=[t_rs])
                for c in range(3):
                    self.stt(qst[:, c, :], pq[:, c, :], V(66 + c, 67 + c), rs[:, 0 if c < 2 else 1, :], ALU.mult, ALU.mult,
                             r=[t_pq, t_vecs, t_rs], w=[t_qst])
                    self.dma(d_qnT[c, :, cs], qst[:, c, :], r=[t_qst], w=[t_dqn[blk]], q="pool")
                shift(3, E3[:], t_E3)
                self.act(wdT[0:64, :], E3[0:64, :], AF.Tanh, r=[t_E3], w=[t_wdT])
                shift(4, E4[:], t_E4)
                self.act(adT[0:64, :], E4[0:64, :], AF.Copy, r=[t_E4], w=[t_adT])
                self.tt(E3[64:96, :], E3[64:96, :], cosF_[64:96, :], ALU.mult, r=[t_E3, t_rope], w=[t_E3])
                self.tt(E4[64:96, :], E4[64:96, :], sinF_[64:96, :], ALU.mult, r=[t_E4, t_rope], w=[t_E4])
                self.tt(krst[64:96, :], E3[64:96, :], E4[64:96, :], ALU.add, r=[t_E3, t_E4], w=[t_krst])
                self.dma(d_krot[64:96, cs], krst[64:96, :], r=[t_krst], w=[t_dkrot[blk]], q="pool")
                shift(5, E3[:], t_E3)
                self.act(sgT[:], E3[:], AF.Sigmoid, r=[t_E3], w=[t_sgT])
                for fc in range(4):
                    shift(6 + fc, Ev[:, fc, :], t_Ev[fc])
                if l == 0 or "novres" in dbg:
                    for fc in range(4):
                        self.dma(vfirst[fc, :, cs], Ev[:, fc, :], r=[t_Ev[fc]], w=[t_vfirst[blk]], q="pool")
                else:
                    for fc in range(4):
                        self.act(Evb[:, fc, :], Ev[:, fc, :], AF.Copy, r=[t_Ev[fc]], w=[t_Evb])
                    pb, tpb = bank()
                    for fc in range(4):
                        self.mm(pb[0:32, :], vr1[:, fc, :], Evb[:, fc, :], fc == 0, fc == 3, r=[t_vr, t_Evb], w=[tpb])
                    self.act(vv1[:], pb[0:32, :], AF.Copy, r=[tpb], w=[t_vv1])
                    for fc in range(4):
                        pb, tpb = bank()
                        self.mm(pb[:], vr2[0:32, fc * 128:(fc + 1) * 128], vv1[0:32, :], True, True, r=[t_vr, t_vv1], w=[tpb])
                        x1 = Et["Ex1"]; self.act(x1[:], pb[:], AF.Sigmoid, r=[tpb, t_vecs], w=[tE["Ex1"]], bias=V(101 + fc, 102 + fc))
                        self.dma(vf[:], vfirst[fc, :, cs], r=[t_vfirst[blk]], w=[t_vf])
                        self.tt(vf[:], vf[:], Ev[:, fc, :], ALU.subtract, r=[t_vf, t_Ev[fc]], w=[t_vf])
                        self.tt(vf[:], vf[:], x1[:], ALU.mult, r=[t_vf, tE["Ex1"]], w=[t_vf])
                        self.tt(Ev[:, fc, :], Ev[:, fc, :], vf[:], ALU.add, r=[t_Ev[fc], t_vf], w=[t_Ev[fc]])
                def fcchain(fc, sb_):
                    Er, Ek, Ea, Eld, Ekap, Ekm, Eb, EL, Ex1, Ex2 = [Ets[sb_][n] for n in names]
                    tr_, tk_, ta_, tld, tkap, tkm, tb_, tL, tx1, tx2 = [tEs[sb_][n] for n in names]
                    fs = slice(fc * 128, (fc + 1) * 128)
                    sg_ = stg[:, sb_]; tsg = t_stg[sb_]
                    shift(10 + 2 * fc, Er[:], tr_)
                    shift(11 + 2 * fc, Ek[:], tk_)
                    pb, tpb = bank()
                    self.mm(pb[:], wlora[0:64, fs], wdT[0:64, :], True, True, r=[t_wl, t_wdT], w=[tpb])
                    yield
                    self.act(Ex1[:], pb[:], AF.Exp, r=[tpb, t_dvec], w=[tx1], scale=-1.0, bias=dvec[:, l, 18 + fc:19 + fc])
                    yield
                    self.act(Ex1[:], Ex1[:], AF.Ln, r=[tx1, t_cst], w=[tx1], bias=c_one)
                    yield
                    self.act(Ex1[:], Ex1[:], AF.Exp, r=[tx1, t_cst], w=[tx1], scale=-1.0, bias=c_mhalf)
                    yield
                    self.ts(Eld[:], Ex1[:], -1.0, None, ALU.mult, r=[tx1], w=[tld])
                    yield
                    pb, tpb = bank()
                    self.mm(pb[:], wlora[0:64, 512 + fc * 128:512 + (fc + 1) * 128], adT[0:64, :], True, True, r=[t_wl, t_adT], w=[tpb])
                    yield
                    self.act(Ea[:], pb[:], AF.Sigmoid, r=[tpb, t_vecs], w=[ta_], bias=V(77 + fc, 78 + fc))
                    yield
                    self.ts(Ekap[:], Ek[:], V(81 + fc, 82 + fc), None, ALU.mult, r=[tk_, t_vecs], w=[tkap])
                    yield
                    sq1 = stg[:, sb_, 5, :]
                    self.act(sq1, Ekap[:], AF.Square, r=[tkap], w=[tsg])
                    yield
                    pb, tpb = bank()
                    self.mm(pb[:], bones_b, sq1, True, True, r=[t_cstb, tsg], w=[tpb])
                    yield
                    self.act(Ex1[:], pb[:], AF.Sqrt, r=[tpb], w=[tx1])
                    yield
                    self.ts(Ex1[:], Ex1[:], 1e-12, None, ALU.max, r=[tx1], w=[tx1])
                    yield
                    self.recip(Ex1[:], Ex1[:], r=[tx1], w=[tx1])
                    yield
                    self.tt(Ekap[:], Ekap[:], Ex1[:], ALU.mult, r=[tkap, tx1], w=[tkap])
                    yield
                    self.ts(Ekm[:], Ea[:], -1.0, V(85 + fc, 86 + fc), ALU.add, ALU.mult, r=[ta_, t_vecs], w=[tkm])
                    yield
                    self.stt(Ekm[:], Ekm[:], 1.0, Ek[:], ALU.add, ALU.mult, r=[tkm, tk_], w=[tkm])
                    yield
                    self.tt(Eb[:], Ekap[:], Ea[:], ALU.mult, r=[tkap, ta_], w=[tb_])
                    yield
                    self.stt(Ex1[:], Er[:], V(89 + fc, 90 + fc), Ekm[:], ALU.mult, ALU.mult, r=[tr_, t_vecs, tkm], w=[tx1])
                    yield
                    self.act(sq1, Ex1[:], AF.Copy, r=[tx1], w=[tsg])
                    yield
                    pb, tpb = bank()
                    self.mm(pb[:], bones_b, sq1, True, True, r=[t_cstb, tsg], w=[tpb])
                    yield
                    self.tt(Ex2[:], pb[:], Ev[:, fc, :], ALU.mult, r=[tpb, t_Ev[fc]], w=[tx2])
                    yield
                    self.dma(d_bonus[fc, :, cs], Ex2[:], r=[tx2], w=[t_dbonus[blk]], q="pool")
                    yield
                    for q in range(4):
                        qs = slice(q * 128, (q + 1) * 128)
                        self.K.op("dve", lambda e, qs=qs, EL=EL, Eld=Eld: e.tensor_tensor_scan(EL[:, qs], ones_f, Eld[:, qs], 0.0, ALU.mult, ALU.add),
                                  [t_cst, tld], [tL])
                    gq = gcs[:, sb_, :]
                    self.act(gq, EL[:].rearrange("p (q t) -> p q t", t=128)[:, :, 127], AF.Exp, r=[tL], w=[t_gcs[sb_]])
                    yield
                    self.dma(d_gC[fc, :, blk * 4:(blk + 1) * 4], gq, r=[t_gcs[sb_]], w=[t_dgC[blk]], q="pool")
                    yield
                    self.tt(Ex1[:], EL[:], Eld[:], ALU.subtract, r=[tL, tld], w=[tx1])
                    yield
                    self.act(Ex1[:], Ex1[:], AF.Exp, r=[tx1], w=[tx1])
                    yield
                    self.tt(sg_[:, 0, :], Ekap[:], Ex1[:], ALU.mult, r=[tkap, tx1], w=[tsg])
                    yield
                    self.act(Ex1[:], EL[:], AF.Exp, r=[tL], w=[tx1])
                    yield
                    self.tt(sg_[:, 1, :], Er[:], Ex1[:], ALU.mult, r=[tr_, tx1], w=[tsg])
                    yield
                    self.act(Ex1[:], EL[:], AF.Exp, r=[tL], w=[tx1], scale=-1.0)
                    yield
                    self.tt(sg_[:, 2, :], Ekm[:], Ex1[:], ALU.mult, r=[tkm, tx1], w=[tsg])
                    yield
                    self.tt(sg_[:, 3, :], Eb[:], Ex1[:], ALU.mult, r=[tb_, tx1], w=[tsg])
                    yield
                    self.act(sg_[:, 4, :], Ev[:, fc, :], AF.Copy, r=[t_Ev[fc]], w=[tsg])
                    yield
                    for k5 in range(5):
                        self.dma(d_rw[k5, fc, :, cs], sg_[:, k5, :], r=[tsg], w=[t_drw[blk]], q="pool")
                    pb, tpb = bank()
                    self.mm(pb[:], wlora[:, 1024 + fc * 128:1024 + (fc + 1) * 128], sgT[:], True, True, r=[t_wl, t_sgT], w=[tpb])
                    yield
                    self.act(sg_[:, 5, :], pb[:], AF.Copy, r=[tpb], w=[tsg])
                    yield
                    self.dma(d_gT[fc, :, cs], sg_[:, 5, :], r=[tsg], w=[t_dgT[blk]], q="pool")
                    yield

                for f0 in (0, 2):
                    gens = [fcchain(f0, 0), fcchain(f0 + 1, 1)]
                    while gens:
                        for g in list(gens):
                            try: next(g)
                            except StopIteration: gens.remove(g)
            self.pop()
            if stop_after == f"1a{l}":
                break
            self.push()
            ld5 = [self.sb(f"ld5_{i}", [64, 5, 8, 128], BF16) for i in range(3)]; t_ld5 = [Trk() for _ in range(3)]
            tm_s = [self.sb(f"tm{i}", [128, 4, 8, 64], BF16) for i in range(3)]; t_tm_s = [[Trk() for _ in range(4)] for _ in range(3)]
            gCall = self.sb("gCall", [64, 8, NT]); t_gCall = Trk()
            self.dma(gCall[:], d_gC.rearrange("f (hp j) n -> j (f hp) n", hp=2), r=t_dgC, w=[t_gCall])
            A1_s = [self.sb(f"A1{i}", [128, 8, 256], BF16) for i in range(3)]; t_A1_s = [Trk() for _ in range(3)]
            A2_s = [self.sb(f"A2{i}", [128, 8, 256], BF16) for i in range(3)]; t_A2_s = [Trk() for _ in range(3)]
            Lm_s = [self.sb(f"Lm{i}", [128, 8, 128], BF16) for i in range(3)]; t_Lm_s = [Trk() for _ in range(3)]
            Pp_s = [[self.sb(f"Pp{j}_{i}", [128, 8, 128], BF16) for i in range(2)] for j in range(3)]; t_Pp_s = [[Trk(), Trk()] for _ in range(3)]
            Qq_s = [[self.sb(f"Qq{j}_{i}", [128, 8, 128], BF16) for i in range(2)] for j in range(3)]; t_Qq_s = [[Trk(), Trk()] for _ in range(3)]
            Yf_s = [self.sb(f"Yf{i}", [128, 8, 128]) for i in range(3)]; t_Yf_s = [Trk() for _ in range(3)]
            Yb_s = [self.sb(f"Yb{i}", [128, 8, 128], BF16) for i in range(3)]; t_Yb_s = [Trk() for _ in range(3)]
            kapP_s = [self.sb(f"kapP{i}", [64, 8, 128], BF16) for i in range(3)]; t_kapP_s = [Trk() for _ in range(3)]
            LkV_s = [self.sb(f"LkV{i}", [128, 8, 64], BF16) for i in range(3)]; t_LkV_s = [Trk() for _ in range(3)]
            Uloc_s = [self.sb(f"Uloc{i}", [128, 8, 64]) for i in range(3)]; t_Uloc_s = [Trk() for _ in range(3)]
            Ub = self.sb("Ub", [128, 8, 64], BF16); t_Ub = Trk()
            Un = self.sb("Un", [128, 8, 64], BF16); t_Un = Trk()
            KV_s = [self.sb(f"KV{i}", [64, 8, 64]) for i in range(3)]; t_KV_s = [Trk() for _ in range(3)]
            Tst = self.sb("Tst", [64, 8, 64]); t_T = Trk()
            Tb = [self.sb(f"Tb{i}", [64, 8, 64], BF16) for i in range(2)]; t_Tb = [Trk(), Trk()]
            Ttmp = self.sb("Ttmp", [64, 8, 64]); t_Ttmp = Trk()
            Of = self.sb("Of", [128, 8, 64]); t_Of = Trk()
            On = self.sb("On", [128, 8, 64]); t_On = Trk()
            gst = self.sb("gst", [128, 8, 8]); t_gst = Trk()
            gmv = self.sb("gmv", [128, 8, 2]); t_gmv = Trk()
            bon_s = [self.sb(f"bon{i}", [128, 4, 128]) for i in range(3)]; t_bon_s = [Trk() for _ in range(3)]
            gTt_s = [self.sb(f"gTt{i}", [128, 4, 128], BF16) for i in range(3)]; t_gTt_s = [Trk() for _ in range(3)]
            R1 = self.sb("R1", [128, 4, 128]); t_R1 = Trk()
            rot = self.sb("rot", [128, 4, 128], BF16); t_rot = Trk()
            self.memset(Tst[:], 0.0, w=[t_T])
            self.memset(Tb[0][:], 0.0, w=[t_Tb[0]])
            mask_ui = cstb[:, 128:384].rearrange("p (o n) -> p o n", o=1)
            mask_sl = m_sl_b.rearrange("p (o n) -> p o n", o=1)
            identb3 = ident_f.rearrange("p (o n) -> p o n", o=1)
            def bfv(pb):
                return pb[:].bitcast(BF16)
            def chunk(c):
                st = c % 3
                A1 = A1_s[st]; t_A1 = t_A1_s[st]
                A2 = A2_s[st]; t_A2 = t_A2_s[st]
                Lm = Lm_s[st]; t_Lm = t_Lm_s[st]
                Yf = Yf_s[st]; t_Yf = t_Yf_s[st]
                Yb = Yb_s[st]; t_Yb = t_Yb_s[st]
                kapP = kapP_s[st]; t_kapP = t_kapP_s[st]
                LkV = LkV_s[st]; t_LkV = t_LkV_s[st]
                Uloc = Uloc_s[st]; t_Uloc = t_Uloc_s[st]
                KV = KV_s[st]; t_KV = t_KV_s[st]
                bon = bon_s[st]; t_bon = t_bon_s[st]
                gTt = gTt_s[st]; t_gTt = t_gTt_s[st]
                tm = tm_s[st]; t_tm = t_tm_s[st]
                Pp = Pp_s[st]; t_Pp = t_Pp_s[st]
                Qq = Qq_s[st]; t_Qq = t_Qq_s[st]
                blk = c // 4; cs = slice(c * 128, (c + 1) * 128)
                lb = ld5[c % 3]; tlb = t_ld5[c % 3]
                for k5 in range(5):
                    self.dma(lb[:, k5, :, :], d_rw[k5].rearrange("f (hp j) s -> j (f hp) s", hp=2)[:, :, cs],
                             r=[t_drw[blk]], w=[tlb])
                self.dma(bon[:], d_bonus.rearrange("f p s -> p f s")[:, :, cs], r=[t_dbonus[blk]], w=[t_bon])
                self.dma(gTt[:], d_gT.rearrange("f p s -> p f s")[:, :, cs], r=[t_dgT[blk]], w=[t_gTt])
                for ti, k5 in enumerate((0, 2, 3, 4)):
                    pb, tpb = bank()
                    pv = bfv(pb)
                    for h in range(8):
                        self.tr(pv[:, h * 64:(h + 1) * 64], lb[0:64, k5, h, :], ident_b[0:64, 0:64], r=[tlb, t_cstb], w=[tpb])
                    self.cp(tm[:, ti, :, :], pv[:, 0:512].rearrange("p (h i) -> p h i", h=8), r=[tpb], w=[t_tm[ti]])
                yield 0
                for hp in range(4):
                    for which, (Adst, tA, k5) in enumerate(((A1, t_A1, 2), (A2, t_A2, 3))):
                        pb, tpb = bank()
                        for hh in range(2):
                            h = 2 * hp + hh
                            self.mm(pb[:, hh * 256:(hh + 1) * 256], lb[0:64, k5, h, :], lb[0:64, 0:2, h, :], True, True, r=[tlb], w=[tpb])
                        self.tt(Adst[:, 2 * hp:2 * hp + 2, :], pb[:].rearrange("p (h n) -> p h n", h=2),
                                mask_ui.broadcast_to([128, 2, 256]), ALU.mult, r=[tpb, t_cstb], w=[tA])
                for half in range(2):
                    pb, tpb = bank()
                    for hh in range(4):
                        h = 4 * half + hh
                        self.mm(pb[:, hh * 128:(hh + 1) * 128], lb[0:64, 0, h, :], lb[0:64, 3, h, :], True, True, r=[tlb], w=[tpb])
                    self.tt(Lm[:, 4 * half:4 * half + 4, :], pb[:].rearrange("p (h n) -> p h n", h=4),
                            mask_sl.broadcast_to([128, 4, 128]), ALU.mult, r=[tpb, t_cstb], w=[t_Lm])
                yield 0
                Nm = A2[:, :, 0:128]
                self.tt(Yf[:], identb3.broadcast_to([128, 8, 128]), Nm, ALU.subtract, r=[t_cst, t_A2], w=[t_Yf])
                self.act(Yb[:], Yf[:], AF.Copy, r=[t_Yf], w=[t_Yb])
                Qc, tQc, Pc, tPc = Lm, t_Lm, Nm, t_A2
                for k in range(6):
                    yield 0
                    Qn, tQn = Qq[k % 2], t_Qq[k % 2]
                    Pn, tPn = Pp[k % 2], t_Pp[k % 2]
                    for half in range(2):
                        pb, tpb = bank()
                        for hh in range(4):
                            h = 4 * half + hh
                            self.mm(pb[:, hh * 128:(hh + 1) * 128], Pc[:, h, :], Qc[:, h, :], True, True, r=[tPc, tQc], w=[tpb])
                        self.act(Qn[:, 4 * half:4 * half + 4, :], pb[:].rearrange("p (h n) -> p h n", h=4), AF.Copy, r=[tpb], w=[tQn])
                    if k < 5:
                        for half in range(2):
                            pb, tpb = bank()
                            for hh in range(4):
                                h = 4 * half + hh
                                self.mm(pb[:, hh * 128:(hh + 1) * 128], Qc[:, h, :], Pc[:, h, :], True, True, r=[tPc, tQc], w=[tpb])
                            self.cp(Pn[:, 4 * half:4 * half + 4, :], pb[:].rearrange("p (h n) -> p h n", h=4), r=[tpb], w=[tPn])
                    yield 0
                    for half in range(2):
                        pb, tpb = bank()
                        for hh in range(4):
                            h = 4 * half + hh
                            self.mm(pb[:, hh * 128:(hh + 1) * 128], Qn[:, h, :], Yb[:, h, :], True, True, r=[tQn, t_Yb], w=[tpb])
                        self.tt(Yf[:, 4 * half:4 * half + 4, :], Yf[:, 4 * half:4 * half + 4, :],
                                pb[:].rearrange("p (h n) -> p h n", h=4), ALU.add, r=[tpb, t_Yf], w=[t_Yf])
                    self.act(Yb[:], Yf[:], AF.Copy, r=[t_Yf], w=[t_Yb])
                    Qc, tQc, Pc, tPc = Qn, tQn, Pn, tPn
                yield 0
                for half in range(2):
                    pb, tpb = bank()
                    for hh in range(4):
                        h = 4 * half + hh
                        self.mm(pb[0:64, hh * 128:(hh + 1) * 128], tm[:, 0, h, :], Yb[:, h, :], True, True, r=[t_tm[0], t_Yb], w=[tpb])
                    self.act(kapP[:, 4 * half:4 * half + 4, :], pb[0:64, :].rearrange("p (h n) -> p h n", h=4), AF.Copy, r=[tpb], w=[t_kapP])
                pb, tpb = bank()
                for h in range(8):
                    self.mm(pb[0:64, h * 64:(h + 1) * 64], tm[:, 1, h, :], tm[:, 3, h, :], True, True, r=[t_tm[1], t_tm[3]], w=[tpb])
                self.cp(KV[:], pb[0:64, :].rearrange("p (h n) -> p h n", h=8), r=[tpb], w=[t_KV])
                pb, tpb = bank()
                for h in range(8):
                    self.mm(pb[:, h * 64:(h + 1) * 64], A1[:, h, 0:128], tm[:, 3, h, :], True, True, r=[t_A1, t_tm[3]], w=[tpb])
                self.act(LkV[:], pb[:].rearrange("p (h n) -> p h n", h=8), AF.Copy, r=[tpb], w=[t_LkV])
                pb, tpb = bank()
                for h in range(8):
                    self.mm(pb[:, h * 64:(h + 1) * 64], Yb[:, h, :], LkV[:, h, :], True, True, r=[t_Yb, t_LkV], w=[tpb])
                self.cp(Uloc[:], pb[:].rearrange("p (h n) -> p h n", h=8), r=[tpb], w=[t_Uloc])
                yield 1
                Tc, tTc = Tb[c % 2], t_Tb[c % 2]
                Tn_, tTn = Tb[(c + 1) % 2], t_Tb[(c + 1) % 2]
                pb, tpb = bank()
                for h in range(8):
                    self.mm(pb[:, h * 64:(h + 1) * 64], kapP[0:64, h, :], Tc[0:64, h, :], True, True, r=[t_kapP, tTc], w=[tpb])
                self.tt(Ub[:], pb[:].rearrange("p (h n) -> p h n", h=8), Uloc[:], ALU.add, r=[tpb, t_Uloc], w=[t_Ub])
                self.ts(Un[:], Ub[:], -1.0, None, ALU.mult, r=[t_Ub], w=[t_Un], eng="pool")
                self.tt(Ttmp[:], Tst[:], KV[:], ALU.add, r=[t_T, t_KV], w=[t_Ttmp])
                pb2, tpb2 = bank()
                for h in range(8):
                    self.mm(pb2[0:64, h * 64:(h + 1) * 64], tm[:, 2, h, :], Ub[:, h, :], True, True, r=[t_tm[2], t_Ub], w=[tpb2])
                self.tt(Ttmp[:], Ttmp[:], pb2[0:64, :].rearrange("p (h n) -> p h n", h=8), ALU.subtract, r=[t_Ttmp, tpb2], w=[t_Ttmp])
                self.tt(Tst[:], Ttmp[:], gCall[:, :, c:c + 1].broadcast_to([64, 8, 64]), ALU.mult, r=[t_Ttmp, t_gCall], w=[t_T])
                self.act(Tn_[:], Tst[:], AF.Copy, r=[t_T], w=[tTn])
                pb, tpb = bank()
                for h in range(8):
                    o_ = pb[:, h * 64:(h + 1) * 64]
                    self.mm(o_, lb[0:64, 1, h, :], Tc[0:64, h, :], True, False, r=[tlb, tTc], w=[tpb])
                    self.mm(o_, A1[:, h, 128:256], tm[:, 3, h, :], False, False, r=[t_A1, t_tm[3]], w=[tpb])
                    self.mm(o_, A2[:, h, 128:256], Un[:, h, :], False, True, r=[t_A2, t_Un], w=[tpb])
                self.act(Of[:], pb[:].rearrange("p (h n) -> p h n", h=8), AF.Copy, r=[tpb], w=[t_Of])
                for h in range(8):
                    self.K.op("dve", lambda e, h=h, gst=gst, Of=Of: e.bn_stats(gst[:, h, 0:6], Of[:, h, :]), [t_Of], [t_gst])
                for h in range(8):
                    self.K.op("dve", lambda e, h=h, gst=gst, gmv=gmv: e.bn_aggr(gmv[:, h, :], gst[:, h, 0:6]), [t_gst], [t_gmv])
                self.act(gst[:, :, 6], gmv[:, :, 1], AF.Sqrt, r=[t_gmv, t_cst], w=[t_gst], bias=c_gneps)
                self.recip(gst[:, :, 7], gst[:, :, 6], r=[t_gst], w=[t_gst])
                self.tt(On[:], Of[:], gmv[:, :, 0:1].broadcast_to([128, 8, 64]), ALU.subtract, r=[t_Of, t_gmv], w=[t_On])
                self.tt(On[:], On[:], gst[:, :, 7:8].broadcast_to([128, 8, 64]), ALU.mult, r=[t_On, t_gst], w=[t_On])
                pb, tpb = bank()
                for fc in range(4):
                    self.tr(pb[:, fc * 128:(fc + 1) * 128], On[:, 2 * fc:2 * fc + 2, :].rearrange("p h i -> p (h i)"), ident_f,
                            r=[t_On, t_cst], w=[tpb])
                for fc in range(4):
                    self.act(R1[:, fc, :], pb[:, fc * 128:(fc + 1) * 128], AF.Identity, r=[tpb, t_vecs], w=[t_R1],
                             scale=V(93 + fc, 94 + fc), bias=V(97 + fc, 98 + fc))
                self.tt(R1[:], R1[:], bon[:], ALU.add, r=[t_R1, t_bon], w=[t_R1])
                self.tt(rot[:], R1[:], gTt[:], ALU.mult, r=[t_R1, t_gTt], w=[t_rot])
                self.dma(d_ro.rearrange("f p s -> p f s")[:, :, cs], rot[:], r=[t_rot], w=[t_dro[c]], q="pool")
            def drain(g):
                for _ in g: pass
            for c0 in range(0, NT, 3):
                gs = [chunk(c0 + i) for i in range(min(3, NT - c0))]
                live = list(gs)
                while live:
                    for g in list(live):
                        if next(g) == 1: live.remove(g)
                for g in gs: drain(g)
            self.pop()
            if stop_after == f"1b{l}":
                break
            self.push()
            cosB = [self.sb(f"cosB{i}", [128, 512]) for i in range(2)]; sinB = [self.sb(f"sinB{i}", [128, 512]) for i in range(2)]
            t_ropeB = [Trk(), Trk()]
            qnT = self.sb("qnT", [128, 2, S], BF16); t_qnT = Trk()
            ckvT = self.sb("ckvT", [128, S], BF16); t_ckvT = Trk()
            kTb = [self.sb(f"kTb{i}", [128, S], BF16) for i in range(2)]; t_kT = [Trk(), Trk()]
            self.dma(qnT[:], d_qnT[0:2].rearrange("c p s -> p c s"), r=t_dqn, w=[t_qnT])
            self.dma(ckvT[:], d_qnT[2], r=t_dqn, w=[t_ckvT])
            for i in range(2):
                self.dma(kTb[i][64:96, :], d_krot[64:96, :], r=t_dkrot, w=[t_kT[i]])
            wuq = self.sb("wuq", [128, 2, 1536], BF16); wukv = self.sb("wukv", [128, 1024], BF16); wo = self.sb("wo", [128, 8, D], BF16)
            t_w2 = Trk()
            self.dma(wuq[:], w_uq_in[l], w=[t_w2], q="pool")
            self.dma(wukv[:], w_ukv_in[l], w=[t_w2], q="pool")
            self.dma(wo[:], w_o_in[l], w=[t_w2], q="pool")
            Vaug = self.sb("Vaug", [128, NT, 8, 65], BF16); t_V = [Trk() for _ in range(NT)]
            self.K.op("pool", lambda e, Vaug=Vaug: e.memset(Vaug[:], 1.0), (), t_V)
            lnB = [self.sb(f"lnB{i}", [128, D]) for i in range(2)]; t_lnB = [Trk(), Trk()]
            self.dma(lnB[0][:], rows_in[l:l + 1, 0:D].broadcast_to([128, D]), w=[t_lnB[0]])
            self.dma(lnB[1][:], rows_in[l:l + 1, D:2 * D].broadcast_to([128, D]), w=[t_lnB[1]])
            gtB = self.sb("gtB", [128, D]); t_gtB = Trk()
            dg = self.sb("dg", [128, 128]); t_dg = Trk()
            for half in range(2):
                pb, tpb = bank()
                for q in range(4):
                    c = half * 4 + q
                    self.ts(dg[:], ident_f, modT[:, l, 16 + c:17 + c], None, ALU.mult, r=[t_cst, t_mod], w=[t_dg])
                    self.mm(pb[:, q * 128:(q + 1) * 128], ones_f, dg[:], True, True, r=[t_cst, t_dg], w=[tpb])
                self.cp(gtB[:, half * 512:(half + 1) * 512], pb[:], r=[tpb], w=[t_gtB])
            qTh = [self.sb(f"qTh{i}", [128, 512], BF16) for i in range(2)]; t_qTh = [Trk(), Trk()]
            rq1 = self.sb("rq1", [128, 512]); rq2 = self.sb("rq2", [128, 512]); t_rq = Trk()
            PT = [self.sb(f"PT{i}", [128, 512], BF16) for i in range(4)]; t_PT = [Trk() for _ in range(4)]
            oTs = self.sb("oTs", [128, 512]); t_oTs = Trk()
            o_tm = self.sb("o_tm", [128, 4, 512]); t_otm = Trk()
            rec = self.sb("rec", [128, 8]); t_rec = Trk()
            junk = self.sb("junk", [128, 512]); t_junk = Trk()
            mixT = self.sb("mixT", [128, 8, 512], BF16); t_mixT = Trk()
            ymix = self.sb("ymix", [128, D]); t_ymix = Trk()
            SCALE = float(96 ** -0.5)
            ptc = [0]
            ptn = 0
            for QB in range(NB):
                cs = slice(QB * 512, (QB + 1) * 512)
                cosF_, sinF_, t_rope = cosB[QB % 2], sinB[QB % 2], t_ropeB[QB % 2]
                self.dma(cosF_[64:96, :], d_rope[0][:, cs], r=[t_drope], w=[t_rope])
                self.dma(sinF_[64:96, :], d_rope[1][:, cs], r=[t_drope], w=[t_rope])
                for j in range(4):
                    tt_ = QB * 4 + j
                    pb, tpb = bank()
                    self.mm(pb[:], ckvT[:, tt_ * 128:(tt_ + 1) * 128], wukv[:, 512:1024], True, True, r=[t_ckvT, t_w2], w=[tpb])
                    self.cp(Vaug[:, tt_, :, 0:64], pb[:].rearrange("p (h i) -> p h i", h=8), r=[tpb], w=[t_V[tt_]])
                def prep(h):
                    kb_ = kTb[h % 2]; tkb = t_kT[h % 2]
                    for kb in range(QB + 1):
                        pb, tpb = bank()
                        self.mm(pb[0:64, :], wukv[:, h * 64:(h + 1) * 64], ckvT[:, kb * 512:(kb + 1) * 512], True, True, r=[t_w2, t_ckvT], w=[tpb])
                        self.act(kb_[0:64, kb * 512:(kb + 1) * 512], pb[0:64, :], AF.Copy, r=[tpb], w=[tkb])
                    qh = qTh[h % 2]; tqh = t_qTh[h % 2]
                    pbA, tpA = bank()
                    for kc in range(2):
                        self.mm(pbA[0:96, :], wuq[:, kc, h * 96:(h + 1) * 96], qnT[:, kc, cs], kc == 0, kc == 1, r=[t_w2, t_qnT], w=[tpA])
                    pbB, tpB = bank()
                    for kc in range(2):
                        self.mm(pbB[0:96, :], wuq[:, kc, 768 + h * 96:768 + (h + 1) * 96], qnT[:, kc, cs], kc == 0, kc == 1, r=[t_w2, t_qnT], w=[tpB])
                    self.act(qh[0:64, :], pbA[0:64, :], AF.Copy, r=[tpA], w=[tqh])
                    self.tt(rq1[64:96, :], pbA[64:96, :], cosF_[64:96, :], ALU.mult, r=[tpA, t_rope], w=[t_rq])
                    self.tt(rq2[64:96, :], pbB[64:96, :], sinF_[64:96, :], ALU.mult, r=[tpB, t_rope], w=[t_rq])
                    self.tt(qh[64:96, :], rq1[64:96, :], rq2[64:96, :], ALU.add, r=[t_rq], w=[tqh])
                def attn(h):
                    kb_ = kTb[h % 2]; tkb = t_kT[h % 2]
                    qh = qTh[h % 2]; tqh = t_qTh[h % 2]
                    pbO, tpO = PS[7], t_PS[7]
                    nkt = QB * 4 + 4
                    def smm(kt):
                        j = kt - QB * 4
                        n0 = 0 if j < 0 else j * 128
                        N = 512 - n0
                        pbS, tpS = bank()
                        self.mm(pbS[:, 0:N], kb_[0:96, kt * 128:(kt + 1) * 128], qh[0:96, n0:512], True, True, r=[tkb, tqh], w=[tpS])
                        return pbS, tpS, j, n0, N
                    nxt = smm(0)
                    for kt in range(nkt):
                        pbS, tpS, j, n0, N = nxt
                        if kt + 1 < nkt: nxt = smm(kt + 1)
                        pt = PT[ptc[0] % 4]; tpt = t_PT[ptc[0] % 4]; ptc[0] += 1
                        self.act(pt[:, 0:N], pbS[:, 0:N], AF.Exp, r=[tpS], w=[tpt], scale=SCALE)
                        if j >= 0:
                            self.tt(pt[:, 0:128], pt[:, 0:128], m_iu_b, ALU.mult, r=[tpt, t_cstb], w=[tpt])
                        self.mm(pbO[0:65, n0:512], Vaug[:, kt, h, :], pt[:, 0:N], kt == 0, kt == nkt - 1, r=[t_V[kt], tpt], w=[tpO])
                    self.act(oTs[0:65, :], pbO[0:65, :], AF.Copy, r=[tpO], w=[t_oTs])
                    pbt, tpt_ = bank()
                    for j in range(4):
                        self.tr(pbt[:, j * 65:(j + 1) * 65], oTs[0:65, j * 128:(j + 1) * 128], ident_f[0:65, 0:65], r=[t_oTs, t_cst], w=[tpt_])
                    pv = pbt[:, 0:260].rearrange("p (j n) -> p j n", j=4)
                    self.recip(rec[:, 0:4], pv[:, :, 64], r=[tpt_], w=[t_rec])
                    self.tt(o_tm[:, :, h * 64:(h + 1) * 64], pv[:, :, 0:64], rec[:, 0:4].rearrange("p (j o) -> p j o", o=1).broadcast_to([128, 4, 64]),
                            ALU.mult, r=[tpt_, t_rec], w=[t_otm])
                prep(0)
                for h in range(8):
                    if h + 1 < 8: prep(h + 1)
                    attn(h)
                self.dma(mixT[:, 4:8, :], d_ro.rearrange("f p s -> p f s")[:, :, cs], r=t_dro[QB * 4:QB * 4 + 4], w=[t_mixT])
                for j in range(4):
                    self.act(junk[:], o_tm[:, j, :], AF.Square, r=[t_otm], w=[t_junk, t_rec], accum=rec[:, 4:5])
                    self.act(rec[:, 5:6], rec[:, 4:5], AF.Sqrt, r=[t_rec, t_cst], w=[t_rec], scale=1.0 / 512, bias=c_rmseps)
                    self.recip(rec[:, 6:7], rec[:, 5:6], r=[t_rec], w=[t_rec])
                    self.ts(o_tm[:, j, :], o_tm[:, j, :], rec[:, 6:7], None, ALU.mult, r=[t_otm, t_rec], w=[t_otm])
                    pb, tpb = bank()
                    for c in range(4):
                        self.tr(pb[:, c * 128:(c + 1) * 128], o_tm[:, j, c * 128:(c + 1) * 128], ident_f, r=[t_otm, t_cst], w=[tpb])
                    for c in range(4):
                        self.act(mixT[:, c, j * 128:(j + 1) * 128], pb[:, c * 128:(c + 1) * 128], AF.Copy, r=[tpb, t_vecs], w=[t_mixT],
                                 scale=V(69 + c, 70 + c))
                if f"mixT{l}" in dbg:
                    if QB == 0: self.dbg_mix = self.dout(f"dbg_mixT{l}", [128, 8, S], BF16)
                    self.dma(self.dbg_mix[:, :, cs], mixT[:], r=[t_mixT])
                for j in range(4):
                    tt_ = QB * 4 + j; bi = tt_ % 2
                    self.dma(xt[bi][:], xres[tt_ * 128:(tt_ + 1) * 128, :], r=[t_xres[tt_]], w=[t_xt[bi]])
                    for half in range(2):
                        pb, tpb = bank()
                        for c in range(8):
                            self.mm(pb[:], mixT[:, c, j * 128:(j + 1) * 128], wo[:, c, half * 512:(half + 1) * 512], c == 0, c == 7,
                                    r=[t_mixT, t_w2], w=[tpb])
                        self.tt(ymix[:, half * 512:(half + 1) * 512], pb[:], gtB[:, half * 512:(half + 1) * 512], ALU.mult, r=[tpb, t_gtB], w=[t_ymix])
                    self.stt(xt[bi][:], xt[bi][:], ALPHA, ymix[:], ALU.mult, ALU.add, r=[t_xt[bi], t_ymix], w=[t_xt[bi]])
                    layernorm(xt[bi][:], t_xt[bi], lnB[0][:], lnB[1][:], t_lnB[0], t_lnB[1])
                    self.dma(xmid[tt_ * 128:(tt_ + 1) * 128, :], xt[bi][:], r=[t_xt[bi]], w=[t_xmid[tt_]], q="pool")
            self.pop()
            if "pt_at2" in dbg:
                tst = self.sb("tst", [128, 64]); t_tst = Trk()
                self.memset(tst[:], 3.0, w=[t_tst], eng="pool")
                o = self.dout("dbg_pt2", [128, 64]); self.dma(o[:, :], tst[:], r=[t_tst])
            if stop_after == f"2{l}":
                break
            self.push()
            NBLK = 4 * NT + NE; NSLOT = NBLK * 128; BIG = 1000000.0
            w1rows = self.ins.get("exp_w1"); w2rows = self.ins.get("exp_w2")
            if w1rows is None: w1rows = self.din("exp_w1", [L * NE * 128, 8 * 2 * D])
            if w2rows is None: w2rows = self.din("exp_w2", [L * NE * 128, 8 * D])
            b1rows = b1v_in
            d_hs = self.scr_hs; d_ys = self.scr_ys
            t_hs = [Trk() for _ in range(NBLK)]; t_ys = [Trk() for _ in range(NBLK)]
            lnB = [self.sb(f"ln2B{i}", [128, D]) for i in range(2)]; t_lnB = [Trk(), Trk()]
            self.dma(lnB[0][:], rows_in[l:l + 1, 2 * D:3 * D].broadcast_to([128, D]), w=[t_lnB[0]])
            self.dma(lnB[1][:], rows_in[l:l + 1, 3 * D:4 * D].broadcast_to([128, D]), w=[t_lnB[1]])
            b2all = self.sb("b2all", [NE, D], BF16); t_rw = Trk()
            self.dma(b2all[:], exp_b2_in[l], w=[t_rw], q="pool")
            gates = self.sb("gates", [128, NT, NE]); t_gates = [Trk() for _ in range(NT)]
            gk = self.sb("gk", [128, NT, 4]); t_gk = [Trk() for _ in range(NT)]
            slotidx = self.sb("slotidx", [128, NT, 4], I32); t_slotidx = [Trk() for _ in range(NT)]
            widx = self.sb("widx", [128, NBLK], I32); t_widx = Trk()
            eidx = self.sb("eidx", [128, NBLK], I32); t_eidx = Trk()
            gtB = self.sb("gt2B", [128, D]); t_gtB = Trk()
            self.push()
            rbB = self.sb("rbB", [128, NE]); t_rbB = Trk()
            self.dma(rbB[:], rows_in[l:l + 1, 4 * D:4 * D + NE].broadcast_to([128, NE]), w=[t_rbB])
            rwf = self.sb("rwf", [128, 8, NE]); h2f_s = [self.sb(f"h2f{i}", [128, 8, 128]) for i in range(4)]; t_h2f_s = [Trk() for _ in range(4)]
            xr_s = [self.sb(f"xr{i}", [128, D]) for i in range(4)]; t_xr_s = [Trk() for _ in range(4)]
            self.dma(rwf[:], router_w_in[l], w=[t_rw])
            dg = self.sb("dg2", [128, 128]); t_dg = Trk()
            def bcast_tile(name, col0, tl=None, ttr=None):
                if tl is None:
                    tl = self.sb(name, [128, D]); ttr = Trk()
                for half in range(2):
                    pb, tpb = bank()
                    for q in range(4):
                        c = half * 4 + q
                        self.ts(dg[:], ident_f, modT[:, l, col0 + c:col0 + c + 1], None, ALU.mult, r=[t_cst, t_mod], w=[t_dg])
                        self.mm(pb[:, q * 128:(q + 1) * 128], ones_f, dg[:], True, True, r=[t_cst, t_dg], w=[tpb])
                    self.cp(tl[:, half * 512:(half + 1) * 512], pb[:], r=[tpb], w=[ttr])
                return tl, ttr
            bcast_tile("gt2B", 40, gtB, t_gtB)
            scB, t_scB = bcast_tile("sc2B", 32)
            shB, t_shB = bcast_tile("sh2B", 24)
            lgs = self.sb("lgs", [128, NT, NE]); t_lgs = [Trk() for _ in range(NT)]
            m8s = self.sb("m8s", [128, NT, 8]); t_m8s = [Trk() for _ in range(NT)]
            maskb = self.sb("maskb", [128, NT, NE], BF16); t_maskb = [Trk() for _ in range(NT)]
            lg_s = [self.sb(f"lg{i}", [128, NE]) for i in range(4)]; m8_s = [self.sb(f"m8{i}", [128, 16]) for i in range(4)]; t_lg_s = [Trk() for _ in range(4)]; t_m8_s = [Trk() for _ in range(4)]
            msk_s = [self.sb(f"msk{i}", [128, NE]) for i in range(4)]; t_msk_s = [Trk() for _ in range(4)]
            def route_tile(tt_):
                st = tt_ % 4
                h2f, t_h2f, lg, t_lg, m8, t_m8, msk, t_msk = h2f_s[st], t_h2f_s[st], lg_s[st], t_lg_s[st], m8_s[st], t_m8_s[st], msk_s[st], t_msk_s[st]
                xq, t_xq = xr_s[st], t_xr_s[st]
                self.dma(xq[:], xmid[tt_ * 128:(tt_ + 1) * 128, :], r=[t_xmid[tt_]], w=[t_xq])
                yield
                for half in range(2):
                    pb, tpb = bank()
                    for q in range(4):
                        c = half * 4 + q
                        self.tr(pb[:, q * 128:(q + 1) * 128], xq[:, c * 128:(c + 1) * 128], ident_f, r=[t_xq, t_cst], w=[tpb])
                    for q in range(4):
                        c = half * 4 + q
                        self.ts(h2f[:, c, :], pb[:, q * 128:(q + 1) * 128], modT[:, l, 32 + c:33 + c], modT[:, l, 24 + c:25 + c], ALU.mult, ALU.add,
                                r=[tpb, t_mod], w=[t_h2f])
                pb, tpb = bank()
                for kc in range(8):
                    self.mm(pb[:, 0:NE], h2f[:, kc, :], rwf[:, kc, :], kc == 0, kc == 7, r=[t_h2f, t_rw], w=[tpb])
                self.tt(lgs[:, tt_, :], pb[:, 0:NE], rbB[:], ALU.add, r=[tpb, t_rbB], w=[t_lgs[tt_]])
                yield
                self.K.op("dve", lambda e, m8s=m8s, lgs=lgs, tt_=tt_: e.max(out=m8s[:, tt_, :], in_=lgs[:, tt_, :]), [t_lgs[tt_]], [t_m8s[tt_]])
                yield
                self.ts(msk[:], lgs[:, tt_, :], m8s[:, tt_, 3:4], None, ALU.is_ge, r=[t_lgs[tt_], t_m8s[tt_]], w=[t_msk])
                yield
                self.cp(maskb[:, tt_, :], msk[:], r=[t_msk], w=[t_maskb[tt_]])
                yield
                self.ts(m8[:, 8:9], m8s[:, tt_, 0:1], -1.0, None, ALU.mult, r=[t_m8s[tt_]], w=[t_m8])
                yield
                self.act(lg[:], lgs[:, tt_, :], AF.Exp, r=[t_lgs[tt_], t_m8], w=[t_lg], bias=m8[:, 8:9])
                yield
                self.tt(lg[:], lg[:], msk[:], ALU.mult, r=[t_lg, t_msk], w=[t_lg])
                yield
                self.K.op("dve", lambda e, m8=m8, lg=lg: e.reduce_sum(m8[:, 9:10], lg[:], AX.X), [t_lg], [t_m8])
                yield
                self.recip(m8[:, 10:11], m8[:, 9:10], r=[t_m8], w=[t_m8])
                yield
                self.ts(gates[:, tt_, :], lg[:], m8[:, 10:11], None, ALU.mult, r=[t_lg, t_m8], w=[t_gates[tt_]])
                yield
            def rr(gens):
                gens = list(gens)
                while gens:
                    for g in list(gens):
                        try: next(g)
                        except StopIteration: gens.remove(g)
            for t0 in range(0, NT, 4):
                rr([route_tile(t0 + i) for i in range(min(4, NT - t0))])
            if f"gates{l}" in dbg:
                dbg_out(f"dbg_gates{l}", [128, NT, NE], gates[:], t_gates)
            rt = self.sb("rt", [128, 8, NE]); t_rt = Trk()
            rti = self.sb("rti", [128, NE], I32); t_rti = Trk()
            pb, tpb = bank()
            for tt_ in range(NT):
                self.mm(pb[:, 0:NE], ones_b, maskb[:, tt_, :], tt_ == 0, tt_ == NT - 1, r=[t_cstb, t_maskb[tt_]], w=[tpb])
            self.ts(rt[:, 1, :], pb[:, 0:NE], 127.0, 1.0 / 128, ALU.add, ALU.mult, r=[tpb], w=[t_rt])
            self.ts(rt[:, 1, :], rt[:, 1, :], -0.49609375, None, ALU.add, r=[t_rt], w=[t_rt])
            self.cp(rti[:], rt[:, 1, :], r=[t_rt], w=[t_rti])
            self.cp(rt[:, 2, :], rti[:], r=[t_rti], w=[t_rt])
            self.ts(rt[:, 3, :], rt[:, 2, :], 128.0, None, ALU.mult, r=[t_rt], w=[t_rt])
            self.K.op("dve", lambda e, rt=rt: e.tensor_tensor_scan(rt[:, 4, :], ones_f[:, 0:NE], rt[:, 3, :], 0.0, ALU.mult, ALU.add), [t_rt, t_cst], [t_rt])
            self.tt(rt[:, 5, :], rt[:, 4, :], rt[:, 3, :], ALU.subtract, r=[t_rt], w=[t_rt])
            bx = self.sb("bx", [128, 6, NBLK]); t_bx = Trk()
            pcol = self.sb("pcol", [128, 1]); t_pcol = Trk()
            b128 = cst[:, 1024:1024 + NBLK]
            self.memset(bx[:, 0, :], 0.0, w=[t_bx])
            for e_ in range(NE):
                self.stt(bx[:, 0, :], b128, rt[:, 4, e_:e_ + 1], bx[:, 0, :], ALU.is_ge, ALU.add, r=[t_cst, t_rt, t_bx], w=[t_bx])
            self.ts(bx[:, 1, :], bx[:, 0, :], 31.5, None, ALU.is_lt, r=[t_bx], w=[t_bx])
            self.memset(bx[:, 2, 0:1], 1.0, w=[t_bx])
            self.tt(bx[:, 2, 1:NBLK], bx[:, 0, 1:NBLK], bx[:, 0, 0:NBLK - 1], ALU.not_equal, r=[t_bx], w=[t_bx])
            self.memset(bx[:, 2, NBLK // 2:NBLK // 2 + 1], 1.0, w=[t_bx])
            self.tt(bx[:, 2, :], bx[:, 2, :], bx[:, 1, :], ALU.mult, r=[t_bx], w=[t_bx])
            self.ts(pcol[:], cst[:, 776:777], float(l * NE * 128) - BIG, None, ALU.add, r=[t_cst], w=[t_pcol])
            self.ts(bx[:, 3, :], bx[:, 0, :], 128.0, pcol[:, 0:1], ALU.mult, ALU.add, r=[t_bx, t_pcol], w=[t_bx])
            self.tt(bx[:, 3, :], bx[:, 3, :], bx[:, 2, :], ALU.mult, r=[t_bx], w=[t_bx])
            self.ts(bx[:, 3, :], bx[:, 3, :], BIG, None, ALU.add, r=[t_bx], w=[t_bx])
            self.cp(widx[:], bx[:, 3, :], r=[t_bx], w=[t_widx])
            self.ts(bx[:, 4, :], bx[:, 0, :], float(l * NE) - BIG, None, ALU.add, r=[t_bx], w=[t_bx])
            self.tt(bx[:, 4, :], bx[:, 4, :], bx[:, 2, :], ALU.mult, r=[t_bx], w=[t_bx])
            self.ts(bx[:, 4, :], bx[:, 4, :], BIG, None, ALU.add, r=[t_bx], w=[t_bx])
            self.cp(eidx[:], bx[:, 4, :], r=[t_bx], w=[t_eidx])
            if f"route{l}" in dbg:
                dbg_out(f"dbg_rt{l}", [128, 8, NE], rt[:], [t_rt]); dbg_out(f"dbg_bx{l}", [128, 6, NBLK], bx[:], [t_bx])
                dbg_out(f"dbg_widx{l}", [128, NBLK], widx[:], [t_widx], I32)
            slotf_s = [self.sb(f"slotf{i}", [128, NE]) for i in range(2)]; t_slotf_s = [Trk(), Trk()]
            oh_s = [self.sb(f"oh{i}", [128, NE]) for i in range(2)]; t_oh_s = [Trk(), Trk()]
            tmp32_s = [self.sb(f"tmp32{i}", [128, NE]) for i in range(2)]; t_tmp32_s = [Trk(), Trk()]
            sk_s = [self.sb(f"sk{i}", [128, 4]) for i in range(2)]; t_sk_s = [Trk(), Trk()]
            h2a_s = [self.sb(f"h2a{i}", [128, D]) for i in range(2)]; t_h2a_s = [Trk(), Trk()]
            h2tm = [self.sb(f"h2tm{i}", [128, D], BF16) for i in range(2)]; t_h2tm = [Trk(), Trk()]
            def slot_tile(tt_):
                bi = tt_ % 2
                slotf, t_slotf, oh, t_oh, tmp32, t_tmp32, sk, t_sk, h2a, t_h2a = [x[bi] for x in (slotf_s, t_slotf_s, oh_s, t_oh_s, tmp32_s, t_tmp32_s, sk_s, t_sk_s, h2a_s, t_h2a_s)]
                xq, t_xq = xr_s[bi], t_xr_s[bi]
                pb, tpb = bank()
                for u in range(tt_):
                    self.mm(pb[:, 0:NE], ones_b, maskb[:, u, :], u == 0, False, r=[t_cstb, t_maskb[u]], w=[tpb])
                    yield
                self.mm(pb[:, 0:NE], m_su_b, maskb[:, tt_, :], tt_ == 0, True, r=[t_cstb, t_maskb[tt_]], w=[tpb])
                yield
                self.tt(slotf[:], pb[:, 0:NE], rt[:, 5, :], ALU.add, r=[tpb, t_rt], w=[t_slotf])
                yield
                for k in range(4):
                    self.ts(oh[:], lgs[:, tt_, :], m8s[:, tt_, k:k + 1], None, ALU.is_equal, r=[t_lgs[tt_], t_m8s[tt_]], w=[t_oh])
                    yield
                    self.tt(tmp32[:], oh[:], slotf[:], ALU.mult, r=[t_oh, t_slotf], w=[t_tmp32])
                    yield
                    self.K.op("dve", lambda e, sk=sk, tmp32=tmp32, k=k: e.reduce_sum(sk[:, k:k + 1], tmp32[:], AX.X), [t_tmp32], [t_sk])
                    self.tt(tmp32[:], oh[:], gates[:, tt_, :], ALU.mult, r=[t_oh, t_gates[tt_]], w=[t_tmp32])
                    yield
                    self.K.op("dve", lambda e, gk=gk, tmp32=tmp32, k=k, tt_=tt_: e.reduce_sum(gk[:, tt_, k:k + 1], tmp32[:], AX.X), [t_tmp32], [t_gk[tt_]])
                self.cp(slotidx[:, tt_, :], sk[:], r=[t_sk], w=[t_slotidx[tt_]])
                yield
                self.dma(xq[:], xmid[tt_ * 128:(tt_ + 1) * 128, :], r=[t_xmid[tt_]], w=[t_xq])
                yield
                self.tt(h2a[:], xq[:], scB[:], ALU.mult, r=[t_xq, t_scB], w=[t_h2a])
                yield
                hb = h2tm[bi]; thb = t_h2tm[bi]
                self.tt(hb[:], h2a[:], shB[:], ALU.add, r=[t_h2a, t_shB], w=[thb])
                yield
                for k in range(4):
                    self.K.op("pool", lambda e, hb=hb, slotidx=slotidx, tt_=tt_, k=k: e.indirect_dma_start(
                        out=d_hs[:, :], out_offset=bass.IndirectOffsetOnAxis(ap=slotidx[:, tt_, k:k + 1], axis=0), in_=hb[:, :], in_offset=None,
                        bounds_check=self.breg(e, NSLOT - 1), oob_is_err=False), [thb, t_slotidx[tt_]], t_hs, dma=True)
            for t0 in range(0, NT, 2):
                rr([slot_tile(t0 + i) for i in range(min(2, NT - t0))])
            if f"route{l}" in dbg:
                dbg_out(f"dbg_slotidx{l}", [128, NT, 4], slotidx[:], t_slotidx, I32); dbg_out(f"dbg_gk{l}", [128, NT, 4], gk[:], t_gk)
            self.pop()
            self.push()
            W1s = [self.sb(f"W1_{i}", [128, 8, 2 * D], BF16) for i in range(2)]; t_W1s = [Trk(), Trk()]
            W2s = [self.sb(f"W2_{i}", [128, 8, D], BF16) for i in range(2)]; t_W2s = [Trk(), Trk()]
            b1Bs = [self.sb(f"b1B{i}", [128, 2 * D]) for i in range(2)]; t_b1s = [Trk(), Trk()]
            utm = [self.sb(f"utm{i}", [128, D], BF16) for i in range(2)]; t_utm = [Trk(), Trk()]
            xs = [self.sb(f"xs{i}", [128, D], BF16) for i in range(2)]; t_xs = [Trk(), Trk()]
            xsT = [self.sb(f"xsT{i}", [128, 8, 128], BF16) for i in range(2)]; t_xsT = [Trk(), Trk()]
            uT = [self.sb(f"uT{i}", [128, 8, 128], BF16) for i in range(2)]; t_uT = [Trk(), Trk()]
            g1s = [self.sb(f"g1_{i}", [128, 512]) for i in range(2)]; sgs = [self.sb(f"sg_{i}", [128, 512]) for i in range(2)]
            l1s = [self.sb(f"l1_{i}", [128, 512]) for i in range(2)]
            t_g1s = [Trk(), Trk()]; t_sgs = [Trk(), Trk()]; t_l1s = [Trk(), Trk()]
            yo = [self.sb(f"yo{i}", [128, D]) for i in range(2)]; t_yo = [Trk(), Trk()]
            order = []
            for i in range(NBLK // 2):
                order += [(i, 0), (NBLK // 2 + i, 1)]
            hnc = [0]
            def stG(seq):
                b, sid = order[seq]; bi = seq % 2
                W1, W2, b1B = W1s[sid], W2s[sid], b1Bs[sid]; t_W1, t_W2, t_b1 = t_W1s[sid], t_W2s[sid], t_b1s[sid]
                self.K.op("pool", lambda e, W1=W1, widx=widx, b=b: e.indirect_dma_start(
                    out=W1[:].rearrange("p k n -> p (k n)"), out_offset=None, in_=w1rows[:, :],
                    in_offset=bass.IndirectOffsetOnAxis(ap=widx[:, b:b + 1], axis=0), bounds_check=self.breg(e, L * NE * 128 - 1), oob_is_err=False),
                    [t_widx], [t_W1], dma=True)
                self.K.op("pool", lambda e, W2=W2, widx=widx, b=b: e.indirect_dma_start(
                    out=W2[:].rearrange("p k n -> p (k n)"), out_offset=None, in_=w2rows[:, :],
                    in_offset=bass.IndirectOffsetOnAxis(ap=widx[:, b:b + 1], axis=0), bounds_check=self.breg(e, L * NE * 128 - 1), oob_is_err=False),
                    [t_widx], [t_W2], dma=True)
                self.K.op("pool", lambda e, b1B=b1B, eidx=eidx, b=b: e.indirect_dma_start(
                    out=b1B[:, :], out_offset=None, in_=b1tab_in[:, :],
                    in_offset=bass.IndirectOffsetOnAxis(ap=eidx[:, b:b + 1], axis=0), bounds_check=self.breg(e, L * NE - 1), oob_is_err=False),
                    [t_eidx], [t_b1], dma=True)
            def stT(seq):
                b, sid = order[seq]; bi = seq % 2
                W1, W2, b1B = W1s[sid], W2s[sid], b1Bs[sid]; t_W1, t_W2, t_b1 = t_W1s[sid], t_W2s[sid], t_b1s[sid]
                self.dma(xs[bi][:], d_hs[b * 128:(b + 1) * 128, :], r=[t_hs[b]], w=[t_xs[bi]])
                pb, tpb = bank()
                pv = pb[:].bitcast(BF16)
                for c in range(8):
                    self.tr(pv[:, c * 128:(c + 1) * 128], xs[bi][:, c * 128:(c + 1) * 128], ident_b, r=[t_xs[bi], t_cstb], w=[tpb])
                self.act(xsT[bi][:], pv[:, 0:1024].rearrange("p (c n) -> p c n", c=8), AF.Copy, r=[tpb], w=[t_xsT[bi]])
            def stM1(seq):
                b, sid = order[seq]; bi = seq % 2
                W1, W2, b1B = W1s[sid], W2s[sid], b1Bs[sid]; t_W1, t_W2, t_b1 = t_W1s[sid], t_W2s[sid], t_b1s[sid]
                for half in range(2):
                    gi = hnc[0] % 2; hnc[0] += 1
                    pG, tpG = bank(); pL, tpL = bank()
                    for kc in range(8):
                        self.mm(pG[:], xsT[bi][:, kc, :], W1[:, kc, half * 512:(half + 1) * 512], kc == 0, kc == 7, r=[t_W1, t_xsT[bi]], w=[tpG])
                    for kc in range(8):
                        self.mm(pL[:], xsT[bi][:, kc, :], W1[:, kc, D + half * 512:D + (half + 1) * 512], kc == 0, kc == 7, r=[t_W1, t_xsT[bi]], w=[tpL])
                    g1_, sg_, l1_ = g1s[gi], sgs[gi], l1s[gi]
                    self.tt(g1_[:], pG[:], b1B[:, half * 512:(half + 1) * 512], ALU.add, r=[tpG, t_b1], w=[t_g1s[gi]])
                    self.ts(g1_[:], g1_[:], 7.0, None, ALU.min, r=[t_g1s[gi]], w=[t_g1s[gi]])
                    self.act(sg_[:], g1_[:], AF.Sigmoid, r=[t_g1s[gi]], w=[t_sgs[gi]], scale=1.702)
                    self.tt(l1_[:], pL[:], b1B[:, D + half * 512:D + (half + 1) * 512], ALU.add, r=[tpL, t_b1], w=[t_l1s[gi]])
                    self.ts(l1_[:], l1_[:], 7.0, -7.0, ALU.min, ALU.max, r=[t_l1s[gi]], w=[t_l1s[gi]])
                    self.tt(g1_[:], g1_[:], sg_[:], ALU.mult, r=[t_g1s[gi], t_sgs[gi]], w=[t_g1s[gi]])
                    self.stt(utm[bi][:, half * 512:(half + 1) * 512], l1_[:], 1.0, g1_[:], ALU.add, ALU.mult, r=[t_l1s[gi], t_g1s[gi]], w=[t_utm[bi]])
                pb, tpb = bank()
                pv = pb[:].bitcast(BF16)
                for c in range(8):
                    self.tr(pv[:, c * 128:(c + 1) * 128], utm[bi][:, c * 128:(c + 1) * 128], ident_b, r=[t_utm[bi], t_cstb], w=[tpb])
                self.act(uT[bi][:], pv[:, 0:1024].rearrange("p (c n) -> p c n", c=8), AF.Copy, r=[tpb], w=[t_uT[bi]])
            def stM2(seq):
                b, sid = order[seq]; bi = seq % 2
                W1, W2, b1B = W1s[sid], W2s[sid], b1Bs[sid]; t_W1, t_W2, t_b1 = t_W1s[sid], t_W2s[sid], t_b1s[sid]
                y_ = yo[bi]; ty = t_yo[bi]
                for half in range(2):
                    pb, tpb = bank()
                    for m in range(8):
                        self.mm(pb[:], uT[bi][:, m, :], W2[:, m, half * 512:(half + 1) * 512], m == 0, m == 7, r=[t_uT[bi], t_W2], w=[tpb])
                    self.act(y_[:, half * 512:(half + 1) * 512], pb[:], AF.Copy, r=[tpb], w=[ty])
                self.dma(d_ys[b * 128:(b + 1) * 128, :], y_[:], r=[ty], w=[t_ys[b]])
            nseq = len(order)
            stG(0); stG(1)
            stT(0); stT(1)
            stM1(0)
            for i in range(nseq):
                if i + 2 < nseq: stT(i + 2)
                if i + 1 < nseq: stM1(i + 1)
                stM2(i)
                if i + 2 < nseq: stG(i + 2)
            self.pop()
            gT32 = self.sb("gT32", [NE, 128], BF16); t_gT32 = Trk()
            ya = self.sb("ya", [128, D]); t_ya = Trk()
            yg = [self.sb(f"yg{i}", [128, D]) for i in range(8)]; t_yg = [Trk() for _ in range(8)]
            def gath(tt_):
                for k in range(4):
                    yg_, tyg = yg[(tt_ % 2) * 4 + k], t_yg[(tt_ % 2) * 4 + k]
                    self.K.op("pool", lambda e, yg_=yg_, slotidx=slotidx, tt_=tt_, k=k: e.indirect_dma_start(
                        out=yg_[:, :], out_offset=None, in_=d_ys[:, :], in_offset=bass.IndirectOffsetOnAxis(ap=slotidx[:, tt_, k:k + 1], axis=0),
                        bounds_check=self.breg(e, NSLOT - 1), oob_is_err=False), t_ys + [t_slotidx[tt_]], [tyg], dma=True)
            gath(0)
            for tt_ in range(NT):
                bi = tt_ % 2
                pbg, tpg = bank()
                self.tr(pbg[0:NE, 0:128], gates[:, tt_, :], ident_f, r=[t_gates[tt_], t_cst], w=[tpg])
                self.act(gT32[:], pbg[0:NE, 0:128], AF.Copy, r=[tpg], w=[t_gT32])
                self.dma(xt[bi][:], xmid[tt_ * 128:(tt_ + 1) * 128, :], r=[t_xmid[tt_]], w=[t_xt[bi]])
                for half in range(2):
                    hs = slice(half * 512, (half + 1) * 512)
                    pb, tpb = bank()
                    self.mm(pb[:], gT32[0:NE, :], b2all[0:NE, hs], True, True, r=[t_gT32, t_rw], w=[tpb])
                    self.cp(ya[:, hs], pb[:], r=[tpb], w=[t_ya])
                for k in range(4):
                    yg_, tyg = yg[(tt_ % 2) * 4 + k], t_yg[(tt_ % 2) * 4 + k]
                    self.stt(ya[:], yg_[:], gk[:, tt_, k:k + 1], ya[:], ALU.mult, ALU.add, r=[tyg, t_gk[tt_], t_ya], w=[t_ya])
                if tt_ + 1 < NT: gath(tt_ + 1)
                if f"ffn{l}" in dbg:
                    if tt_ == 0: self.dbg_ffn = self.dout(f"dbg_ffn{l}", [S, D])
                    self.dma(self.dbg_ffn[tt_ * 128:(tt_ + 1) * 128, :], ya[:], r=[t_ya])
                self.tt(ya[:], ya[:], gtB[:], ALU.mult, r=[t_ya, t_gtB], w=[t_ya])
                self.stt(xt[bi][:], xt[bi][:], ALPHA, ya[:], ALU.mult, ALU.add, r=[t_xt[bi], t_ya], w=[t_xt[bi]])
                layernorm(xt[bi][:], t_xt[bi], lnB[0][:], lnB[1][:], t_lnB[0], t_lnB[1])
                if l == L - 1:
                    self.dma(y_out[tt_ * 128:(tt_ + 1) * 128, :], xt[bi][:], r=[t_xt[bi]])
                else:
                    self.dma(xres[tt_ * 128:(tt_ + 1) * 128, :], xt[bi][:], r=[t_xt[bi]], w=[t_xres[tt_]])
            self.pop()
            if "pooltest" in dbg:
                if "pt2" in dbg:
                    self.memset(cst[:, 900:1000], 3.0, w=[t_cst], eng="pool")
                else:
                    self.dma(cst[:], consts_in[:, :], w=[t_cst], q="pool")
                o = self.dout("dbg_pt", [128, 1024]); self.dma(o[:, :], cst[:], r=[t_cst])
            if stop_after == f"3{l}":
                break

        self.scr = dict(xres=xres, xmid=xmid, d_qnT=d_qnT, d_krot=d_krot, d_rw=d_rw, d_gC=d_gC, d_bonus=d_bonus, d_gT=d_gT,
                        d_ro=d_ro, vfirst=vfirst)
        for name in dbg:
            if name.startswith("scr:"):
                k_ = name[4:]; a = self.scr[k_]
                o = self.dout("dbg_" + k_, list(a.shape), a.dtype if hasattr(a, "dtype") else F32)
                self.K.barrier()
                self.K.op("sp", lambda e, o=o, a=a: e.dma_start(out=o, in_=a), (), (), dma=True)
        return self.finish()

    def finish(self):
        self.K.emit(self.nc)
        return self.nc


def _perm_w_in():
    MLA_IN = 416
    cols = -np.ones(NCH * 128, np.int64)
    cols[0:256] = np.arange(0, 256)
    cols[256:384] = np.arange(256, 384)
    r0 = MLA_IN; k0 = r0 + 512; v0 = k0 + 512; wd0 = v0 + 512; ad0 = wd0 + 64; gd0 = ad0 + 64
    kr = np.arange(384, 416)
    krs = kr.reshape(16, 2)[:, ::-1].reshape(32)
    cols[384:448] = np.arange(wd0, wd0 + 64); cols[448:480] = kr
    cols[512:576] = np.arange(ad0, ad0 + 64); cols[576:608] = krs
    cols[640:768] = np.arange(gd0, gd0 + 128)
    cols[768:1280] = np.arange(v0, v0 + 512)
    for fc in range(4):
        cols[(10 + 2 * fc) * 128:(11 + 2 * fc) * 128] = np.arange(r0 + fc * 128, r0 + (fc + 1) * 128)
        cols[(11 + 2 * fc) * 128:(12 + 2 * fc) * 128] = np.arange(k0 + fc * 128, k0 + (fc + 1) * 128)
    return cols

def _take_cols(w, cols):
    out = np.zeros(w.shape[:-1] + (len(cols),), w.dtype)
    m = cols >= 0
    out[..., m] = w[..., cols[m]]
    return out

def _kc(w):
    K, N = w.shape[-2:]
    return np.ascontiguousarray(np.moveaxis(w.reshape(w.shape[:-2] + (K // 128, 128, N)), -3, -2))

def _fm(v, n):
    return np.ascontiguousarray(v.reshape(v.shape[0], n, 128).transpose(0, 2, 1))

def _consts():
    c = np.zeros((128, 1280), np.float32)
    i = np.arange(128)
    c[:, 776] = i
    c[:, 1024:1024 + 256] = 128.0 * np.arange(256)[None, :]
    c[:, 0:128] = np.eye(128)
    c[:, 128:256] = (i[None, :] > i[:, None])
    c[:, 256:384] = (i[None, :] >= i[:, None])
    c[:, 384:512] = (i[None, :] < i[:, None])
    c[:, 512:640] = (i[None, :] // 64 == i[:, None] // 64)
    c[:, 640:768] = 1.0
    fi = np.clip((i - 64) // 2, 0, 15)
    inv_freq = (10000.0 ** (-np.arange(0, 32, 2, dtype=np.float32) / 32)).astype(np.float32)
    c[:, 768] = inv_freq[fi] / np.float32(2 * np.pi)
    c[:, 769] = np.where(i % 2 == 0, -1.0, 1.0)
    c[:, 770] = RMS_EPS; c[:, 771] = 1.0; c[:, 772] = -0.5; c[:, 773] = GN_EPS; c[:, 774] = 0.0; c[:, 775] = LN_EPS
    return c

def prep_shared(inp):
    f = lambda a: np.ascontiguousarray(a, dtype=np.float32)
    cols = _perm_w_in()
    sh = {}
    sh["consts"] = _consts()
    sh["embrow"] = f(np.stack([inp["emb_ln_g"], inp["emb_ln_b"]]))
    sh["ada_w"] = _kc(f(inp["ada_w"]))
    vecs = np.zeros((L, 128, 128), np.float32)
    vecs[:, :, 0:48] = _fm(f(inp["ada_b"]), 48)
    mu_full = np.zeros((L, 2208), np.float32); mu_full[:, 416:] = inp["rwkv_mu"]
    mu_p = _take_cols(mu_full, cols)
    mu_p[:, 0:384] = 0; mu_p[:, 448:512] = 0; mu_p[:, 576:640] = 0
    vecs[:, :, 48:66] = _fm(mu_p, NCH)
    vecs[:, :, 66:68] = _fm(f(inp["q_norm_g"]), 2)
    vecs[:, :, 68:69] = _fm(f(inp["kv_norm_g"]), 1)
    vecs[:, :, 69:73] = _fm(f(inp["mla_out_g"]), 4)
    vecs[:, :, 73:77] = _fm(f(inp["rwkv_w0"]), 4)
    vecs[:, :, 77:81] = _fm(f(inp["rwkv_a0"]), 4)
    vecs[:, :, 81:85] = _fm(f(inp["rwkv_k_k"]), 4)
    vecs[:, :, 85:89] = _fm(f(inp["rwkv_k_a"]), 4)
    vecs[:, :, 89:93] = _fm(f(inp["rwkv_r_k"]).reshape(L, 512), 4)
    vecs[:, :, 93:97] = _fm(f(inp["rwkv_gn_g"]), 4)
    vecs[:, :, 97:101] = _fm(f(inp["rwkv_gn_b"]), 4)
    vecs[1:, :, 101:105] = _fm(f(inp["vres_v0"]), 4)
    sh["vecs"] = vecs
    sh["w_in"] = _kc(_take_cols(f(inp["w_in"]), cols))
    wuq = f(inp["w_uq"])
    sw = np.arange(768).reshape(8, 96).copy()
    sw[:, 64:96] = sw[:, 64:96].reshape(8, 16, 2)[:, :, ::-1].reshape(8, 32)
    sh["w_uq"] = _kc(np.concatenate([wuq, wuq[:, :, sw.reshape(-1)]], axis=-1))
    sh["w_ukv"] = f(np.concatenate([inp["w_uk"], inp["w_uv"]], axis=-1))
    wl = np.zeros((L, 128, 1536), np.float32)
    wl[:, 0:64, 0:512] = inp["rwkv_w2"]; wl[:, 0:64, 512:1024] = inp["rwkv_a2"]; wl[:, :, 1024:1536] = inp["rwkv_g2"]
    sh["w_lora"] = wl
    sh["vres1"] = _kc(f(inp["vres_v1"][0])); sh["vres2"] = f(inp["vres_v2"][0])
    sh["w_o"] = _kc(f(inp["w_o"]))
    sh["rows"] = f(np.concatenate([inp["ln1_g"], inp["ln1_b"], inp["ln2_g"], inp["ln2_b"], inp["router_b"]], axis=-1))
    sh["router_w"] = _kc(f(inp["router_w"]))
    gl = np.concatenate([np.arange(0, 2 * D, 2), np.arange(1, 2 * D, 2)])
    sh["_gl"] = gl
    sh["b1v"] = np.ascontiguousarray(f(inp["exp_b1"])[:, :, gl].reshape(L, NE, 16, 128).transpose(0, 1, 3, 2)).reshape(L * NE * 128, 16)
    sh["exp_b2"] = f(inp["exp_b2"])
    sh["b1tab"] = np.ascontiguousarray(f(inp["exp_b1"])[:, :, gl].reshape(L * NE, 2 * D))
    return sh

def prep_big(inp, sh):
    gl = sh["_gl"]
    w1 = np.asarray(inp["exp_w1"], dtype=np.float32)
    sh["exp_w1"] = np.ascontiguousarray(np.moveaxis(w1[..., gl].reshape(L, NE, 8, 128, 2 * D), 2, 3)).reshape(L * NE * 128, 8 * 2 * D)
    w2 = np.asarray(inp["exp_w2"], dtype=np.float32)
    sh["exp_w2"] = np.ascontiguousarray(np.moveaxis(w2.reshape(L, NE, 8, 128, D), 2, 3)).reshape(L * NE * 128, 8 * D)

def prep_core(inp, sh, b, S, names):
    f = lambda a: np.ascontiguousarray(a, dtype=np.float32)
    d = {k: v for k, v in sh.items() if k in names}
    d["x"] = f(inp["x"][b, :S]); d["cT"] = f(inp["c"][b].reshape(8, 128).T)
    d["pos"] = np.ascontiguousarray(inp["positions"][b, :S].reshape(1, S).astype(np.int32))
    return d


def kernel(**inputs):
    S = 4096
    bld = B(S)
    nc = bld.build()
    names = set(bld.ins.keys())
    sh = prep_shared(inputs)
    if "exp_w1" in names: prep_big(inputs, sh)
    per = [prep_core(inputs, sh, b, S, names) for b in range(8)]
    res = run_bass_kernel_spmd(nc, per, core_ids=list(range(8)))
    return np.stack([r["y"] for r in res.results], axis=0)
```

```python
import numpy as np
from contextlib import ExitStack
import concourse.bass as bass
import concourse.mybir as mybir
from concourse.bass_utils import run_bass_kernel_spmd

F32 = mybir.dt.float32; BF16 = mybir.dt.bfloat16; I32 = mybir.dt.int32; U32 = mybir.dt.uint32
AF = mybir.ActivationFunctionType; ALU = mybir.AluOpType; AX = mybir.AxisListType

D = 1024; L = 2; NE = 32; TOPK = 4
ALPHA = (2 * L) ** 0.25
LN_EPS = 1e-5; RMS_EPS = 1e-6; GN_EPS = 64e-5
CAP = 1024
NCH = 18

class Trk:
    __slots__ = ("w", "r", "excl")
    def __init__(self, excl=False): self.w = None; self.r = {}; self.excl = excl

ENGS = ("pe", "act", "dve", "pool", "sp")
NDS = 8
class Sched:
    def __init__(self):
        self.ops = {e: [] for e in ENGS}
        self.cnt = {e: 0 for e in ENGS}
        self.dcnt = {e: 0 for e in ENGS}
        self.waited = {e: {} for e in ENGS}
        self.lastdma = {}
    def op(self, eng, fn, r=(), w=(), dma=False, serial=False):
        deps = []
        if serial and self.lastdma.get(eng) is not None: deps.append((self.lastdma[eng], True))
        for t in r:
            if t.w is not None: deps.append((t.w, True))
            if t.excl:
                for ev in t.r.values(): deps.append((ev, False))
        for t in w:
            if t.w is not None: deps.append((t.w, False))
            for ev in t.r.values(): deps.append((ev, False))
        waits = {}
        for (semkey, val, src, isdma), raw in deps:
            if src == eng and not isdma:
                if eng == "pe": continue
                if not raw: continue
            if self.waited[eng].get(semkey, 0) >= val: continue
            if waits.get(semkey, 0) < val: waits[semkey] = val
        for k, v in waits.items(): self.waited[eng][k] = v
        if dma:
            i = self.dcnt[eng]; self.dcnt[eng] += 1
            semkey = ("d", eng, i % NDS); val = 16 * (i // NDS + 1); inc = 16
        else:
            self.cnt[eng] += 1; semkey = ("e", eng); val = self.cnt[eng]; inc = 1
        ev = (semkey, val, eng, dma)
        if dma: self.lastdma[eng] = ev
        self.ops[eng].append((list(waits.items()), fn, semkey, inc))
        for t in r:
            old = t.r.get(semkey)
            if old is None or old[1] < val: t.r[semkey] = ev
        for t in w:
            t.w = ev; t.r = {}
    def barrier(self):
        snap = []
        for en in ENGS:
            if self.cnt[en] > 0: snap.append((("e", en), self.cnt[en]))
            n = self.dcnt[en]
            for k in range(NDS):
                c = (n - k + NDS - 1) // NDS if n > k else 0
                if c > 0: snap.append((("d", en, k), 16 * c))
        for eng in ENGS:
            waits = [(k, v) for k, v in snap if self.waited[eng].get(k, 0) < v and k != ("e", eng)]
            for k, v in waits: self.waited[eng][k] = v
            self.ops[eng].append((waits, None, None, 0))
    def emit(self, nc):
        with ExitStack() as st:
            sems = {}
            for e in ENGS:
                sems[("e", e)] = st.enter_context(nc.semaphore("se_" + e))
                for k in range(NDS):
                    sems[("d", e, k)] = st.enter_context(nc.semaphore(f"sd_{e}_{k}"))
            block = st.enter_context(nc.Block())
            def mk(ename):
                ops = self.ops[ename]
                def body(e):
                    for waits, fn, semkey, inc in ops:
                        for k, v in waits: e.wait_ge(sems[k], v)
                        if fn is not None: fn(e).then_inc(sems[semkey], inc)
                    if ename == "sp":
                        for en in ENGS:
                            n = self.dcnt[en]
                            for k in range(NDS):
                                c = (n - k + NDS - 1) // NDS if n > k else 0
                                if c > 0: e.wait_ge(sems[("d", en, k)], 16 * c)
                            if self.cnt[en] > 0 and en != ename: e.wait_ge(sems[("e", en)], self.cnt[en])
                return body
            block.tensor(mk("pe")); block.scalar(mk("act")); block.vector(mk("dve"))
            block.gpsimd(mk("pool")); block.sync(mk("sp"))

class B:
    def __init__(self, S, dbg=()):
        self.S = S; self.NT = S // 128; self.NB = S // 512
        self.nc = bass.Bass("TRN2", target_bir_lowering=False)
        self.K = Sched()
        self.stacks = [ExitStack()]
        self.dbg = dbg
        self.ins = {}; self.outs = {}
        self.rr = 0
    def din(self, name, shape, dt=F32):
        a = self.nc.dram_tensor(name, list(shape), dt, kind="ExternalInput").ap(); self.ins[name] = a; return a
    def dout(self, name, shape, dt=F32):
        a = self.nc.dram_tensor(name, list(shape), dt, kind="ExternalOutput").ap(); self.outs[name] = a; return a
    def dscr(self, name, shape, dt=F32):
        return self.nc.dram_tensor("i_" + name, list(shape), dt, kind="Internal").ap()
    def sb(self, name, shape, dt=F32):
        self.uid = getattr(self, "uid", 0) + 1
        return self.stacks[-1].enter_context(self.nc.sbuf_tensor(f"s{self.uid}_{name}", list(shape), dt))
    def ps(self, name, shape, dt=F32):
        return self.stacks[0].enter_context(self.nc.psum_tensor("p_" + name, list(shape), dt))
    def dma(self, out, in_, r=(), w=(), q="sp"):
        self.K.op(q, lambda e: e.dma_start(out=out, in_=in_), r, w, dma=True)
    def mm(self, out, lhsT, rhs, start, stop, r=(), w=()):
        self.K.op("pe", lambda e: e.matmul(out, lhsT, rhs, start=start, stop=stop), r, w)
    def tr(self, out, in_, ident, r=(), w=()):
        self.K.op("pe", lambda e: e.transpose(out, in_, ident), r, w)
    def act(self, out, in_, func, r=(), w=(), bias=None, scale=None, accum=None):
        kw = {}
        if bias is not None: kw["bias"] = bias
        if scale is not None: kw["scale"] = scale
        if accum is not None: kw["accum_out"] = accum
        self.K.op("act", lambda e: e.activation(out=out, in_=in_, func=func, **kw), r, w)
    def tt(self, out, in0, in1, op, r=(), w=(), eng="dve"):
        self.K.op(eng, lambda e: e.tensor_tensor(out, in0, in1, op), r, w)
    def ts(self, out, in0, s1, s2, op0, op1=None, r=(), w=(), eng="dve"):
        if op1 is None:
            self.K.op(eng, lambda e: e.tensor_scalar(out, in0, s1, None, op0), r, w)
        else:
            self.K.op(eng, lambda e: e.tensor_scalar(out, in0, s1, s2, op0, op1), r, w)
    def stt(self, out, in0, scalar, in1, op0, op1, r=(), w=()):
        self.K.op("dve", lambda e: e.scalar_tensor_tensor(out, in0, scalar, in1, op0, op1), r, w)
    def cp(self, out, in_, r=(), w=(), eng="dve"):
        self.K.op(eng, lambda e: e.tensor_copy(out, in_), r, w)
    def recip(self, out, in_, r=(), w=()):
        self.K.op("dve", lambda e: e.reciprocal(out, in_), r, w)
    def breg(self, e, v):
        if not hasattr(self, "_bregs"): self._bregs = {}
        if v not in self._bregs: self._bregs[v] = e.to_reg(v)
        return self._bregs[v]
    def memset(self, ap, v, w=(), eng="dve"):
        self.K.op(eng, lambda e: e.memset(ap, v), (), w)

    def push(self):
        self.stacks.append(ExitStack())
    def pop(self):
        self.K.barrier()
        self.stacks.pop().close()

    def build(self, stop_after=None, layers=L, l0=0):
        S, NT, NB = self.S, self.NT, self.NB
        nc = self.nc
        dbg = self.dbg
        x_in = self.din("x", [S, D])
        cT_in = self.din("cT", [128, 8])
        pos_in = self.din("pos", [1, S], I32)
        consts_in = self.din("consts", [128, 1280])
        embrow_in = self.din("embrow", [2, D])
        ada_w_in = self.din("ada_w", [L, 128, 8, 6 * D])
        vecs_in = self.din("vecs", [L, 128, 128])
        w_in_in = self.din("w_in", [L, 128, 8, NCH * 128])
        w_uq_in = self.din("w_uq", [L, 128, 2, 1536])
        w_ukv_in = self.din("w_ukv", [L, 128, 1024])
        w_lora_in = self.din("w_lora", [L, 128, 1536])
        vres1_in = self.din("vres1", [128, 4, 32])
        vres2_in = self.din("vres2", [32, 512])
        w_o_in = self.din("w_o", [L, 128, 8, D])
        rows_in = self.din("rows", [L, 4 * D + NE])
        router_w_in = self.din("router_w", [L, 128, 8, NE])
        b1v_in = self.din("b1v", [L * NE * 128, 16])
        exp_b2_in = self.din("exp_b2", [L, NE, D])
        y_out = self.dout("y", [S, D])
        xres = self.dscr("xres", [S, D]); t_xres = [Trk() for _ in range(NT)]
        xmid = self.dscr("xmid", [S, D]); t_xmid = [Trk() for _ in range(NT)]
        vfirst = self.dscr("vfirst", [4, 128, S]); t_vfirst = [Trk() for _ in range(NB)]
        d_qnT = self.dscr("qnT", [3, 128, S], BF16); t_dqn = [Trk() for _ in range(NB)]
        d_krot = self.dscr("krot", [128, S], BF16); t_dkrot = [Trk() for _ in range(NB)]
        d_rw = self.dscr("rw", [5, 4, 128, S], BF16); t_drw = [Trk() for _ in range(NB)]
        d_gC = self.dscr("gC", [4, 128, NT]); t_dgC = [Trk() for _ in range(NB)]
        d_bonus = self.dscr("bonus", [4, 128, S]); t_dbonus = [Trk() for _ in range(NB)]
        d_gT = self.dscr("gT", [4, 128, S], BF16); t_dgT = [Trk() for _ in range(NB)]
        d_ro = self.dscr("ro", [4, 128, S], BF16); t_dro = [Trk() for _ in range(NT)]
        self.scr_hs = self.dscr("hslots", [(4 * NT + NE) * 128, D], BF16); self.scr_ys = self.dscr("yslots", [(4 * NT + NE) * 128, D])
        d_rope = self.dscr("ropetab", [2, 32, S]); t_drope = Trk()
        cst = self.sb("cst", [128, 1280]); t_cst = Trk()
        cstb = self.sb("cstb", [128, 1024], BF16); t_cstb = Trk()
        self.dma(cst[:], consts_in[:, :], w=[t_cst])
        self.dma(cstb[:], consts_in[:, 0:1024], w=[t_cstb], q="pool")
        ident_f = cst[:, 0:128]; ident_b = cstb[:, 0:128]; ones_f = cst[:, 640:768]
        m_su_b = cstb[:, 128:256]; m_iu_b = cstb[:, 256:384]; m_sl_b = cstb[:, 384:512]
        bones_b = cstb[:, 512:640]; ones_b = cstb[:, 640:768]
        c_rmseps = cst[:, 770:771]; c_one = cst[:, 771:772]; c_mhalf = cst[:, 772:773]; c_gneps = cst[:, 773:774]
        c_zero = cst[:, 774:775]
        PS = [self.ps(f"ps{i}", [128, 512]) for i in range(8)]
        t_PS = [Trk(excl=True) for _ in range(8)]
        def bank():
            i = self.rr % 7; self.rr += 1; return PS[i], t_PS[i]
        vecs = self.sb("vecs", [128, L, 128]); t_vecs = Trk()
        for l in range(L):
            self.dma(vecs[:, l, :], vecs_in[l], w=[t_vecs])
        modT = self.sb("modT", [128, L, 48]); t_mod = Trk()
        dvec = self.sb("dvec", [128, L, 32]); t_dvec = Trk()
        stat = self.sb("stat", [128, 16]); t_stat = Trk()
        xt = [self.sb(f"xt{i}", [128, D]) for i in range(2)]; t_xt = [Trk(), Trk()]

        def layernorm(xa, t_x, gB, bB, t_g, t_b):
            self.K.op("dve", lambda e: e.bn_stats(stat[:, 0:6], xa[:, 0:512]), [t_x], [t_stat])
            self.K.op("dve", lambda e: e.bn_stats(stat[:, 6:12], xa[:, 512:1024]), [t_x], [t_stat])
            self.K.op("dve", lambda e: e.bn_aggr(stat[:, 12:14], stat[:, 0:12]), [t_stat], [t_stat])
            self.ts(stat[:, 14:15], stat[:, 13:14], LN_EPS, None, ALU.add, r=[t_stat], w=[t_stat])
            self.act(stat[:, 14:15], stat[:, 14:15], AF.Sqrt, r=[t_stat], w=[t_stat])
            self.recip(stat[:, 15:16], stat[:, 14:15], r=[t_stat], w=[t_stat])
            self.ts(xa, xa, stat[:, 12:13], stat[:, 15:16], ALU.subtract, ALU.mult, r=[t_x, t_stat], w=[t_x])
            self.tt(xa, xa, gB, ALU.mult, r=[t_x, t_g], w=[t_x])
            self.tt(xa, xa, bB, ALU.add, r=[t_x, t_b], w=[t_x])

        self.push()
        cT = self.sb("cTf", [128, 8]); t_cT = Trk()
        self.dma(cT[:], cT_in[:, :], w=[t_cT])
        cTb = self.sb("cTb", [128, 8], BF16); t_cTb = Trk()
        self.act(cTb[:], cT[:], AF.Silu, r=[t_cT], w=[t_cTb])
        adaw = [self.sb(f"adaw{i}", [128, 8, 384], BF16) for i in range(2)]; t_adaw = [Trk(), Trk()]
        for l in range(L):
            pb, tpb = bank()
            for cg in range(16):
                bi = cg % 2
                self.dma(adaw[bi][:], ada_w_in[l, :, :, cg * 384:(cg + 1) * 384], w=[t_adaw[bi]], q="pool")
                for cc in range(3):
                    col = cg * 3 + cc
                    for kc in range(8):
                        self.mm(pb[:, col:col + 1], adaw[bi][:, kc, cc * 128:(cc + 1) * 128], cTb[:, kc:kc + 1],
                                kc == 0, kc == 7, r=[t_adaw[bi], t_cTb], w=[tpb])
            self.tt(modT[:, l, :], pb[:, 0:48], vecs[:, l, 0:48], ALU.add, r=[tpb, t_vecs], w=[t_mod])
            for v in (1, 2, 4, 5):
                self.ts(modT[:, l, v * 8:(v + 1) * 8], modT[:, l, v * 8:(v + 1) * 8], 1.0, None, ALU.add, r=[t_mod], w=[t_mod])
            self.ts(dvec[:, l, 0:18], vecs[:, l, 48:66], -1.0, 1.0, ALU.mult, ALU.add, r=[t_vecs], w=[t_dvec])
            self.ts(dvec[:, l, 18:22], vecs[:, l, 73:77], -1.0, None, ALU.mult, r=[t_vecs], w=[t_dvec])
        if "mod" in dbg:
            o = self.dout("dbg_mod", [128, L, 48]); self.dma(o[:, :, :], modT[:], r=[t_mod])
        cosF = self.sb("cosF", [128, S]); sinF = self.sb("sinF", [128, S]); t_rope = Trk()
        posi = self.sb("posi", [128, 512], I32); t_posi = Trk()
        rt1 = self.sb("rt1", [128, 512]); t_rt1 = Trk()
        rt2 = self.sb("rt2", [128, 512]); t_rt2 = Trk()
        for blk in range(NB):
            cs = slice(blk * 512, (blk + 1) * 512)
            self.dma(posi[:], pos_in[:, cs].broadcast_to([128, 512]), w=[t_posi])
            self.cp(rt1[:], posi[:], r=[t_posi], w=[t_rt1])
            self.ts(rt1[:], rt1[:], cst[:, 768:769], None, ALU.mult, r=[t_rt1, t_cst], w=[t_rt1])
            self.cp(posi[:], rt1[:], r=[t_rt1], w=[t_posi])
            self.cp(rt2[:], posi[:], r=[t_posi], w=[t_rt2])
            self.tt(rt2[:], rt1[:], rt2[:], ALU.subtract, r=[t_rt1, t_rt2], w=[t_rt2])
            self.act(sinF[:, cs], rt2[:], AF.Sin, r=[t_rt2], w=[t_rope], scale=float(2 * np.pi))
            self.ts(sinF[:, cs], sinF[:, cs], cst[:, 769:770], None, ALU.mult, r=[t_rope, t_cst], w=[t_rope])
            self.ts(rt1[:], rt1[:], 0.25, None, ALU.add, r=[t_rt1], w=[t_rt1])
            self.cp(posi[:], rt1[:], r=[t_rt1], w=[t_posi])
            self.cp(rt2[:], posi[:], r=[t_posi], w=[t_rt2])
            self.tt(rt2[:], rt1[:], rt2[:], ALU.subtract, r=[t_rt1, t_rt2], w=[t_rt2])
            self.act(cosF[:, cs], rt2[:], AF.Sin, r=[t_rt2], w=[t_rope], scale=float(2 * np.pi))
        self.dma(d_rope[0], cosF[64:96, :], r=[t_rope], w=[t_drope])
        self.dma(d_rope[1], sinF[64:96, :], r=[t_rope], w=[t_drope])
        rowB = [self.sb(f"rowB{i}", [128, D]) for i in range(2)]; t_rowB = [Trk() for _ in range(2)]
        self.dma(rowB[0][:], embrow_in[0:1, :].broadcast_to([128, D]), w=[t_rowB[0]])
        self.dma(rowB[1][:], embrow_in[1:2, :].broadcast_to([128, D]), w=[t_rowB[1]])
        for tt_ in range(NT):
            bi = tt_ % 2
            self.dma(xt[bi][:], x_in[tt_ * 128:(tt_ + 1) * 128, :], w=[t_xt[bi]])
            layernorm(xt[bi][:], t_xt[bi], rowB[0][:], rowB[1][:], t_rowB[0], t_rowB[1])
            self.dma(xres[tt_ * 128:(tt_ + 1) * 128, :], xt[bi][:], r=[t_xt[bi]], w=[t_xres[tt_]])
        self.pop()

        def dbg_out(name, shape, src_ap, r, dt=F32):
            o = self.dout(name, shape, dt)
            self.dma(o, src_ap, r=r)

        for l in range(l0, layers):
            V = lambda a, b: vecs[:, l, a:b]
            self.push()
            cosB = [self.sb(f"cosB{i}", [128, 512]) for i in range(2)]; sinB = [self.sb(f"sinB{i}", [128, 512]) for i in range(2)]
            t_ropeB = [Trk(), Trk()]
            hT = self.sb("hT", [128, 8, 512], BF16); t_hT = Trk()
            xa = [xt[0], xt[1]] + [self.sb(f"xa{i}", [128, D]) for i in range(2)]; t_xa = [t_xt[0], t_xt[1], Trk(), Trk()]
            winb = self.sb("winb", [128, 8, NCH * 128], BF16); t_win = Trk()
            self.dma(winb[:], w_in_in[l], w=[t_win], q="pool")
            wlora = self.sb("wlora", [128, 1536], BF16); t_wl = Trk()
            self.dma(wlora[:], w_lora_in[l], w=[t_wl], q="pool")
            vr1 = self.sb("vr1", [128, 4, 32], BF16); vr2 = self.sb("vr2", [32, 512], BF16); t_vr = Trk()
            self.dma(vr1[:], vres1_in[:, :, :], w=[t_vr], q="pool")
            self.dma(vr2[:], vres2_in[:, :], w=[t_vr], q="pool")
            praw = self.sb("praw", [128, 15, 513]); t_praw = [Trk() for _ in range(15)]
            pq = self.sb("pq", [128, 3, 512]); t_pq = Trk()
            sqb = self.sb("sqb", [128, 3, 512], BF16); t_sqb = Trk()
            rs = self.sb("rs", [128, 2, 512]); t_rs = Trk()
            qst = self.sb("qst", [128, 3, 512], BF16); t_qst = Trk()
            krst = self.sb("krst", [128, 512], BF16); t_krst = Trk()
            E3 = self.sb("E3", [128, 512]); t_E3 = Trk()
            E4 = self.sb("E4", [128, 512]); t_E4 = Trk()
            wdT = self.sb("wdT", [128, 512], BF16); adT = self.sb("adT", [128, 512], BF16); sgT = self.sb("sgT", [128, 512], BF16)
            t_wdT = Trk(); t_adT = Trk(); t_sgT = Trk()
            Ev = self.sb("Ev", [128, 4, 512]); t_Ev = [Trk() for _ in range(4)]
            Evb = self.sb("Evb", [128, 4, 512], BF16); t_Evb = Trk()
            vv1 = self.sb("vv1", [32, 512], BF16); t_vv1 = Trk()
            vf = self.sb("vf", [128, 512]); t_vf = Trk()
            names = ["Er", "Ek", "Ea", "Eld", "Ekap", "Ekm", "Eb", "EL", "Ex1", "Ex2"]
            Ets = [{n: self.sb(f"{n}_{i}", [128, 512]) for n in names} for i in range(2)]; tEs = [{n: Trk() for n in names} for i in range(2)]
            Et, tE = Ets[0], tEs[0]
            stg = self.sb("stg", [128, 2, 6, 512], BF16); t_stg = [Trk(), Trk()]
            gcs = self.sb("gcs", [128, 4, 4]); t_gcs = [Trk() for _ in range(4)]
            for blk in range(NB):
                cs = slice(blk * 512, (blk + 1) * 512)
                cosF_, sinF_, t_rope = cosB[blk % 2], sinB[blk % 2], t_ropeB[blk % 2]
                self.dma(cosF_[64:96, :], d_rope[0][:, cs], r=[t_drope], w=[t_rope])
                self.dma(sinF_[64:96, :], d_rope[1][:, cs], r=[t_drope], w=[t_rope])
                if blk == 0:
                    for j in range(4):
                        self.dma(xa[j][:], xres[j * 128:(j + 1) * 128, :], r=[t_xres[j]], w=[t_xa[j]])
                for j in range(4):
                    tt_ = blk * 4 + j; bi = j
                    for half in range(2):
                        pb, tpb = bank()
                        for q in range(4):
                            c = half * 4 + q
                            self.tr(pb[:, q * 128:(q + 1) * 128], xa[bi][:, c * 128:(c + 1) * 128], ident_f,
                                    r=[t_xa[bi], t_cst], w=[tpb])
                        for q in range(4):
                            c = half * 4 + q
                            self.act(hT[:, c, j * 128:(j + 1) * 128], pb[:, q * 128:(q + 1) * 128], AF.Identity,
                                     r=[tpb, t_mod], w=[t_hT], scale=modT[:, l, 8 + c:9 + c], bias=modT[:, l, c:c + 1])
                if blk + 1 < NB:
                    for j in range(4):
                        tn = (blk + 1) * 4 + j
                        self.dma(xa[j][:], xres[tn * 128:(tn + 1) * 128, :], r=[t_xres[tn]], w=[t_xa[j]])
                def pchunk(c):
                    pb, tpb = bank()
                    for kc in range(8):
                        self.mm(pb[:], winb[:, kc, c * 128:(c + 1) * 128], hT[:, kc, :], kc == 0, kc == 7,
                                r=[t_win, t_hT], w=[tpb])
                    return pb, tpb
                def shift(c, out_ap, t_out):
                    pb, tpb = pchunk(c)
                    ci = c - 3
                    if blk == 0:
                        self.memset(praw[:, ci, 0:1], 0.0, w=[t_praw[ci]])
                    else:
                        self.cp(praw[:, ci, 0:1], praw[:, ci, 512:513], r=[t_praw[ci]], w=[t_praw[ci]])
                    self.act(praw[:, ci, 1:513], pb[:], AF.Copy, r=[tpb], w=[t_praw[ci]])
                    self.ts(out_ap, praw[:, ci, 1:513], dvec[:, l, c:c + 1], None, ALU.mult, r=[t_praw[ci], t_dvec], w=[t_out])
                    self.stt(out_ap, praw[:, ci, 0:512], vecs[:, l, 48 + c:49 + c], out_ap, ALU.mult, ALU.add,
                             r=[t_praw[ci], t_vecs, t_out], w=[t_out])
                for c in range(3):
                    pb, tpb = pchunk(c)
                    self.act(pq[:, c, :], pb[:], AF.Copy, r=[tpb], w=[t_pq])
                    self.act(sqb[:, c, :], pb[:], AF.Square, r=[tpb], w=[t_sqb])
                pb, tpb = bank()
                self.mm(pb[:], ones_b, sqb[:, 0, :], True, False, r=[t_cstb, t_sqb], w=[tpb])
                self.mm(pb[:], ones_b, sqb[:, 1, :], False, True, r=[t_cstb, t_sqb], w=[tpb])
                self.act(rs[:, 0, :], pb[:], AF.Sqrt, r=[tpb, t_cst], w=[t_rs], scale=1.0 / 256, bias=c_rmseps)
                pb, tpb = bank()
                self.mm(pb[:], ones_b, sqb[:, 2, :], True, True, r=[t_cstb, t_sqb], w=[tpb])
                self.act(rs[:, 1, :], pb[:], AF.Sqrt, r=[tpb, t_cst], w=[t_rs], scale=1.0 / 128, bias=c_rmseps)
                self.recip(rs[:], rs[:], r=[t_rs], w=[t_rs])
                for c in range(3):
                    self.stt(qst[:, c, :], pq[:, c, :], V(66 + c, 67 + c), rs[:, 0 if c < 2 else 1, :], ALU.mult, ALU.mult,
                             r=[t_pq, t_vecs, t_rs], w=[t_qst])
                    if c == 2: self.dma(d_qnT[:, :, cs].rearrange("c p s -> p c s"), qst[:], r=[t_qst], w=[t_dqn[blk]])
                shift(3, E3[:], t_E3)
                self.act(wdT[0:64, :], E3[0:64, :], AF.Tanh, r=[t_E3], w=[t_wdT])
                shift(4, E4[:], t_E4)
                self.act(adT[0:64, :], E4[0:64, :], AF.Copy, r=[t_E4], w=[t_adT])
                self.tt(E3[64:96, :], E3[64:96, :], cosF_[64:96, :], ALU.mult, r=[t_E3, t_rope], w=[t_E3])
                self.tt(E4[64:96, :], E4[64:96, :], sinF_[64:96, :], ALU.mult, r=[t_E4, t_rope], w=[t_E4])
                self.tt(krst[64:96, :], E3[64:96, :], E4[64:96, :], ALU.add, r=[t_E3, t_E4], w=[t_krst])
                self.dma(d_krot[64:96, cs], krst[64:96, :], r=[t_krst], w=[t_dkrot[blk]])
                shift(5, E3[:], t_E3)
                self.act(sgT[:], E3[:], AF.Sigmoid, r=[t_E3], w=[t_sgT])
                for fc in range(4):
                    shift(6 + fc, Ev[:, fc, :], t_Ev[fc])
                if l == 0 or "novres" in dbg:
                    self.dma(vfirst[:, :, cs].rearrange("f p s -> p f s"), Ev[:], r=t_Ev, w=[t_vfirst[blk]])
                else:
                    for fc in range(4):
                        self.act(Evb[:, fc, :], Ev[:, fc, :], AF.Copy, r=[t_Ev[fc]], w=[t_Evb])
                    pb, tpb = bank()
                    for fc in range(4):
                        self.mm(pb[0:32, :], vr1[:, fc, :], Evb[:, fc, :], fc == 0, fc == 3, r=[t_vr, t_Evb], w=[tpb])
                    self.act(vv1[:], pb[0:32, :], AF.Copy, r=[tpb], w=[t_vv1])
                    for fc in range(4):
                        pb, tpb = bank()
                        self.mm(pb[:], vr2[0:32, fc * 128:(fc + 1) * 128], vv1[0:32, :], True, True, r=[t_vr, t_vv1], w=[tpb])
                        x1 = Et["Ex1"]; self.act(x1[:], pb[:], AF.Sigmoid, r=[tpb, t_vecs], w=[tE["Ex1"]], bias=V(101 + fc, 102 + fc))
                        self.dma(vf[:], vfirst[fc, :, cs], r=[t_vfirst[blk]], w=[t_vf])
                        self.tt(vf[:], vf[:], Ev[:, fc, :], ALU.subtract, r=[t_vf, t_Ev[fc]], w=[t_vf])
                        self.tt(vf[:], vf[:], x1[:], ALU.mult, r=[t_vf, tE["Ex1"]], w=[t_vf])
                        self.tt(Ev[:, fc, :], Ev[:, fc, :], vf[:], ALU.add, r=[t_Ev[fc], t_vf], w=[t_Ev[fc]])
                def fcchain(fc, sb_):
                    Er, Ek, Ea, Eld, Ekap, Ekm, Eb, EL, Ex1, Ex2 = [Ets[sb_][n] for n in names]
                    tr_, tk_, ta_, tld, tkap, tkm, tb_, tL, tx1, tx2 = [tEs[sb_][n] for n in names]
                    fs = slice(fc * 128, (fc + 1) * 128)
                    sg_ = stg[:, sb_]; tsg = t_stg[sb_]
                    shift(10 + 2 * fc, Er[:], tr_)
                    shift(11 + 2 * fc, Ek[:], tk_)
                    pb, tpb = bank()
                    self.mm(pb[:], wlora[0:64, fs], wdT[0:64, :], True, True, r=[t_wl, t_wdT], w=[tpb])
                    yield
                    self.act(Ex1[:], pb[:], AF.Exp, r=[tpb, t_dvec], w=[tx1], scale=-1.0, bias=dvec[:, l, 18 + fc:19 + fc])
                    yield
                    self.act(Ex1[:], Ex1[:], AF.Ln, r=[tx1, t_cst], w=[tx1], bias=c_one)
                    yield
                    self.act(Ex1[:], Ex1[:], AF.Exp, r=[tx1, t_cst], w=[tx1], scale=-1.0, bias=c_mhalf)
                    yield
                    self.ts(Eld[:], Ex1[:], -1.0, None, ALU.mult, r=[tx1], w=[tld])
                    yield
                    pb, tpb = bank()
                    self.mm(pb[:], wlora[0:64, 512 + fc * 128:512 + (fc + 1) * 128], adT[0:64, :], True, True, r=[t_wl, t_adT], w=[tpb])
                    yield
                    self.act(Ea[:], pb[:], AF.Sigmoid, r=[tpb, t_vecs], w=[ta_], bias=V(77 + fc, 78 + fc))
                    yield
                    self.ts(Ekap[:], Ek[:], V(81 + fc, 82 + fc), None, ALU.mult, r=[tk_, t_vecs], w=[tkap])
                    yield
                    sq1 = stg[:, sb_, 5, :]
                    self.act(sq1, Ekap[:], AF.Square, r=[tkap], w=[tsg])
                    yield
                    pb, tpb = bank()
                    self.mm(pb[:], bones_b, sq1, True, True, r=[t_cstb, tsg], w=[tpb])
                    yield
                    self.act(Ex1[:], pb[:], AF.Sqrt, r=[tpb], w=[tx1])
                    yield
                    self.ts(Ex1[:], Ex1[:], 1e-12, None, ALU.max, r=[tx1], w=[tx1])
                    yield
                    self.recip(Ex1[:], Ex1[:], r=[tx1], w=[tx1])
                    yield
                    self.tt(Ekap[:], Ekap[:], Ex1[:], ALU.mult, r=[tkap, tx1], w=[tkap])
                    yield
                    self.ts(Ekm[:], Ea[:], -1.0, V(85 + fc, 86 + fc), ALU.add, ALU.mult, r=[ta_, t_vecs], w=[tkm])
                    yield
                    self.stt(Ekm[:], Ekm[:], 1.0, Ek[:], ALU.add, ALU.mult, r=[tkm, tk_], w=[tkm])
                    yield
                    self.tt(Eb[:], Ekap[:], Ea[:], ALU.mult, r=[tkap, ta_], w=[tb_])
                    yield
                    self.stt(Ex1[:], Er[:], V(89 + fc, 90 + fc), Ekm[:], ALU.mult, ALU.mult, r=[tr_, t_vecs, tkm], w=[tx1])
                    yield
                    self.act(sq1, Ex1[:], AF.Copy, r=[tx1], w=[tsg])
                    yield
                    pb, tpb = bank()
                    self.mm(pb[:], bones_b, sq1, True, True, r=[t_cstb, tsg], w=[tpb])
                    yield
                    self.tt(Ex2[:], pb[:], Ev[:, fc, :], ALU.mult, r=[tpb, t_Ev[fc]], w=[tx2])
                    yield
                    self.dma(d_bonus[fc, :, cs], Ex2[:], r=[tx2], w=[t_dbonus[blk]])
                    yield
                    for q in range(4):
                        qs = slice(q * 128, (q + 1) * 128)
                        self.K.op("dve", lambda e, qs=qs, EL=EL, Eld=Eld: e.tensor_tensor_scan(EL[:, qs], ones_f, Eld[:, qs], 0.0, ALU.mult, ALU.add),
                                  [t_cst, tld], [tL])
                    gq = gcs[:, fc, :]
                    self.act(gq, EL[:].rearrange("p (q t) -> p q t", t=128)[:, :, 127], AF.Exp, r=[tL], w=[t_gcs[fc]])
                    yield
                    self.dma(d_gC[fc, :, blk * 4:(blk + 1) * 4], gq, r=[t_gcs[fc]], w=[t_dgC[blk]])
                    yield
                    self.tt(Ex1[:], EL[:], Eld[:], ALU.subtract, r=[tL, tld], w=[tx1])
                    yield
                    self.act(Ex1[:], Ex1[:], AF.Exp, r=[tx1], w=[tx1])
                    yield
                    self.tt(sg_[:, 0, :], Ekap[:], Ex1[:], ALU.mult, r=[tkap, tx1], w=[tsg])
                    yield
                    self.act(Ex1[:], EL[:], AF.Exp, r=[tL], w=[tx1])
                    yield
                    self.tt(sg_[:, 1, :], Er[:], Ex1[:], ALU.mult, r=[tr_, tx1], w=[tsg])
                    yield
                    self.act(Ex1[:], EL[:], AF.Exp, r=[tL], w=[tx1], scale=-1.0)
                    yield
                    self.tt(sg_[:, 2, :], Ekm[:], Ex1[:], ALU.mult, r=[tkm, tx1], w=[tsg])
                    yield
                    self.tt(sg_[:, 3, :], Eb[:], Ex1[:], ALU.mult, r=[tb_, tx1], w=[tsg])
                    yield
                    self.act(sg_[:, 4, :], Ev[:, fc, :], AF.Copy, r=[t_Ev[fc]], w=[tsg])
                    yield
                    self.dma(d_rw[:, fc, :, cs].rearrange("k p s -> p k s"), sg_[:, 0:5, :], r=[tsg], w=[t_drw[blk]])
                    pb, tpb = bank()
                    self.mm(pb[:], wlora[:, 1024 + fc * 128:1024 + (fc + 1) * 128], sgT[:], True, True, r=[t_wl, t_sgT], w=[tpb])
                    yield
                    self.act(sg_[:, 5, :], pb[:], AF.Copy, r=[tpb], w=[tsg])
                    yield
                    self.dma(d_gT[fc, :, cs], sg_[:, 5, :], r=[tsg], w=[t_dgT[blk]])
                    yield

                for f0 in (0, 2):
                    gens = [fcchain(f0, 0), fcchain(f0 + 1, 1)]
                    while gens:
                        for g in list(gens):
                            try: next(g)
                            except StopIteration: gens.remove(g)
            self.pop()
            if stop_after == f"1a{l}":
                break
            self.push()
            ld5 = [self.sb(f"ld5_{i}", [64, 5, 8, 128], BF16) for i in range(3)]; t_ld5 = [Trk() for _ in range(3)]
            tm_s = [self.sb(f"tm{i}", [128, 4, 8, 64], BF16) for i in range(3)]; t_tm_s = [[Trk() for _ in range(4)] for _ in range(3)]
            gCall = self.sb("gCall", [64, 8, NT]); t_gCall = Trk()
            self.dma(gCall[:], d_gC.rearrange("f (hp j) n -> j (f hp) n", hp=2), r=t_dgC, w=[t_gCall])
            A1_s = [self.sb(f"A1{i}", [128, 8, 256], BF16) for i in range(3)]; t_A1_s = [Trk() for _ in range(3)]
            A2_s = [self.sb(f"A2{i}", [128, 8, 256], BF16) for i in range(3)]; t_A2_s = [Trk() for _ in range(3)]
            Lm_s = [self.sb(f"Lm{i}", [128, 8, 128], BF16) for i in range(3)]; t_Lm_s = [Trk() for _ in range(3)]
            Pp_s = [[self.sb(f"Pp{j}_{i}", [128, 8, 128], BF16) for i in range(2)] for j in range(3)]; t_Pp_s = [[Trk(), Trk()] for _ in range(3)]
            Qq_s = [[self.sb(f"Qq{j}_{i}", [128, 8, 128], BF16) for i in range(2)] for j in range(3)]; t_Qq_s = [[Trk(), Trk()] for _ in range(3)]
            Yf_s = [self.sb(f"Yf{i}", [128, 8, 128]) for i in range(3)]; t_Yf_s = [Trk() for _ in range(3)]
            Yb_s = [self.sb(f"Yb{i}", [128, 8, 128], BF16) for i in range(3)]; t_Yb_s = [Trk() for _ in range(3)]
            kapP_s = [self.sb(f"kapP{i}", [64, 8, 128], BF16) for i in range(3)]; t_kapP_s = [Trk() for _ in range(3)]
            LkV_s = [self.sb(f"LkV{i}", [128, 8, 64], BF16) for i in range(3)]; t_LkV_s = [Trk() for _ in range(3)]
            Uloc_s = [self.sb(f"Uloc{i}", [128, 8, 64]) for i in range(3)]; t_Uloc_s = [Trk() for _ in range(3)]
            Ub = self.sb("Ub", [128, 8, 64], BF16); t_Ub = Trk()
            Un = self.sb("Un", [128, 8, 64], BF16); t_Un = Trk()
            KV_s = [self.sb(f"KV{i}", [64, 8, 64]) for i in range(3)]; t_KV_s = [Trk() for _ in range(3)]
            Tst = self.sb("Tst", [64, 8, 64]); t_T = Trk()
            Tb = [self.sb(f"Tb{i}", [64, 8, 64], BF16) for i in range(2)]; t_Tb = [Trk(), Trk()]
            Ttmp = self.sb("Ttmp", [64, 8, 64]); t_Ttmp = Trk()
            Of = self.sb("Of", [128, 8, 64]); t_Of = Trk()
            On = self.sb("On", [128, 8, 64]); t_On = Trk()
            gst = self.sb("gst", [128, 8, 8]); t_gst = Trk()
            gmv = self.sb("gmv", [128, 8, 2]); t_gmv = Trk()
            bon_s = [self.sb(f"bon{i}", [128, 4, 128]) for i in range(3)]; t_bon_s = [Trk() for _ in range(3)]
            gTt_s = [self.sb(f"gTt{i}", [128, 4, 128], BF16) for i in range(3)]; t_gTt_s = [Trk() for _ in range(3)]
            R1 = self.sb("R1", [128, 4, 128]); t_R1 = Trk()
            rot = self.sb("rot", [128, 4, 128], BF16); t_rot = Trk()
            self.memset(Tst[:], 0.0, w=[t_T])
            self.memset(Tb[0][:], 0.0, w=[t_Tb[0]])
            mask_ui = cstb[:, 128:384].rearrange("p (o n) -> p o n", o=1)
            mask_sl = m_sl_b.rearrange("p (o n) -> p o n", o=1)
            identb3 = ident_f.rearrange("p (o n) -> p o n", o=1)
            def bfv(pb):
                return pb[:].bitcast(BF16)
            def chunk(c):
                st = c % 3
                A1 = A1_s[st]; t_A1 = t_A1_s[st]
                A2 = A2_s[st]; t_A2 = t_A2_s[st]
                Lm = Lm_s[st]; t_Lm = t_Lm_s[st]
                Yf = Yf_s[st]; t_Yf = t_Yf_s[st]
                Yb = Yb_s[st]; t_Yb = t_Yb_s[st]
                kapP = kapP_s[st]; t_kapP = t_kapP_s[st]
                LkV = LkV_s[st]; t_LkV = t_LkV_s[st]
                Uloc = Uloc_s[st]; t_Uloc = t_Uloc_s[st]
                KV = KV_s[st]; t_KV = t_KV_s[st]
                bon = bon_s[st]; t_bon = t_bon_s[st]
                gTt = gTt_s[st]; t_gTt = t_gTt_s[st]
                tm = tm_s[st]; t_tm = t_tm_s[st]
                Pp = Pp_s[st]; t_Pp = t_Pp_s[st]
                Qq = Qq_s[st]; t_Qq = t_Qq_s[st]
                blk = c // 4; cs = slice(c * 128, (c + 1) * 128)
                lb = ld5[c % 3]; tlb = t_ld5[c % 3]
                for k5 in range(5):
                    self.dma(lb[:, k5, :, :], d_rw[k5].rearrange("f (hp j) s -> j (f hp) s", hp=2)[:, :, cs],
                             r=[t_drw[blk]], w=[tlb])
                self.dma(bon[:], d_bonus.rearrange("f p s -> p f s")[:, :, cs], r=[t_dbonus[blk]], w=[t_bon])
                self.dma(gTt[:], d_gT.rearrange("f p s -> p f s")[:, :, cs], r=[t_dgT[blk]], w=[t_gTt])
                for ti, k5 in enumerate((0, 2, 3, 4)):
                    pb, tpb = bank()
                    pv = bfv(pb)
                    for h in range(8):
                        self.tr(pv[:, h * 64:(h + 1) * 64], lb[0:64, k5, h, :], ident_b[0:64, 0:64], r=[tlb, t_cstb], w=[tpb])
                    self.cp(tm[:, ti, :, :], pv[:, 0:512].rearrange("p (h i) -> p h i", h=8), r=[tpb], w=[t_tm[ti]])
                yield 0
                for hp in range(4):
                    for which, (Adst, tA, k5) in enumerate(((A1, t_A1, 2), (A2, t_A2, 3))):
                        pb, tpb = bank()
                        for hh in range(2):
                            h = 2 * hp + hh
                            self.mm(pb[:, hh * 256:(hh + 1) * 256], lb[0:64, k5, h, :], lb[0:64, 0:2, h, :], True, True, r=[tlb], w=[tpb])
                        self.tt(Adst[:, 2 * hp:2 * hp + 2, :], pb[:].rearrange("p (h n) -> p h n", h=2),
                                mask_ui.broadcast_to([128, 2, 256]), ALU.mult, r=[tpb, t_cstb], w=[tA])
                for half in range(2):
                    pb, tpb = bank()
                    for hh in range(4):
                        h = 4 * half + hh
                        self.mm(pb[:, hh * 128:(hh + 1) * 128], lb[0:64, 0, h, :], lb[0:64, 3, h, :], True, True, r=[tlb], w=[tpb])
                    self.tt(Lm[:, 4 * half:4 * half + 4, :], pb[:].rearrange("p (h n) -> p h n", h=4),
                            mask_sl.broadcast_to([128, 4, 128]), ALU.mult, r=[tpb, t_cstb], w=[t_Lm])
                yield 0
                Nm = A2[:, :, 0:128]
                self.tt(Yf[:], identb3.broadcast_to([128, 8, 128]), Nm, ALU.subtract, r=[t_cst, t_A2], w=[t_Yf])
                self.act(Yb[:], Yf[:], AF.Copy, r=[t_Yf], w=[t_Yb])
                Qc, tQc, Pc, tPc = Lm, t_Lm, Nm, t_A2
                for k in range(6):
                    yield 0
                    Qn, tQn = Qq[k % 2], t_Qq[k % 2]
                    Pn, tPn = Pp[k % 2], t_Pp[k % 2]
                    for half in range(2):
                        pb, tpb = bank()
                        for hh in range(4):
                            h = 4 * half + hh
                            self.mm(pb[:, hh * 128:(hh + 1) * 128], Pc[:, h, :], Qc[:, h, :], True, True, r=[tPc, tQc], w=[tpb])
                        self.act(Qn[:, 4 * half:4 * half + 4, :], pb[:].rearrange("p (h n) -> p h n", h=4), AF.Copy, r=[tpb], w=[tQn])
                    if k < 5:
                        for half in range(2):
                            pb, tpb = bank()
                            for hh in range(4):
                                h = 4 * half + hh
                                self.mm(pb[:, hh * 128:(hh + 1) * 128], Qc[:, h, :], Pc[:, h, :], True, True, r=[tPc, tQc], w=[tpb])
                            self.cp(Pn[:, 4 * half:4 * half + 4, :], pb[:].rearrange("p (h n) -> p h n", h=4), r=[tpb], w=[tPn])
                    yield 0
                    for half in range(2):
                        pb, tpb = bank()
                        for hh in range(4):
                            h = 4 * half + hh
                            self.mm(pb[:, hh * 128:(hh + 1) * 128], Qn[:, h, :], Yb[:, h, :], True, True, r=[tQn, t_Yb], w=[tpb])
                        self.tt(Yf[:, 4 * half:4 * half + 4, :], Yf[:, 4 * half:4 * half + 4, :],
                                pb[:].rearrange("p (h n) -> p h n", h=4), ALU.add, r=[tpb, t_Yf], w=[t_Yf])
                    self.act(Yb[:], Yf[:], AF.Copy, r=[t_Yf], w=[t_Yb])
                    Qc, tQc, Pc, tPc = Qn, tQn, Pn, tPn
                yield 0
                for half in range(2):
                    pb, tpb = bank()
                    for hh in range(4):
                        h = 4 * half + hh
                        self.mm(pb[0:64, hh * 128:(hh + 1) * 128], tm[:, 0, h, :], Yb[:, h, :], True, True, r=[t_tm[0], t_Yb], w=[tpb])
                    self.act(kapP[:, 4 * half:4 * half + 4, :], pb[0:64, :].rearrange("p (h n) -> p h n", h=4), AF.Copy, r=[tpb], w=[t_kapP])
                pb, tpb = bank()
                for h in range(8):
                    self.mm(pb[0:64, h * 64:(h + 1) * 64], tm[:, 1, h, :], tm[:, 3, h, :], True, True, r=[t_tm[1], t_tm[3]], w=[tpb])
                self.cp(KV[:], pb[0:64, :].rearrange("p (h n) -> p h n", h=8), r=[tpb], w=[t_KV])
                pb, tpb = bank()
                for h in range(8):
                    self.mm(pb[:, h * 64:(h + 1) * 64], A1[:, h, 0:128], tm[:, 3, h, :], True, True, r=[t_A1, t_tm[3]], w=[tpb])
                self.act(LkV[:], pb[:].rearrange("p (h n) -> p h n", h=8), AF.Copy, r=[tpb], w=[t_LkV])
                pb, tpb = bank()
                for h in range(8):
                    self.mm(pb[:, h * 64:(h + 1) * 64], Yb[:, h, :], LkV[:, h, :], True, True, r=[t_Yb, t_LkV], w=[tpb])
                self.cp(Uloc[:], pb[:].rearrange("p (h n) -> p h n", h=8), r=[tpb], w=[t_Uloc])
                yield 1
                Tc, tTc = Tb[c % 2], t_Tb[c % 2]
                Tn_, tTn = Tb[(c + 1) % 2], t_Tb[(c + 1) % 2]
                pb, tpb = bank()
                for h in range(8):
                    self.mm(pb[:, h * 64:(h + 1) * 64], kapP[0:64, h, :], Tc[0:64, h, :], True, True, r=[t_kapP, tTc], w=[tpb])
                self.tt(Ub[:], pb[:].rearrange("p (h n) -> p h n", h=8), Uloc[:], ALU.add, r=[tpb, t_Uloc], w=[t_Ub])
                self.ts(Un[:], Ub[:], -1.0, None, ALU.mult, r=[t_Ub], w=[t_Un], eng="pool")
                self.tt(Ttmp[:], Tst[:], KV[:], ALU.add, r=[t_T, t_KV], w=[t_Ttmp])
                pb2, tpb2 = bank()
                for h in range(8):
                    self.mm(pb2[0:64, h * 64:(h + 1) * 64], tm[:, 2, h, :], Ub[:, h, :], True, True, r=[t_tm[2], t_Ub], w=[tpb2])
                self.tt(Ttmp[:], Ttmp[:], pb2[0:64, :].rearrange("p (h n) -> p h n", h=8), ALU.subtract, r=[t_Ttmp, tpb2], w=[t_Ttmp])
                self.tt(Tst[:], Ttmp[:], gCall[:, :, c:c + 1].broadcast_to([64, 8, 64]), ALU.mult, r=[t_Ttmp, t_gCall], w=[t_T])
                self.act(Tn_[:], Tst[:], AF.Copy, r=[t_T], w=[tTn])
                pb, tpb = bank()
                for h in range(8):
                    o_ = pb[:, h * 64:(h + 1) * 64]
                    self.mm(o_, lb[0:64, 1, h, :], Tc[0:64, h, :], True, False, r=[tlb, tTc], w=[tpb])
                    self.mm(o_, A1[:, h, 128:256], tm[:, 3, h, :], False, False, r=[t_A1, t_tm[3]], w=[tpb])
                    self.mm(o_, A2[:, h, 128:256], Un[:, h, :], False, True, r=[t_A2, t_Un], w=[tpb])
                self.act(Of[:], pb[:].rearrange("p (h n) -> p h n", h=8), AF.Copy, r=[tpb], w=[t_Of])
                for h in range(8):
                    self.K.op("dve", lambda e, h=h, gst=gst, Of=Of: e.bn_stats(gst[:, h, 0:6], Of[:, h, :]), [t_Of], [t_gst])
                for h in range(8):
                    self.K.op("dve", lambda e, h=h, gst=gst, gmv=gmv: e.bn_aggr(gmv[:, h, :], gst[:, h, 0:6]), [t_gst], [t_gmv])
                self.act(gst[:, :, 6], gmv[:, :, 1], AF.Sqrt, r=[t_gmv, t_cst], w=[t_gst], bias=c_gneps)
                self.recip(gst[:, :, 7], gst[:, :, 6], r=[t_gst], w=[t_gst])
                self.tt(On[:], Of[:], gmv[:, :, 0:1].broadcast_to([128, 8, 64]), ALU.subtract, r=[t_Of, t_gmv], w=[t_On])
                self.tt(On[:], On[:], gst[:, :, 7:8].broadcast_to([128, 8, 64]), ALU.mult, r=[t_On, t_gst], w=[t_On])
                pb, tpb = bank()
                for fc in range(4):
                    self.tr(pb[:, fc * 128:(fc + 1) * 128], On[:, 2 * fc:2 * fc + 2, :].rearrange("p h i -> p (h i)"), ident_f,
                            r=[t_On, t_cst], w=[tpb])
                for fc in range(4):
                    self.act(R1[:, fc, :], pb[:, fc * 128:(fc + 1) * 128], AF.Identity, r=[tpb, t_vecs], w=[t_R1],
                             scale=V(93 + fc, 94 + fc), bias=V(97 + fc, 98 + fc))
                self.tt(R1[:], R1[:], bon[:], ALU.add, r=[t_R1, t_bon], w=[t_R1])
                self.tt(rot[:], R1[:], gTt[:], ALU.mult, r=[t_R1, t_gTt], w=[t_rot])
                self.dma(d_ro.rearrange("f p s -> p f s")[:, :, cs], rot[:], r=[t_rot], w=[t_dro[c]], q="pool")
            def drain(g):
                for _ in g: pass
            for c0 in range(0, NT, 3):
                gs = [chunk(c0 + i) for i in range(min(3, NT - c0))]
                live = list(gs)
                while live:
                    for g in list(live):
                        if next(g) == 1: live.remove(g)
                for g in gs: drain(g)
            self.pop()
            if stop_after == f"1b{l}":
                break
            self.push()
            cosB = [self.sb(f"cosB{i}", [128, 512]) for i in range(2)]; sinB = [self.sb(f"sinB{i}", [128, 512]) for i in range(2)]
            t_ropeB = [Trk(), Trk()]
            qnT = self.sb("qnT", [128, 2, S], BF16); t_qnT = Trk()
            ckvT = self.sb("ckvT", [128, S], BF16); t_ckvT = Trk()
            kTb = [self.sb(f"kTb{i}", [128, S], BF16) for i in range(2)]; t_kT = [Trk(), Trk()]
            self.dma(qnT[:], d_qnT[0:2].rearrange("c p s -> p c s"), r=t_dqn, w=[t_qnT])
            self.dma(ckvT[:], d_qnT[2], r=t_dqn, w=[t_ckvT])
            for i in range(2):
                self.dma(kTb[i][64:96, :], d_krot[64:96, :], r=t_dkrot, w=[t_kT[i]])
            wuq = self.sb("wuq", [128, 2, 1536], BF16); wukv = self.sb("wukv", [128, 1024], BF16); wo = self.sb("wo", [128, 8, D], BF16)
            t_w2 = Trk()
            self.dma(wuq[:], w_uq_in[l], w=[t_w2], q="pool")
            self.dma(wukv[:], w_ukv_in[l], w=[t_w2], q="pool")
            self.dma(wo[:], w_o_in[l], w=[t_w2], q="pool")
            Vaug = self.sb("Vaug", [128, NT, 8, 65], BF16); t_V = [Trk() for _ in range(NT)]
            self.K.op("pool", lambda e, Vaug=Vaug: e.memset(Vaug[:], 1.0), (), t_V)
            lnB = [self.sb(f"lnB{i}", [128, D]) for i in range(2)]; t_lnB = [Trk(), Trk()]
            self.dma(lnB[0][:], rows_in[l:l + 1, 0:D].broadcast_to([128, D]), w=[t_lnB[0]])
            self.dma(lnB[1][:], rows_in[l:l + 1, D:2 * D].broadcast_to([128, D]), w=[t_lnB[1]])
            gtB = self.sb("gtB", [128, D]); t_gtB = Trk()
            dg = self.sb("dg", [128, 128]); t_dg = Trk()
            for half in range(2):
                pb, tpb = bank()
                for q in range(4):
                    c = half * 4 + q
                    self.ts(dg[:], ident_f, modT[:, l, 16 + c:17 + c], None, ALU.mult, r=[t_cst, t_mod], w=[t_dg])
                    self.mm(pb[:, q * 128:(q + 1) * 128], ones_f, dg[:], True, True, r=[t_cst, t_dg], w=[tpb])
                self.cp(gtB[:, half * 512:(half + 1) * 512], pb[:], r=[tpb], w=[t_gtB])
            qTh = [self.sb(f"qTh{i}", [128, 512], BF16) for i in range(2)]; t_qTh = [Trk(), Trk()]
            rq1 = self.sb("rq1", [128, 512]); rq2 = self.sb("rq2", [128, 512]); t_rq = Trk()
            PT = [self.sb(f"PT{i}", [128, 512], BF16) for i in range(4)]; t_PT = [Trk() for _ in range(4)]
            oTs = self.sb("oTs", [128, 512]); t_oTs = Trk()
            o_tm = self.sb("o_tm", [128, 4, 512]); t_otm = Trk()
            rec = self.sb("rec", [128, 8]); t_rec = Trk()
            junk = self.sb("junk", [128, 512]); t_junk = Trk()
            mixT = self.sb("mixT", [128, 8, 512], BF16); t_mixT = Trk()
            ymix = self.sb("ymix", [128, D]); t_ymix = Trk()
            SCALE = float(96 ** -0.5)
            ptc = [0]
            ptn = 0
            for QB in range(NB):
                cs = slice(QB * 512, (QB + 1) * 512)
                cosF_, sinF_, t_rope = cosB[QB % 2], sinB[QB % 2], t_ropeB[QB % 2]
                self.dma(cosF_[64:96, :], d_rope[0][:, cs], r=[t_drope], w=[t_rope])
                self.dma(sinF_[64:96, :], d_rope[1][:, cs], r=[t_drope], w=[t_rope])
                for j in range(4):
                    tt_ = QB * 4 + j
                    pb, tpb = bank()
                    self.mm(pb[:], ckvT[:, tt_ * 128:(tt_ + 1) * 128], wukv[:, 512:1024], True, True, r=[t_ckvT, t_w2], w=[tpb])
                    self.cp(Vaug[:, tt_, :, 0:64], pb[:].rearrange("p (h i) -> p h i", h=8), r=[tpb], w=[t_V[tt_]])
                def prep(h):
                    kb_ = kTb[h % 2]; tkb = t_kT[h % 2]
                    for kb in range(QB + 1):
                        pb, tpb = bank()
                        self.mm(pb[0:64, :], wukv[:, h * 64:(h + 1) * 64], ckvT[:, kb * 512:(kb + 1) * 512], True, True, r=[t_w2, t_ckvT], w=[tpb])
                        self.act(kb_[0:64, kb * 512:(kb + 1) * 512], pb[0:64, :], AF.Copy, r=[tpb], w=[tkb])
                    qh = qTh[h % 2]; tqh = t_qTh[h % 2]
                    pbA, tpA = bank()
                    for kc in range(2):
                        self.mm(pbA[0:96, :], wuq[:, kc, h * 96:(h + 1) * 96], qnT[:, kc, cs], kc == 0, kc == 1, r=[t_w2, t_qnT], w=[tpA])
                    pbB, tpB = bank()
                    for kc in range(2):
                        self.mm(pbB[0:96, :], wuq[:, kc, 768 + h * 96:768 + (h + 1) * 96], qnT[:, kc, cs], kc == 0, kc == 1, r=[t_w2, t_qnT], w=[tpB])
                    self.act(qh[0:64, :], pbA[0:64, :], AF.Copy, r=[tpA], w=[tqh])
                    self.tt(rq1[64:96, :], pbA[64:96, :], cosF_[64:96, :], ALU.mult, r=[tpA, t_rope], w=[t_rq])
                    self.tt(rq2[64:96, :], pbB[64:96, :], sinF_[64:96, :], ALU.mult, r=[tpB, t_rope], w=[t_rq])
                    self.tt(qh[64:96, :], rq1[64:96, :], rq2[64:96, :], ALU.add, r=[t_rq], w=[tqh])
                def attn(h):
                    kb_ = kTb[h % 2]; tkb = t_kT[h % 2]
                    qh = qTh[h % 2]; tqh = t_qTh[h % 2]
                    pbO, tpO = PS[7], t_PS[7]
                    nkt = QB * 4 + 4
                    def smm(kt):
                        j = kt - QB * 4
                        n0 = 0 if j < 0 else j * 128
                        N = 512 - n0
                        pbS, tpS = bank()
                        self.mm(pbS[:, 0:N], kb_[0:96, kt * 128:(kt + 1) * 128], qh[0:96, n0:512], True, True, r=[tkb, tqh], w=[tpS])
                        return pbS, tpS, j, n0, N
                    nxt = smm(0)
                    for kt in range(nkt):
                        pbS, tpS, j, n0, N = nxt
                        if kt + 1 < nkt: nxt = smm(kt + 1)
                        pt = PT[ptc[0] % 4]; tpt = t_PT[ptc[0] % 4]; ptc[0] += 1
                        self.act(pt[:, 0:N], pbS[:, 0:N], AF.Exp, r=[tpS], w=[tpt], scale=SCALE)
                        if j >= 0:
                            self.tt(pt[:, 0:128], pt[:, 0:128], m_iu_b, ALU.mult, r=[tpt, t_cstb], w=[tpt])
                        self.mm(pbO[0:65, n0:512], Vaug[:, kt, h, :], pt[:, 0:N], kt == 0, kt == nkt - 1, r=[t_V[kt], tpt], w=[tpO])
                    self.act(oTs[0:65, :], pbO[0:65, :], AF.Copy, r=[tpO], w=[t_oTs])
                    pbt, tpt_ = bank()
                    for j in range(4):
                        self.tr(pbt[:, j * 65:(j + 1) * 65], oTs[0:65, j * 128:(j + 1) * 128], ident_f[0:65, 0:65], r=[t_oTs, t_cst], w=[tpt_])
                    pv = pbt[:, 0:260].rearrange("p (j n) -> p j n", j=4)
                    self.recip(rec[:, 0:4], pv[:, :, 64], r=[tpt_], w=[t_rec])
                    self.tt(o_tm[:, :, h * 64:(h + 1) * 64], pv[:, :, 0:64], rec[:, 0:4].rearrange("p (j o) -> p j o", o=1).broadcast_to([128, 4, 64]),
                            ALU.mult, r=[tpt_, t_rec], w=[t_otm])
                prep(0)
                for h in range(8):
                    if h + 1 < 8: prep(h + 1)
                    attn(h)
                self.dma(mixT[:, 4:8, :], d_ro.rearrange("f p s -> p f s")[:, :, cs], r=t_dro[QB * 4:QB * 4 + 4], w=[t_mixT])
                for j in range(4):
                    self.act(junk[:], o_tm[:, j, :], AF.Square, r=[t_otm], w=[t_junk, t_rec], accum=rec[:, 4:5])
                    self.act(rec[:, 5:6], rec[:, 4:5], AF.Sqrt, r=[t_rec, t_cst], w=[t_rec], scale=1.0 / 512, bias=c_rmseps)
                    self.recip(rec[:, 6:7], rec[:, 5:6], r=[t_rec], w=[t_rec])
                    self.ts(o_tm[:, j, :], o_tm[:, j, :], rec[:, 6:7], None, ALU.mult, r=[t_otm, t_rec], w=[t_otm])
                    pb, tpb = bank()
                    for c in range(4):
                        self.tr(pb[:, c * 128:(c + 1) * 128], o_tm[:, j, c * 128:(c + 1) * 128], ident_f, r=[t_otm, t_cst], w=[tpb])
                    for c in range(4):
                        self.act(mixT[:, c, j * 128:(j + 1) * 128], pb[:, c * 128:(c + 1) * 128], AF.Copy, r=[tpb, t_vecs], w=[t_mixT],
                                 scale=V(69 + c, 70 + c))
                if f"mixT{l}" in dbg:
                    if QB == 0: self.dbg_mix = self.dout(f"dbg_mixT{l}", [128, 8, S], BF16)
                    self.dma(self.dbg_mix[:, :, cs], mixT[:], r=[t_mixT])
                for j in range(4):
                    tt_ = QB * 4 + j; bi = tt_ % 2
                    self.dma(xt[bi][:], xres[tt_ * 128:(tt_ + 1) * 128, :], r=[t_xres[tt_]], w=[t_xt[bi]])
                    for half in range(2):
                        pb, tpb = bank()
                        for c in range(8):
                            self.mm(pb[:], mixT[:, c, j * 128:(j + 1) * 128], wo[:, c, half * 512:(half + 1) * 512], c == 0, c == 7,
                                    r=[t_mixT, t_w2], w=[tpb])
                        self.tt(ymix[:, half * 512:(half + 1) * 512], pb[:], gtB[:, half * 512:(half + 1) * 512], ALU.mult, r=[tpb, t_gtB], w=[t_ymix])
                    self.stt(xt[bi][:], xt[bi][:], ALPHA, ymix[:], ALU.mult, ALU.add, r=[t_xt[bi], t_ymix], w=[t_xt[bi]])
                    layernorm(xt[bi][:], t_xt[bi], lnB[0][:], lnB[1][:], t_lnB[0], t_lnB[1])
                    self.dma(xmid[tt_ * 128:(tt_ + 1) * 128, :], xt[bi][:], r=[t_xt[bi]], w=[t_xmid[tt_]], q="pool")
            self.pop()
            if "pt_at2" in dbg:
                tst = self.sb("tst", [128, 64]); t_tst = Trk()
                self.memset(tst[:], 3.0, w=[t_tst], eng="pool")
                o = self.dout("dbg_pt2", [128, 64]); self.dma(o[:, :], tst[:], r=[t_tst])
            if stop_after == f"2{l}":
                break
            self.push()
            NBLK = 4 * NT + NE; NSLOT = NBLK * 128; BIG = 1000000.0
            w1rows = self.ins.get("exp_w1"); w2rows = self.ins.get("exp_w2")
            if w1rows is None: w1rows = self.din("exp_w1", [L * NE * 128, 8 * 2 * D])
            if w2rows is None: w2rows = self.din("exp_w2", [L * NE * 128, 8 * D])
            b1rows = b1v_in
            d_hs = self.scr_hs; d_ys = self.scr_ys
            t_hs = [Trk() for _ in range(NBLK)]; t_ys = [Trk() for _ in range(NBLK)]
            lnB = [self.sb(f"ln2B{i}", [128, D]) for i in range(2)]; t_lnB = [Trk(), Trk()]
            self.dma(lnB[0][:], rows_in[l:l + 1, 2 * D:3 * D].broadcast_to([128, D]), w=[t_lnB[0]])
            self.dma(lnB[1][:], rows_in[l:l + 1, 3 * D:4 * D].broadcast_to([128, D]), w=[t_lnB[1]])
            b2all = self.sb("b2all", [NE, D], BF16); t_rw = Trk()
            self.dma(b2all[:], exp_b2_in[l], w=[t_rw], q="pool")
            gates = self.sb("gates", [128, NT, NE]); t_gates = [Trk() for _ in range(NT)]
            gk = self.sb("gk", [128, NT, 4]); t_gk = [Trk() for _ in range(NT)]
            slotidx = self.sb("slotidx", [128, NT, 4], I32); t_slotidx = [Trk() for _ in range(NT)]
            widx = self.sb("widx", [128, NBLK], I32); t_widx = Trk()
            gtB = self.sb("gt2B", [128, D]); t_gtB = Trk()
            self.push()
            rbB = self.sb("rbB", [128, NE]); t_rbB = Trk()
            self.dma(rbB[:], rows_in[l:l + 1, 4 * D:4 * D + NE].broadcast_to([128, NE]), w=[t_rbB])
            rwf = self.sb("rwf", [128, 8, NE]); h2f_s = [self.sb(f"h2f{i}", [128, 8, 128]) for i in range(4)]; t_h2f_s = [Trk() for _ in range(4)]
            xr_s = [self.sb(f"xr{i}", [128, D]) for i in range(4)]; t_xr_s = [Trk() for _ in range(4)]
            self.dma(rwf[:], router_w_in[l], w=[t_rw])
            dg = self.sb("dg2", [128, 128]); t_dg = Trk()
            def bcast_tile(name, col0, tl=None, ttr=None):
                if tl is None:
                    tl = self.sb(name, [128, D]); ttr = Trk()
                for half in range(2):
                    pb, tpb = bank()
                    for q in range(4):
                        c = half * 4 + q
                        self.ts(dg[:], ident_f, modT[:, l, col0 + c:col0 + c + 1], None, ALU.mult, r=[t_cst, t_mod], w=[t_dg])
                        self.mm(pb[:, q * 128:(q + 1) * 128], ones_f, dg[:], True, True, r=[t_cst, t_dg], w=[tpb])
                    self.cp(tl[:, half * 512:(half + 1) * 512], pb[:], r=[tpb], w=[ttr])
                return tl, ttr
            bcast_tile("gt2B", 40, gtB, t_gtB)
            scB, t_scB = bcast_tile("sc2B", 32)
            shB, t_shB = bcast_tile("sh2B", 24)
            lgs = self.sb("lgs", [128, NT, NE]); t_lgs = [Trk() for _ in range(NT)]
            m8s = self.sb("m8s", [128, NT, 8]); t_m8s = [Trk() for _ in range(NT)]
            maskb = self.sb("maskb", [128, NT, NE], BF16); t_maskb = [Trk() for _ in range(NT)]
            lg_s = [self.sb(f"lg{i}", [128, NE]) for i in range(4)]; m8_s = [self.sb(f"m8{i}", [128, 16]) for i in range(4)]; t_lg_s = [Trk() for _ in range(4)]; t_m8_s = [Trk() for _ in range(4)]
            msk_s = [self.sb(f"msk{i}", [128, NE]) for i in range(4)]; t_msk_s = [Trk() for _ in range(4)]
            def route_tile(tt_):
                st = tt_ % 4
                h2f, t_h2f, lg, t_lg, m8, t_m8, msk, t_msk = h2f_s[st], t_h2f_s[st], lg_s[st], t_lg_s[st], m8_s[st], t_m8_s[st], msk_s[st], t_msk_s[st]
                xq, t_xq = xr_s[st], t_xr_s[st]
                self.dma(xq[:], xmid[tt_ * 128:(tt_ + 1) * 128, :], r=[t_xmid[tt_]], w=[t_xq])
                yield
                for half in range(2):
                    pb, tpb = bank()
                    for q in range(4):
                        c = half * 4 + q
                        self.tr(pb[:, q * 128:(q + 1) * 128], xq[:, c * 128:(c + 1) * 128], ident_f, r=[t_xq, t_cst], w=[tpb])
                    for q in range(4):
                        c = half * 4 + q
                        self.ts(h2f[:, c, :], pb[:, q * 128:(q + 1) * 128], modT[:, l, 32 + c:33 + c], modT[:, l, 24 + c:25 + c], ALU.mult, ALU.add,
                                r=[tpb, t_mod], w=[t_h2f])
                pb, tpb = bank()
                for kc in range(8):
                    self.mm(pb[:, 0:NE], h2f[:, kc, :], rwf[:, kc, :], kc == 0, kc == 7, r=[t_h2f, t_rw], w=[tpb])
                self.tt(lgs[:, tt_, :], pb[:, 0:NE], rbB[:], ALU.add, r=[tpb, t_rbB], w=[t_lgs[tt_]])
                yield
                self.K.op("dve", lambda e, m8s=m8s, lgs=lgs, tt_=tt_: e.max(out=m8s[:, tt_, :], in_=lgs[:, tt_, :]), [t_lgs[tt_]], [t_m8s[tt_]])
                yield
                self.ts(msk[:], lgs[:, tt_, :], m8s[:, tt_, 3:4], None, ALU.is_ge, r=[t_lgs[tt_], t_m8s[tt_]], w=[t_msk])
                yield
                self.cp(maskb[:, tt_, :], msk[:], r=[t_msk], w=[t_maskb[tt_]])
                yield
                self.ts(m8[:, 8:9], m8s[:, tt_, 0:1], -1.0, None, ALU.mult, r=[t_m8s[tt_]], w=[t_m8])
                yield
                self.act(lg[:], lgs[:, tt_, :], AF.Exp, r=[t_lgs[tt_], t_m8], w=[t_lg], bias=m8[:, 8:9])
                yield
                self.tt(lg[:], lg[:], msk[:], ALU.mult, r=[t_lg, t_msk], w=[t_lg])
                yield
                self.K.op("dve", lambda e, m8=m8, lg=lg: e.reduce_sum(m8[:, 9:10], lg[:], AX.X), [t_lg], [t_m8])
                yield
                self.recip(m8[:, 10:11], m8[:, 9:10], r=[t_m8], w=[t_m8])
                yield
                self.ts(gates[:, tt_, :], lg[:], m8[:, 10:11], None, ALU.mult, r=[t_lg, t_m8], w=[t_gates[tt_]])
                yield
            def rr(gens):
                gens = list(gens)
                while gens:
                    for g in list(gens):
                        try: next(g)
                        except StopIteration: gens.remove(g)
            for t0 in range(0, NT, 4):
                rr([route_tile(t0 + i) for i in range(min(4, NT - t0))])
            if f"gates{l}" in dbg:
                dbg_out(f"dbg_gates{l}", [128, NT, NE], gates[:], t_gates)
            rt = self.sb("rt", [128, 8, NE]); t_rt = Trk()
            rti = self.sb("rti", [128, NE], I32); t_rti = Trk()
            pb, tpb = bank()
            for tt_ in range(NT):
                self.mm(pb[:, 0:NE], ones_b, maskb[:, tt_, :], tt_ == 0, tt_ == NT - 1, r=[t_cstb, t_maskb[tt_]], w=[tpb])
            self.ts(rt[:, 1, :], pb[:, 0:NE], 127.0, 1.0 / 128, ALU.add, ALU.mult, r=[tpb], w=[t_rt])
            self.ts(rt[:, 1, :], rt[:, 1, :], -0.49609375, None, ALU.add, r=[t_rt], w=[t_rt])
            self.cp(rti[:], rt[:, 1, :], r=[t_rt], w=[t_rti])
            self.cp(rt[:, 2, :], rti[:], r=[t_rti], w=[t_rt])
            self.ts(rt[:, 3, :], rt[:, 2, :], 128.0, None, ALU.mult, r=[t_rt], w=[t_rt])
            self.K.op("dve", lambda e, rt=rt: e.tensor_tensor_scan(rt[:, 4, :], ones_f[:, 0:NE], rt[:, 3, :], 0.0, ALU.mult, ALU.add), [t_rt, t_cst], [t_rt])
            self.tt(rt[:, 5, :], rt[:, 4, :], rt[:, 3, :], ALU.subtract, r=[t_rt], w=[t_rt])
            bx = self.sb("bx", [128, 6, NBLK]); t_bx = Trk()
            pcol = self.sb("pcol", [128, 1]); t_pcol = Trk()
            b128 = cst[:, 1024:1024 + NBLK]
            self.memset(bx[:, 0, :], 0.0, w=[t_bx])
            for e_ in range(NE):
                self.stt(bx[:, 0, :], b128, rt[:, 4, e_:e_ + 1], bx[:, 0, :], ALU.is_ge, ALU.add, r=[t_cst, t_rt, t_bx], w=[t_bx])
            self.ts(bx[:, 1, :], bx[:, 0, :], 31.5, None, ALU.is_lt, r=[t_bx], w=[t_bx])
            self.memset(bx[:, 2, 0:1], 1.0, w=[t_bx])
            self.tt(bx[:, 2, 1:NBLK], bx[:, 0, 1:NBLK], bx[:, 0, 0:NBLK - 1], ALU.not_equal, r=[t_bx], w=[t_bx])
            self.memset(bx[:, 2, NBLK // 2:NBLK // 2 + 1], 1.0, w=[t_bx])
            self.tt(bx[:, 2, :], bx[:, 2, :], bx[:, 1, :], ALU.mult, r=[t_bx], w=[t_bx])
            self.ts(pcol[:], cst[:, 776:777], float(l * NE * 128) - BIG, None, ALU.add, r=[t_cst], w=[t_pcol])
            self.ts(bx[:, 3, :], bx[:, 0, :], 128.0, pcol[:, 0:1], ALU.mult, ALU.add, r=[t_bx, t_pcol], w=[t_bx])
            self.tt(bx[:, 3, :], bx[:, 3, :], bx[:, 2, :], ALU.mult, r=[t_bx], w=[t_bx])
            self.ts(bx[:, 3, :], bx[:, 3, :], BIG, None, ALU.add, r=[t_bx], w=[t_bx])
            self.cp(widx[:], bx[:, 3, :], r=[t_bx], w=[t_widx])
            if f"route{l}" in dbg:
                dbg_out(f"dbg_rt{l}", [128, 8, NE], rt[:], [t_rt]); dbg_out(f"dbg_bx{l}", [128, 6, NBLK], bx[:], [t_bx])
                dbg_out(f"dbg_widx{l}", [128, NBLK], widx[:], [t_widx], I32)
            slotf_s = [self.sb(f"slotf{i}", [128, NE]) for i in range(2)]; t_slotf_s = [Trk(), Trk()]
            oh_s = [self.sb(f"oh{i}", [128, NE]) for i in range(2)]; t_oh_s = [Trk(), Trk()]
            tmp32_s = [self.sb(f"tmp32{i}", [128, NE]) for i in range(2)]; t_tmp32_s = [Trk(), Trk()]
            sk_s = [self.sb(f"sk{i}", [128, 4]) for i in range(2)]; t_sk_s = [Trk(), Trk()]
            h2a_s = [self.sb(f"h2a{i}", [128, D]) for i in range(2)]; t_h2a_s = [Trk(), Trk()]
            h2tm = [self.sb(f"h2tm{i}", [128, D], BF16) for i in range(2)]; t_h2tm = [Trk(), Trk()]
            def slot_tile(tt_):
                bi = tt_ % 2
                slotf, t_slotf, oh, t_oh, tmp32, t_tmp32, sk, t_sk, h2a, t_h2a = [x[bi] for x in (slotf_s, t_slotf_s, oh_s, t_oh_s, tmp32_s, t_tmp32_s, sk_s, t_sk_s, h2a_s, t_h2a_s)]
                xq, t_xq = xr_s[bi], t_xr_s[bi]
                pb, tpb = bank()
                for u in range(tt_):
                    self.mm(pb[:, 0:NE], ones_b, maskb[:, u, :], u == 0, False, r=[t_cstb, t_maskb[u]], w=[tpb])
                    yield
                self.mm(pb[:, 0:NE], m_su_b, maskb[:, tt_, :], tt_ == 0, True, r=[t_cstb, t_maskb[tt_]], w=[tpb])
                yield
                self.tt(slotf[:], pb[:, 0:NE], rt[:, 5, :], ALU.add, r=[tpb, t_rt], w=[t_slotf])
                yield
                for k in range(4):
                    self.ts(oh[:], lgs[:, tt_, :], m8s[:, tt_, k:k + 1], None, ALU.is_equal, r=[t_lgs[tt_], t_m8s[tt_]], w=[t_oh])
                    yield
                    self.tt(tmp32[:], oh[:], slotf[:], ALU.mult, r=[t_oh, t_slotf], w=[t_tmp32])
                    yield
                    self.K.op("dve", lambda e, sk=sk, tmp32=tmp32, k=k: e.reduce_sum(sk[:, k:k + 1], tmp32[:], AX.X), [t_tmp32], [t_sk])
                    self.tt(tmp32[:], oh[:], gates[:, tt_, :], ALU.mult, r=[t_oh, t_gates[tt_]], w=[t_tmp32])
                    yield
                    self.K.op("dve", lambda e, gk=gk, tmp32=tmp32, k=k, tt_=tt_: e.reduce_sum(gk[:, tt_, k:k + 1], tmp32[:], AX.X), [t_tmp32], [t_gk[tt_]])
                self.cp(slotidx[:, tt_, :], sk[:], r=[t_sk], w=[t_slotidx[tt_]])
                yield
                self.dma(xq[:], xmid[tt_ * 128:(tt_ + 1) * 128, :], r=[t_xmid[tt_]], w=[t_xq])
                yield
                self.tt(h2a[:], xq[:], scB[:], ALU.mult, r=[t_xq, t_scB], w=[t_h2a])
                yield
                hb = h2tm[bi]; thb = t_h2tm[bi]
                self.tt(hb[:], h2a[:], shB[:], ALU.add, r=[t_h2a, t_shB], w=[thb])
                yield
                for k in range(4):
                    self.K.op("pool", lambda e, hb=hb, slotidx=slotidx, tt_=tt_, k=k: e.indirect_dma_start(
                        out=d_hs[:, :], out_offset=bass.IndirectOffsetOnAxis(ap=slotidx[:, tt_, k:k + 1], axis=0), in_=hb[:, :], in_offset=None,
                        bounds_check=self.breg(e, NSLOT - 1), oob_is_err=False), [thb, t_slotidx[tt_]], t_hs, dma=True)
            for t0 in range(0, NT, 2):
                rr([slot_tile(t0 + i) for i in range(min(2, NT - t0))])
            if f"route{l}" in dbg:
                dbg_out(f"dbg_slotidx{l}", [128, NT, 4], slotidx[:], t_slotidx, I32); dbg_out(f"dbg_gk{l}", [128, NT, 4], gk[:], t_gk)
            self.pop()
            self.push()
            W1s = [self.sb(f"W1_{i}", [128, 8, 2 * D], BF16) for i in range(2)]; t_W1s = [Trk(), Trk()]
            W2s = [self.sb(f"W2_{i}", [128, 8, D], BF16) for i in range(2)]; t_W2s = [Trk(), Trk()]
            b1ts = [self.sb(f"b1t{i}", [128, 16]) for i in range(2)]; t_b1s = [Trk(), Trk()]
            xs = [self.sb(f"xs{i}", [128, D], BF16) for i in range(2)]; t_xs = [Trk(), Trk()]
            xsT = [self.sb(f"xsT{i}", [128, 8, 128], BF16) for i in range(2)]; t_xsT = [Trk(), Trk()]
            uT = [self.sb(f"uT{i}", [128, 8, 128], BF16) for i in range(2)]; t_uT = [Trk(), Trk()]
            g1s = [self.sb(f"g1_{i}", [128, 4, 128]) for i in range(2)]; sgs = [self.sb(f"sg_{i}", [128, 4, 128]) for i in range(2)]
            l1s = [self.sb(f"l1_{i}", [128, 4, 128]) for i in range(2)]
            t_g1s = [Trk(), Trk()]; t_sgs = [Trk(), Trk()]; t_l1s = [Trk(), Trk()]
            yo = [self.sb(f"yo{i}", [128, D]) for i in range(2)]; t_yo = [Trk(), Trk()]
            order = []
            for i in range(NBLK // 2):
                order += [(i, 0), (NBLK // 2 + i, 1)]
            hnc = [0]
            def stG(seq):
                b, sid = order[seq]; bi = seq % 2
                W1, W2, b1t = W1s[sid], W2s[sid], b1ts[sid]; t_W1, t_W2, t_b1 = t_W1s[sid], t_W2s[sid], t_b1s[sid]
                self.K.op("pool", lambda e, W1=W1, widx=widx, b=b: e.indirect_dma_start(
                    out=W1[:].rearrange("p k n -> p (k n)"), out_offset=None, in_=w1rows[:, :],
                    in_offset=bass.IndirectOffsetOnAxis(ap=widx[:, b:b + 1], axis=0), bounds_check=self.breg(e, L * NE * 128 - 1), oob_is_err=False),
                    [t_widx], [t_W1], dma=True)
                self.K.op("pool", lambda e, W2=W2, widx=widx, b=b: e.indirect_dma_start(
                    out=W2[:].rearrange("p k n -> p (k n)"), out_offset=None, in_=w2rows[:, :],
                    in_offset=bass.IndirectOffsetOnAxis(ap=widx[:, b:b + 1], axis=0), bounds_check=self.breg(e, L * NE * 128 - 1), oob_is_err=False),
                    [t_widx], [t_W2], dma=True)
                self.K.op("pool", lambda e, b1t=b1t, widx=widx, b=b: e.indirect_dma_start(
                    out=b1t[:, :], out_offset=None, in_=b1rows[:, :],
                    in_offset=bass.IndirectOffsetOnAxis(ap=widx[:, b:b + 1], axis=0), bounds_check=self.breg(e, L * NE * 128 - 1), oob_is_err=False),
                    [t_widx], [t_b1], dma=True)
            def stT(seq):
                b, sid = order[seq]; bi = seq % 2
                W1, W2, b1t = W1s[sid], W2s[sid], b1ts[sid]; t_W1, t_W2, t_b1 = t_W1s[sid], t_W2s[sid], t_b1s[sid]
                self.dma(xs[bi][:], d_hs[b * 128:(b + 1) * 128, :], r=[t_hs[b]], w=[t_xs[bi]])
                pb, tpb = bank()
                pv = pb[:].bitcast(BF16)
                for c in range(8):
                    self.tr(pv[:, c * 128:(c + 1) * 128], xs[bi][:, c * 128:(c + 1) * 128], ident_b, r=[t_xs[bi], t_cstb], w=[tpb])
                self.act(xsT[bi][:], pv[:, 0:1024].rearrange("p (c n) -> p c n", c=8), AF.Copy, r=[tpb], w=[t_xsT[bi]])
            def stM1(seq):
                b, sid = order[seq]; bi = seq % 2
                W1, W2, b1t = W1s[sid], W2s[sid], b1ts[sid]; t_W1, t_W2, t_b1 = t_W1s[sid], t_W2s[sid], t_b1s[sid]
                for half in range(2):
                    gi = hnc[0] % 2; hnc[0] += 1
                    pG, tpG = bank(); pL, tpL = bank()
                    for q in range(4):
                        m = half * 4 + q
                        for kc in range(8):
                            self.mm(pG[:, q * 128:(q + 1) * 128], W1[:, kc, m * 128:(m + 1) * 128], xsT[bi][:, kc, :], kc == 0, kc == 7, r=[t_W1, t_xsT[bi]], w=[tpG])
                    for q in range(4):
                        m = half * 4 + q
                        for kc in range(8):
                            self.mm(pL[:, q * 128:(q + 1) * 128], W1[:, kc, D + m * 128:D + (m + 1) * 128], xsT[bi][:, kc, :], kc == 0, kc == 7, r=[t_W1, t_xsT[bi]], w=[tpL])
                    g1_, sg_, l1_ = g1s[gi], sgs[gi], l1s[gi]
                    bg = b1t[:, half * 4:half * 4 + 4].rearrange("p (q o) -> p q o", o=1).broadcast_to([128, 4, 128])
                    bl = b1t[:, 8 + half * 4:8 + half * 4 + 4].rearrange("p (q o) -> p q o", o=1).broadcast_to([128, 4, 128])
                    self.tt(g1_[:], pG[:].rearrange("p (q n) -> p q n", q=4), bg, ALU.add, r=[tpG, t_b1], w=[t_g1s[gi]])
                    self.ts(g1_[:], g1_[:], 7.0, None, ALU.min, r=[t_g1s[gi]], w=[t_g1s[gi]])
                    self.act(sg_[:], g1_[:], AF.Sigmoid, r=[t_g1s[gi]], w=[t_sgs[gi]], scale=1.702)
                    self.tt(l1_[:], pL[:].rearrange("p (q n) -> p q n", q=4), bl, ALU.add, r=[tpL, t_b1], w=[t_l1s[gi]])
                    self.ts(l1_[:], l1_[:], 7.0, -7.0, ALU.min, ALU.max, r=[t_l1s[gi]], w=[t_l1s[gi]])
                    self.tt(g1_[:], g1_[:], sg_[:], ALU.mult, r=[t_g1s[gi], t_sgs[gi]], w=[t_g1s[gi]])
                    self.stt(uT[bi][:, half * 4:half * 4 + 4, :], l1_[:], 1.0, g1_[:], ALU.add, ALU.mult, r=[t_l1s[gi], t_g1s[gi]], w=[t_uT[bi]])
            def stM2(seq):
                b, sid = order[seq]; bi = seq % 2
                W1, W2, b1t = W1s[sid], W2s[sid], b1ts[sid]; t_W1, t_W2, t_b1 = t_W1s[sid], t_W2s[sid], t_b1s[sid]
                y_ = yo[bi]; ty = t_yo[bi]
                for half in range(2):
                    pb, tpb = bank()
                    for m in range(8):
                        self.mm(pb[:], uT[bi][:, m, :], W2[:, m, half * 512:(half + 1) * 512], m == 0, m == 7, r=[t_uT[bi], t_W2], w=[tpb])
                    self.act(y_[:, half * 512:(half + 1) * 512], pb[:], AF.Copy, r=[tpb], w=[ty])
                self.dma(d_ys[b * 128:(b + 1) * 128, :], y_[:], r=[ty], w=[t_ys[b]])
            nseq = len(order)
            stG(0); stG(1)
            stT(0); stT(1)
            stM1(0)
            for i in range(nseq):
                if i + 2 < nseq: stT(i + 2)
                if i + 1 < nseq: stM1(i + 1)
                stM2(i)
                if i + 2 < nseq: stG(i + 2)
            self.pop()
            gT32 = self.sb("gT32", [NE, 128], BF16); t_gT32 = Trk()
            ya = self.sb("ya", [128, D]); t_ya = Trk()
            yg = [self.sb(f"yg{i}", [128, D]) for i in range(8)]; t_yg = [Trk() for _ in range(8)]
            def gath(tt_):
                for k in range(4):
                    yg_, tyg = yg[(tt_ % 2) * 4 + k], t_yg[(tt_ % 2) * 4 + k]
                    self.K.op("pool", lambda e, yg_=yg_, slotidx=slotidx, tt_=tt_, k=k: e.indirect_dma_start(
                        out=yg_[:, :], out_offset=None, in_=d_ys[:, :], in_offset=bass.IndirectOffsetOnAxis(ap=slotidx[:, tt_, k:k + 1], axis=0),
                        bounds_check=self.breg(e, NSLOT - 1), oob_is_err=False), t_ys + [t_slotidx[tt_]], [tyg], dma=True)
            gath(0)
            for tt_ in range(NT):
                bi = tt_ % 2
                pbg, tpg = bank()
                self.tr(pbg[0:NE, 0:128], gates[:, tt_, :], ident_f, r=[t_gates[tt_], t_cst], w=[tpg])
                self.act(gT32[:], pbg[0:NE, 0:128], AF.Copy, r=[tpg], w=[t_gT32])
                self.dma(xt[bi][:], xmid[tt_ * 128:(tt_ + 1) * 128, :], r=[t_xmid[tt_]], w=[t_xt[bi]])
                for half in range(2):
                    hs = slice(half * 512, (half + 1) * 512)
                    pb, tpb = bank()
                    self.mm(pb[:], gT32[0:NE, :], b2all[0:NE, hs], True, True, r=[t_gT32, t_rw], w=[tpb])
                    self.cp(ya[:, hs], pb[:], r=[tpb], w=[t_ya])
                for k in range(4):
                    yg_, tyg = yg[(tt_ % 2) * 4 + k], t_yg[(tt_ % 2) * 4 + k]
                    self.stt(ya[:], yg_[:], gk[:, tt_, k:k + 1], ya[:], ALU.mult, ALU.add, r=[tyg, t_gk[tt_], t_ya], w=[t_ya])
                if tt_ + 1 < NT: gath(tt_ + 1)
                if f"ffn{l}" in dbg:
                    if tt_ == 0: self.dbg_ffn = self.dout(f"dbg_ffn{l}", [S, D])
                    self.dma(self.dbg_ffn[tt_ * 128:(tt_ + 1) * 128, :], ya[:], r=[t_ya])
                self.tt(ya[:], ya[:], gtB[:], ALU.mult, r=[t_ya, t_gtB], w=[t_ya])
                self.stt(xt[bi][:], xt[bi][:], ALPHA, ya[:], ALU.mult, ALU.add, r=[t_xt[bi], t_ya], w=[t_xt[bi]])
                layernorm(xt[bi][:], t_xt[bi], lnB[0][:], lnB[1][:], t_lnB[0], t_lnB[1])
                if l == L - 1:
                    self.dma(y_out[tt_ * 128:(tt_ + 1) * 128, :], xt[bi][:], r=[t_xt[bi]])
                else:
                    self.dma(xres[tt_ * 128:(tt_ + 1) * 128, :], xt[bi][:], r=[t_xt[bi]], w=[t_xres[tt_]])
            self.pop()
            if "pooltest" in dbg:
                if "pt2" in dbg:
                    self.memset(cst[:, 900:1000], 3.0, w=[t_cst], eng="pool")
                else:
                    self.dma(cst[:], consts_in[:, :], w=[t_cst], q="pool")
                o = self.dout("dbg_pt", [128, 1024]); self.dma(o[:, :], cst[:], r=[t_cst])
            if stop_after == f"3{l}":
                break

        self.scr = dict(xres=xres, xmid=xmid, d_qnT=d_qnT, d_krot=d_krot, d_rw=d_rw, d_gC=d_gC, d_bonus=d_bonus, d_gT=d_gT,
                        d_ro=d_ro, vfirst=vfirst)
        for name in dbg:
            if name.startswith("scr:"):
                k_ = name[4:]; a = self.scr[k_]
                o = self.dout("dbg_" + k_, list(a.shape), a.dtype if hasattr(a, "dtype") else F32)
                self.K.barrier()
                self.K.op("sp", lambda e, o=o, a=a: e.dma_start(out=o, in_=a), (), (), dma=True)
        return self.finish()

    def finish(self):
        self.K.emit(self.nc)
        return self.nc


def _perm_w_in():
    MLA_IN = 416
    cols = -np.ones(NCH * 128, np.int64)
    cols[0:256] = np.arange(0, 256)
    cols[256:384] = np.arange(256, 384)
    r0 = MLA_IN; k0 = r0 + 512; v0 = k0 + 512; wd0 = v0 + 512; ad0 = wd0 + 64; gd0 = ad0 + 64
    kr = np.arange(384, 416)
    krs = kr.reshape(16, 2)[:, ::-1].reshape(32)
    cols[384:448] = np.arange(wd0, wd0 + 64); cols[448:480] = kr
    cols[512:576] = np.arange(ad0, ad0 + 64); cols[576:608] = krs
    cols[640:768] = np.arange(gd0, gd0 + 128)
    cols[768:1280] = np.arange(v0, v0 + 512)
    for fc in range(4):
        cols[(10 + 2 * fc) * 128:(11 + 2 * fc) * 128] = np.arange(r0 + fc * 128, r0 + (fc + 1) * 128)
        cols[(11 + 2 * fc) * 128:(12 + 2 * fc) * 128] = np.arange(k0 + fc * 128, k0 + (fc + 1) * 128)
    return cols

def _take_cols(w, cols):
    out = np.zeros(w.shape[:-1] + (len(cols),), w.dtype)
    m = cols >= 0
    out[..., m] = w[..., cols[m]]
    return out

def _kc(w):
    K, N = w.shape[-2:]
    return np.ascontiguousarray(np.moveaxis(w.reshape(w.shape[:-2] + (K // 128, 128, N)), -3, -2))

def _fm(v, n):
    return np.ascontiguousarray(v.reshape(v.shape[0], n, 128).transpose(0, 2, 1))

def _consts():
    c = np.zeros((128, 1280), np.float32)
    i = np.arange(128)
    c[:, 776] = i
    c[:, 1024:1024 + 256] = 128.0 * np.arange(256)[None, :]
    c[:, 0:128] = np.eye(128)
    c[:, 128:256] = (i[None, :] > i[:, None])
    c[:, 256:384] = (i[None, :] >= i[:, None])
    c[:, 384:512] = (i[None, :] < i[:, None])
    c[:, 512:640] = (i[None, :] // 64 == i[:, None] // 64)
    c[:, 640:768] = 1.0
    fi = np.clip((i - 64) // 2, 0, 15)
    inv_freq = (10000.0 ** (-np.arange(0, 32, 2, dtype=np.float32) / 32)).astype(np.float32)
    c[:, 768] = inv_freq[fi] / np.float32(2 * np.pi)
    c[:, 769] = np.where(i % 2 == 0, -1.0, 1.0)
    c[:, 770] = RMS_EPS; c[:, 771] = 1.0; c[:, 772] = -0.5; c[:, 773] = GN_EPS; c[:, 774] = 0.0; c[:, 775] = LN_EPS
    return c

def prep_shared(inp):
    f = lambda a: np.ascontiguousarray(a, dtype=np.float32)
    cols = _perm_w_in()
    sh = {}
    sh["consts"] = _consts()
    sh["embrow"] = f(np.stack([inp["emb_ln_g"], inp["emb_ln_b"]]))
    sh["ada_w"] = _kc(f(inp["ada_w"]))
    vecs = np.zeros((L, 128, 128), np.float32)
    vecs[:, :, 0:48] = _fm(f(inp["ada_b"]), 48)
    mu_full = np.zeros((L, 2208), np.float32); mu_full[:, 416:] = inp["rwkv_mu"]
    mu_p = _take_cols(mu_full, cols)
    mu_p[:, 0:384] = 0; mu_p[:, 448:512] = 0; mu_p[:, 576:640] = 0
    vecs[:, :, 48:66] = _fm(mu_p, NCH)
    vecs[:, :, 66:68] = _fm(f(inp["q_norm_g"]), 2)
    vecs[:, :, 68:69] = _fm(f(inp["kv_norm_g"]), 1)
    vecs[:, :, 69:73] = _fm(f(inp["mla_out_g"]), 4)
    vecs[:, :, 73:77] = _fm(f(inp["rwkv_w0"]), 4)
    vecs[:, :, 77:81] = _fm(f(inp["rwkv_a0"]), 4)
    vecs[:, :, 81:85] = _fm(f(inp["rwkv_k_k"]), 4)
    vecs[:, :, 85:89] = _fm(f(inp["rwkv_k_a"]), 4)
    vecs[:, :, 89:93] = _fm(f(inp["rwkv_r_k"]).reshape(L, 512), 4)
    vecs[:, :, 93:97] = _fm(f(inp["rwkv_gn_g"]), 4)
    vecs[:, :, 97:101] = _fm(f(inp["rwkv_gn_b"]), 4)
    vecs[1:, :, 101:105] = _fm(f(inp["vres_v0"]), 4)
    sh["vecs"] = vecs
    sh["w_in"] = _kc(_take_cols(f(inp["w_in"]), cols))
    wuq = f(inp["w_uq"])
    sw = np.arange(768).reshape(8, 96).copy()
    sw[:, 64:96] = sw[:, 64:96].reshape(8, 16, 2)[:, :, ::-1].reshape(8, 32)
    sh["w_uq"] = _kc(np.concatenate([wuq, wuq[:, :, sw.reshape(-1)]], axis=-1))
    sh["w_ukv"] = f(np.concatenate([inp["w_uk"], inp["w_uv"]], axis=-1))
    wl = np.zeros((L, 128, 1536), np.float32)
    wl[:, 0:64, 0:512] = inp["rwkv_w2"]; wl[:, 0:64, 512:1024] = inp["rwkv_a2"]; wl[:, :, 1024:1536] = inp["rwkv_g2"]
    sh["w_lora"] = wl
    sh["vres1"] = _kc(f(inp["vres_v1"][0])); sh["vres2"] = f(inp["vres_v2"][0])
    sh["w_o"] = _kc(f(inp["w_o"]))
    sh["rows"] = f(np.concatenate([inp["ln1_g"], inp["ln1_b"], inp["ln2_g"], inp["ln2_b"], inp["router_b"]], axis=-1))
    sh["router_w"] = _kc(f(inp["router_w"]))
    gl = np.concatenate([np.arange(0, 2 * D, 2), np.arange(1, 2 * D, 2)])
    sh["_gl"] = gl
    sh["b1v"] = np.ascontiguousarray(f(inp["exp_b1"])[:, :, gl].reshape(L, NE, 16, 128).transpose(0, 1, 3, 2)).reshape(L * NE * 128, 16)
    sh["exp_b2"] = f(inp["exp_b2"])
    return sh

def prep_big(inp, sh):
    gl = sh["_gl"]
    w1 = np.asarray(inp["exp_w1"], dtype=np.float32)
    sh["exp_w1"] = np.ascontiguousarray(np.moveaxis(w1[..., gl].reshape(L, NE, 8, 128, 2 * D), 2, 3)).reshape(L * NE * 128, 8 * 2 * D)
    w2 = np.asarray(inp["exp_w2"], dtype=np.float32)
    sh["exp_w2"] = np.ascontiguousarray(np.moveaxis(w2.reshape(L, NE, 8, 128, D), 2, 3)).reshape(L * NE * 128, 8 * D)

def prep_core(inp, sh, b, S, names):
    f = lambda a: np.ascontiguousarray(a, dtype=np.float32)
    d = {k: v for k, v in sh.items() if k in names}
    d["x"] = f(inp["x"][b, :S]); d["cT"] = f(inp["c"][b].reshape(8, 128).T)
    d["pos"] = np.ascontiguousarray(inp["positions"][b, :S].reshape(1, S).astype(np.int32))
    return d


def kernel(**inputs):
    S = 4096
    bld = B(S)
    nc = bld.build()
    names = set(bld.ins.keys())
    sh = prep_shared(inputs)
    if "exp_w1" in names: prep_big(inputs, sh)
    per = [prep_core(inputs, sh, b, S, names) for b in range(8)]
    res = run_bass_kernel_spmd(nc, per, core_ids=list(range(8)))
    return np.stack([r["y"] for r in res.results], axis=0)
```

```python
import numpy as np
from contextlib import ExitStack
import concourse.bass as bass
import concourse.mybir as mybir
from concourse.bass_utils import run_bass_kernel_spmd

F32 = mybir.dt.float32; BF16 = mybir.dt.bfloat16; I32 = mybir.dt.int32; U32 = mybir.dt.uint32
AF = mybir.ActivationFunctionType; ALU = mybir.AluOpType; AX = mybir.AxisListType

D = 1024; L = 2; NE = 32; TOPK = 4
ALPHA = (2 * L) ** 0.25
LN_EPS = 1e-5; RMS_EPS = 1e-6; GN_EPS = 64e-5
CAP = 1024
NCH = 18

class Trk:
    __slots__ = ("w", "r", "excl")
    def __init__(self, excl=False): self.w = None; self.r = {}; self.excl = excl

ENGS = ("pe", "act", "dve", "pool", "sp")
NDS = 8
class Sched:
    def __init__(self):
        self.ops = {e: [] for e in ENGS}
        self.cnt = {e: 0 for e in ENGS}
        self.dcnt = {e: 0 for e in ENGS}
        self.waited = {e: {} for e in ENGS}
        self.lastdma = {}
    def op(self, eng, fn, r=(), w=(), dma=False, serial=False):
        deps = []
        if serial and self.lastdma.get(eng) is not None: deps.append((self.lastdma[eng], True))
        for t in r:
            if t.w is not None: deps.append((t.w, True))
            if t.excl:
                for ev in t.r.values(): deps.append((ev, False))
        for t in w:
            if t.w is not None: deps.append((t.w, False))
            for ev in t.r.values(): deps.append((ev, False))
        waits = {}
        for (semkey, val, src, isdma), raw in deps:
            if src == eng and not isdma:
                if eng == "pe": continue
                if not raw: continue
            if self.waited[eng].get(semkey, 0) >= val: continue
            if waits.get(semkey, 0) < val: waits[semkey] = val
        for k, v in waits.items(): self.waited[eng][k] = v
        if dma:
            i = self.dcnt[eng]; self.dcnt[eng] += 1
            semkey = ("d", eng, i % NDS); val = 16 * (i // NDS + 1); inc = 16
        else:
            self.cnt[eng] += 1; semkey = ("e", eng); val = self.cnt[eng]; inc = 1
        ev = (semkey, val, eng, dma)
        if dma: self.lastdma[eng] = ev
        self.ops[eng].append((list(waits.items()), fn, semkey, inc))
        for t in r:
            old = t.r.get(semkey)
            if old is None or old[1] < val: t.r[semkey] = ev
        for t in w:
            t.w = ev; t.r = {}
    def barrier(self):
        snap = []
        for en in ENGS:
            if self.cnt[en] > 0: snap.append((("e", en), self.cnt[en]))
            n = self.dcnt[en]
            for k in range(NDS):
                c = (n - k + NDS - 1) // NDS if n > k else 0
                if c > 0: snap.append((("d", en, k), 16 * c))
        for eng in ENGS:
            waits = [(k, v) for k, v in snap if self.waited[eng].get(k, 0) < v and k != ("e", eng)]
            for k, v in waits: self.waited[eng][k] = v
            self.ops[eng].append((waits, None, None, 0))
    def emit(self, nc):
        with ExitStack() as st:
            sems = {}
            for e in ENGS:
                sems[("e", e)] = st.enter_context(nc.semaphore("se_" + e))
                for k in range(NDS):
                    sems[("d", e, k)] = st.enter_context(nc.semaphore(f"sd_{e}_{k}"))
            block = st.enter_context(nc.Block())
            def mk(ename):
                ops = self.ops[ename]
                def body(e):
                    for waits, fn, semkey, inc in ops:
                        for k, v in waits: e.wait_ge(sems[k], v)
                        if fn is not None: fn(e).then_inc(sems[semkey], inc)
                    if ename == "sp":
                        for en in ENGS:
                            n = self.dcnt[en]
                            for k in range(NDS):
                                c = (n - k + NDS - 1) // NDS if n > k else 0
                                if c > 0: e.wait_ge(sems[("d", en, k)], 16 * c)
                            if self.cnt[en] > 0 and en != ename: e.wait_ge(sems[("e", en)], self.cnt[en])
                return body
            block.tensor(mk("pe")); block.scalar(mk("act")); block.vector(mk("dve"))
            block.gpsimd(mk("pool")); block.sync(mk("sp"))

class B:
    def __init__(self, S, dbg=()):
        self.S = S; self.NT = S // 128; self.NB = S // 512
        self.nc = bass.Bass("TRN2", target_bir_lowering=False)
        self.K = Sched()
        self.stacks = [ExitStack()]
        self.dbg = dbg
        self.ins = {}; self.outs = {}
        self.rr = 0
    def din(self, name, shape, dt=F32):
        a = self.nc.dram_tensor(name, list(shape), dt, kind="ExternalInput").ap(); self.ins[name] = a; return a
    def dout(self, name, shape, dt=F32):
        a = self.nc.dram_tensor(name, list(shape), dt, kind="ExternalOutput").ap(); self.outs[name] = a; return a
    def dscr(self, name, shape, dt=F32):
        return self.nc.dram_tensor("i_" + name, list(shape), dt, kind="Internal").ap()
    def sb(self, name, shape, dt=F32):
        self.uid = getattr(self, "uid", 0) + 1
        return self.stacks[-1].enter_context(self.nc.sbuf_tensor(f"s{self.uid}_{name}", list(shape), dt))
    def ps(self, name, shape, dt=F32):
        return self.stacks[0].enter_context(self.nc.psum_tensor("p_" + name, list(shape), dt))
    def dma(self, out, in_, r=(), w=(), q="sp"):
        self.K.op(q, lambda e: e.dma_start(out=out, in_=in_), r, w, dma=True)
    def mm(self, out, lhsT, rhs, start, stop, r=(), w=()):
        self.K.op("pe", lambda e: e.matmul(out, lhsT, rhs, start=start, stop=stop), r, w)
    def tr(self, out, in_, ident, r=(), w=()):
        self.K.op("pe", lambda e: e.transpose(out, in_, ident), r, w)
    def act(self, out, in_, func, r=(), w=(), bias=None, scale=None, accum=None):
        kw = {}
        if bias is not None: kw["bias"] = bias
        if scale is not None: kw["scale"] = scale
        if accum is not None: kw["accum_out"] = accum
        self.K.op("act", lambda e: e.activation(out=out, in_=in_, func=func, **kw), r, w)
    def tt(self, out, in0, in1, op, r=(), w=(), eng="dve"):
        self.K.op(eng, lambda e: e.tensor_tensor(out, in0, in1, op), r, w)
    def ts(self, out, in0, s1, s2, op0, op1=None, r=(), w=(), eng="dve"):
        if op1 is None:
            self.K.op(eng, lambda e: e.tensor_scalar(out, in0, s1, None, op0), r, w)
        else:
            self.K.op(eng, lambda e: e.tensor_scalar(out, in0, s1, s2, op0, op1), r, w)
    def stt(self, out, in0, scalar, in1, op0, op1, r=(), w=()):
        self.K.op("dve", lambda e: e.scalar_tensor_tensor(out, in0, scalar, in1, op0, op1), r, w)
    def cp(self, out, in_, r=(), w=(), eng="dve"):
        self.K.op(eng, lambda e: e.tensor_copy(out, in_), r, w)
    def recip(self, out, in_, r=(), w=()):
        self.K.op("dve", lambda e: e.reciprocal(out, in_), r, w)
    def breg(self, e, v):
        if not hasattr(self, "_bregs"): self._bregs = {}
        if v not in self._bregs: self._bregs[v] = e.to_reg(v)
        return self._bregs[v]
    def memset(self, ap, v, w=(), eng="dve"):
        self.K.op(eng, lambda e: e.memset(ap, v), (), w)

    def push(self):
        self.stacks.append(ExitStack())
    def pop(self):
        self.K.barrier()
        self.stacks.pop().close()

    def build(self, stop_after=None, layers=L, l0=0):
        S, NT, NB = self.S, self.NT, self.NB
        nc = self.nc
        dbg = self.dbg
        x_in = self.din("x", [S, D])
        cT_in = self.din("cT", [128, 8])
        pos_in = self.din("pos", [1, S], I32)
        consts_in = self.din("consts", [128, 1280])
        embrow_in = self.din("embrow", [2, D])
        ada_w_in = self.din("ada_w", [L, 128, 8, 6 * D])
        vecs_in = self.din("vecs", [L, 128, 128])
        w_in_in = self.din("w_in", [L, 128, 8, NCH * 128])
        w_uq_in = self.din("w_uq", [L, 128, 2, 1536])
        w_ukv_in = self.din("w_ukv", [L, 128, 1024])
        w_lora_in = self.din("w_lora", [L, 128, 1536])
        vres1_in = self.din("vres1", [128, 4, 32])
        vres2_in = self.din("vres2", [32, 512])
        w_o_in = self.din("w_o", [L, 128, 8, D])
        rows_in = self.din("rows", [L, 4 * D + NE])
        router_w_in = self.din("router_w", [L, 128, 8, NE])
        b1v_in = self.din("b1v", [L * NE * 128, 16])
        exp_b2_in = self.din("exp_b2", [L, NE, D])
        y_out = self.dout("y", [S, D])
        xres = self.dscr("xres", [S, D]); t_xres = [Trk() for _ in range(NT)]
        xmid = self.dscr("xmid", [S, D]); t_xmid = [Trk() for _ in range(NT)]
        vfirst = self.dscr("vfirst", [4, 128, S]); t_vfirst = [Trk() for _ in range(NB)]
        d_qnT = self.dscr("qnT", [3, 128, S], BF16); t_dqn = [Trk() for _ in range(NB)]
        d_krot = self.dscr("krot", [128, S], BF16); t_dkrot = [Trk() for _ in range(NB)]
        d_rw = self.dscr("rw", [5, 4, 128, S], BF16); t_drw = [Trk() for _ in range(NB)]
        d_gC = self.dscr("gC", [4, 128, NT]); t_dgC = [Trk() for _ in range(NB)]
        d_bonus = self.dscr("bonus", [4, 128, S]); t_dbonus = [Trk() for _ in range(NB)]
        d_gT = self.dscr("gT", [4, 128, S], BF16); t_dgT = [Trk() for _ in range(NB)]
        d_ro = self.dscr("ro", [4, 128, S], BF16); t_dro = [Trk() for _ in range(NT)]
        self.scr_hs = self.dscr("hslots", [(4 * NT + NE) * 128, D], BF16); self.scr_ys = self.dscr("yslots", [(4 * NT + NE) * 128, D])
        d_rope = self.dscr("ropetab", [2, 32, S]); t_drope = Trk()
        cst = self.sb("cst", [128, 1280]); t_cst = Trk()
        cstb = self.sb("cstb", [128, 1024], BF16); t_cstb = Trk()
        self.dma(cst[:], consts_in[:, :], w=[t_cst])
        self.dma(cstb[:], consts_in[:, 0:1024], w=[t_cstb], q="pool")
        ident_f = cst[:, 0:128]; ident_b = cstb[:, 0:128]; ones_f = cst[:, 640:768]
        m_su_b = cstb[:, 128:256]; m_iu_b = cstb[:, 256:384]; m_sl_b = cstb[:, 384:512]
        bones_b = cstb[:, 512:640]; ones_b = cstb[:, 640:768]
        c_rmseps = cst[:, 770:771]; c_one = cst[:, 771:772]; c_mhalf = cst[:, 772:773]; c_gneps = cst[:, 773:774]
        c_zero = cst[:, 774:775]
        PS = [self.ps(f"ps{i}", [128, 512]) for i in range(8)]
        t_PS = [Trk(excl=True) for _ in range(8)]
        def bank():
            i = self.rr % 7; self.rr += 1; return PS[i], t_PS[i]
        vecs = self.sb("vecs", [128, L, 128]); t_vecs = Trk()
        for l in range(L):
            self.dma(vecs[:, l, :], vecs_in[l], w=[t_vecs])
        modT = self.sb("modT", [128, L, 48]); t_mod = Trk()
        dvec = self.sb("dvec", [128, L, 32]); t_dvec = Trk()
        stat = self.sb("stat", [128, 16]); t_stat = Trk()
        xt = [self.sb(f"xt{i}", [128, D]) for i in range(2)]; t_xt = [Trk(), Trk()]

        def layernorm(xa, t_x, gB, bB, t_g, t_b):
            self.K.op("dve", lambda e: e.bn_stats(stat[:, 0:6], xa[:, 0:512]), [t_x], [t_stat])
            self.K.op("dve", lambda e: e.bn_stats(stat[:, 6:12], xa[:, 512:1024]), [t_x], [t_stat])
            self.K.op("dve", lambda e: e.bn_aggr(stat[:, 12:14], stat[:, 0:12]), [t_stat], [t_stat])
            self.ts(stat[:, 14:15], stat[:, 13:14], LN_EPS, None, ALU.add, r=[t_stat], w=[t_stat])
            self.act(stat[:, 14:15], stat[:, 14:15], AF.Sqrt, r=[t_stat], w=[t_stat])
            self.recip(stat[:, 15:16], stat[:, 14:15], r=[t_stat], w=[t_stat])
            self.ts(xa, xa, stat[:, 12:13], stat[:, 15:16], ALU.subtract, ALU.mult, r=[t_x, t_stat], w=[t_x])
            self.tt(xa, xa, gB, ALU.mult, r=[t_x, t_g], w=[t_x])
            self.tt(xa, xa, bB, ALU.add, r=[t_x, t_b], w=[t_x])

        self.push()
        cT = self.sb("cTf", [128, 8]); t_cT = Trk()
        self.dma(cT[:], cT_in[:, :], w=[t_cT])
        cTb = self.sb("cTb", [128, 8], BF16); t_cTb = Trk()
        self.act(cTb[:], cT[:], AF.Silu, r=[t_cT], w=[t_cTb])
        adaw = [self.sb(f"adaw{i}", [128, 8, 384], BF16) for i in range(2)]; t_adaw = [Trk(), Trk()]
        for l in range(L):
            pb, tpb = bank()
            for cg in range(16):
                bi = cg % 2
                self.dma(adaw[bi][:], ada_w_in[l, :, :, cg * 384:(cg + 1) * 384], w=[t_adaw[bi]], q="pool")
                for cc in range(3):
                    col = cg * 3 + cc
                    for kc in range(8):
                        self.mm(pb[:, col:col + 1], adaw[bi][:, kc, cc * 128:(cc + 1) * 128], cTb[:, kc:kc + 1],
                                kc == 0, kc == 7, r=[t_adaw[bi], t_cTb], w=[tpb])
            self.tt(modT[:, l, :], pb[:, 0:48], vecs[:, l, 0:48], ALU.add, r=[tpb, t_vecs], w=[t_mod])
            for v in (1, 2, 4, 5):
                self.ts(modT[:, l, v * 8:(v + 1) * 8], modT[:, l, v * 8:(v + 1) * 8], 1.0, None, ALU.add, r=[t_mod], w=[t_mod])
            self.ts(dvec[:, l, 0:18], vecs[:, l, 48:66], -1.0, 1.0, ALU.mult, ALU.add, r=[t_vecs], w=[t_dvec])
            self.ts(dvec[:, l, 18:22], vecs[:, l, 73:77], -1.0, None, ALU.mult, r=[t_vecs], w=[t_dvec])
        if "mod" in dbg:
            o = self.dout("dbg_mod", [128, L, 48]); self.dma(o[:, :, :], modT[:], r=[t_mod])
        cosF = self.sb("cosF", [128, S]); sinF = self.sb("sinF", [128, S]); t_rope = Trk()
        posi = self.sb("posi", [128, 512], I32); t_posi = Trk()
        rt1 = self.sb("rt1", [128, 512]); t_rt1 = Trk()
        rt2 = self.sb("rt2", [128, 512]); t_rt2 = Trk()
        for blk in range(NB):
            cs = slice(blk * 512, (blk + 1) * 512)
            self.dma(posi[:], pos_in[:, cs].broadcast_to([128, 512]), w=[t_posi])
            self.cp(rt1[:], posi[:], r=[t_posi], w=[t_rt1])
            self.ts(rt1[:], rt1[:], cst[:, 768:769], None, ALU.mult, r=[t_rt1, t_cst], w=[t_rt1])
            self.cp(posi[:], rt1[:], r=[t_rt1], w=[t_posi])
            self.cp(rt2[:], posi[:], r=[t_posi], w=[t_rt2])
            self.tt(rt2[:], rt1[:], rt2[:], ALU.subtract, r=[t_rt1, t_rt2], w=[t_rt2])
            self.act(sinF[:, cs], rt2[:], AF.Sin, r=[t_rt2], w=[t_rope], scale=float(2 * np.pi))
            self.ts(sinF[:, cs], sinF[:, cs], cst[:, 769:770], None, ALU.mult, r=[t_rope, t_cst], w=[t_rope])
            self.ts(rt1[:], rt1[:], 0.25, None, ALU.add, r=[t_rt1], w=[t_rt1])
            self.cp(posi[:], rt1[:], r=[t_rt1], w=[t_posi])
            self.cp(rt2[:], posi[:], r=[t_posi], w=[t_rt2])
            self.tt(rt2[:], rt1[:], rt2[:], ALU.subtract, r=[t_rt1, t_rt2], w=[t_rt2])
            self.act(cosF[:, cs], rt2[:], AF.Sin, r=[t_rt2], w=[t_rope], scale=float(2 * np.pi))
        self.dma(d_rope[0], cosF[64:96, :], r=[t_rope], w=[t_drope])
        self.dma(d_rope[1], sinF[64:96, :], r=[t_rope], w=[t_drope])
        rowB = [self.sb(f"rowB{i}", [128, D]) for i in range(2)]; t_rowB = [Trk() for _ in range(2)]
        self.dma(rowB[0][:], embrow_in[0:1, :].broadcast_to([128, D]), w=[t_rowB[0]])
        self.dma(rowB[1][:], embrow_in[1:2, :].broadcast_to([128, D]), w=[t_rowB[1]])
        for tt_ in range(NT):
            bi = tt_ % 2
            self.dma(xt[bi][:], x_in[tt_ * 128:(tt_ + 1) * 128, :], w=[t_xt[bi]])
            layernorm(xt[bi][:], t_xt[bi], rowB[0][:], rowB[1][:], t_rowB[0], t_rowB[1])
            self.dma(xres[tt_ * 128:(tt_ + 1) * 128, :], xt[bi][:], r=[t_xt[bi]], w=[t_xres[tt_]])
        self.pop()

        def dbg_out(name, shape, src_ap, r, dt=F32):
            o = self.dout(name, shape, dt)
            self.dma(o, src_ap, r=r)

        for l in range(l0, layers):
            V = lambda a, b: vecs[:, l, a:b]
            self.push()
            cosB = [self.sb(f"cosB{i}", [128, 512]) for i in range(2)]; sinB = [self.sb(f"sinB{i}", [128, 512]) for i in range(2)]
            t_ropeB = [Trk(), Trk()]
            hT = self.sb("hT", [128, 8, 512], BF16); t_hT = Trk()
            xa = [xt[0], xt[1]] + [self.sb(f"xa{i}", [128, D]) for i in range(2)]; t_xa = [t_xt[0], t_xt[1], Trk(), Trk()]
            winb = self.sb("winb", [128, 8, NCH * 128], BF16); t_win = Trk()
            self.dma(winb[:], w_in_in[l], w=[t_win], q="pool")
            wlora = self.sb("wlora", [128, 1536], BF16); t_wl = Trk()
            self.dma(wlora[:], w_lora_in[l], w=[t_wl], q="pool")
            vr1 = self.sb("vr1", [128, 4, 32], BF16); vr2 = self.sb("vr2", [32, 512], BF16); t_vr = Trk()
            self.dma(vr1[:], vres1_in[:, :, :], w=[t_vr], q="pool")
            self.dma(vr2[:], vres2_in[:, :], w=[t_vr], q="pool")
            praw = self.sb("praw", [128, 15, 513]); t_praw = [Trk() for _ in range(15)]
            pq = self.sb("pq", [128, 3, 512]); t_pq = Trk()
            sqb = self.sb("sqb", [128, 3, 512], BF16); t_sqb = Trk()
            rs = self.sb("rs", [128, 2, 512]); t_rs = Trk()
            qst = self.sb("qst", [128, 3, 512], BF16); t_qst = Trk()
            krst = self.sb("krst", [128, 512], BF16); t_krst = Trk()
            E3 = self.sb("E3", [128, 512]); t_E3 = Trk()
            E4 = self.sb("E4", [128, 512]); t_E4 = Trk()
            wdT = self.sb("wdT", [128, 512], BF16); adT = self.sb("adT", [128, 512], BF16); sgT = self.sb("sgT", [128, 512], BF16)
            t_wdT = Trk(); t_adT = Trk(); t_sgT = Trk()
            Ev = self.sb("Ev", [128, 4, 512]); t_Ev = [Trk() for _ in range(4)]
            Evb = self.sb("Evb", [128, 4, 512], BF16); t_Evb = Trk()
            vv1 = self.sb("vv1", [32, 512], BF16); t_vv1 = Trk()
            vf = self.sb("vf", [128, 512]); t_vf = Trk()
            names = ["Er", "Ek", "Ea", "Eld", "Ekap", "Ekm", "Eb", "EL", "Ex1", "Ex2"]
            Ets = [{n: self.sb(f"{n}_{i}", [128, 512]) for n in names} for i in range(2)]; tEs = [{n: Trk() for n in names} for i in range(2)]
            Et, tE = Ets[0], tEs[0]
            stg = self.sb("stg", [128, 2, 6, 512], BF16); t_stg = [Trk(), Trk()]
            gcs = self.sb("gcs", [128, 4, 4]); t_gcs = [Trk() for _ in range(4)]
            for blk in range(NB):
                cs = slice(blk * 512, (blk + 1) * 512)
                cosF_, sinF_, t_rope = cosB[blk % 2], sinB[blk % 2], t_ropeB[blk % 2]
                self.dma(cosF_[64:96, :], d_rope[0][:, cs], r=[t_drope], w=[t_rope])
                self.dma(sinF_[64:96, :], d_rope[1][:, cs], r=[t_drope], w=[t_rope])
                if blk == 0:
                    for j in range(4):
                        self.dma(xa[j][:], xres[j * 128:(j + 1) * 128, :], r=[t_xres[j]], w=[t_xa[j]])
                for j in range(4):
                    tt_ = blk * 4 + j; bi = j
                    for half in range(2):
                        pb, tpb = bank()
                        for q in range(4):
                            c = half * 4 + q
                            self.tr(pb[:, q * 128:(q + 1) * 128], xa[bi][:, c * 128:(c + 1) * 128], ident_f,
                                    r=[t_xa[bi], t_cst], w=[tpb])
                        for q in range(4):
                            c = half * 4 + q
                            self.act(hT[:, c, j * 128:(j + 1) * 128], pb[:, q * 128:(q + 1) * 128], AF.Identity,
                                     r=[tpb, t_mod], w=[t_hT], scale=modT[:, l, 8 + c:9 + c], bias=modT[:, l, c:c + 1])
                if blk + 1 < NB:
                    for j in range(4):
                        tn = (blk + 1) * 4 + j
                        self.dma(xa[j][:], xres[tn * 128:(tn + 1) * 128, :], r=[t_xres[tn]], w=[t_xa[j]])
                def pchunk(c):
                    pb, tpb = bank()
                    for kc in range(8):
                        self.mm(pb[:], winb[:, kc, c * 128:(c + 1) * 128], hT[:, kc, :], kc == 0, kc == 7,
                                r=[t_win, t_hT], w=[tpb])
                    return pb, tpb
                def shift(c, out_ap, t_out):
                    pb, tpb = pchunk(c)
                    ci = c - 3
                    if blk == 0:
                        self.memset(praw[:, ci, 0:1], 0.0, w=[t_praw[ci]])
                    else:
                        self.cp(praw[:, ci, 0:1], praw[:, ci, 512:513], r=[t_praw[ci]], w=[t_praw[ci]])
                    self.act(praw[:, ci, 1:513], pb[:], AF.Copy, r=[tpb], w=[t_praw[ci]])
                    self.ts(out_ap, praw[:, ci, 1:513], dvec[:, l, c:c + 1], None, ALU.mult, r=[t_praw[ci], t_dvec], w=[t_out])
                    self.stt(out_ap, praw[:, ci, 0:512], vecs[:, l, 48 + c:49 + c], out_ap, ALU.mult, ALU.add,
                             r=[t_praw[ci], t_vecs, t_out], w=[t_out])
                for c in range(3):
                    pb, tpb = pchunk(c)
                    self.act(pq[:, c, :], pb[:], AF.Copy, r=[tpb], w=[t_pq])
                    self.act(sqb[:, c, :], pb[:], AF.Square, r=[tpb], w=[t_sqb])
                pb, tpb = bank()
                self.mm(pb[:], ones_b, sqb[:, 0, :], True, False, r=[t_cstb, t_sqb], w=[tpb])
                self.mm(pb[:], ones_b, sqb[:, 1, :], False, True, r=[t_cstb, t_sqb], w=[tpb])
                self.act(rs[:, 0, :], pb[:], AF.Sqrt, r=[tpb, t_cst], w=[t_rs], scale=1.0 / 256, bias=c_rmseps)
                pb, tpb = bank()
                self.mm(pb[:], ones_b, sqb[:, 2, :], True, True, r=[t_cstb, t_sqb], w=[tpb])
                self.act(rs[:, 1, :], pb[:], AF.Sqrt, r=[tpb, t_cst], w=[t_rs], scale=1.0 / 128, bias=c_rmseps)
                self.recip(rs[:], rs[:], r=[t_rs], w=[t_rs])
                for c in range(3):
                    self.stt(qst[:, c, :], pq[:, c, :], V(66 + c, 67 + c), rs[:, 0 if c < 2 else 1, :], ALU.mult, ALU.mult,
                             r=[t_pq, t_vecs, t_rs], w=[t_qst])
                    if c == 2: self.dma(d_qnT[:, :, cs].rearrange("c p s -> p c s"), qst[:], r=[t_qst], w=[t_dqn[blk]])
                shift(3, E3[:], t_E3)
                self.act(wdT[0:64, :], E3[0:64, :], AF.Tanh, r=[t_E3], w=[t_wdT])
                shift(4, E4[:], t_E4)
                self.act(adT[0:64, :], E4[0:64, :], AF.Copy, r=[t_E4], w=[t_adT])
                self.tt(E3[64:96, :], E3[64:96, :], cosF_[64:96, :], ALU.mult, r=[t_E3, t_rope], w=[t_E3])
                self.tt(E4[64:96, :], E4[64:96, :], sinF_[64:96, :], ALU.mult, r=[t_E4, t_rope], w=[t_E4])
                self.tt(krst[64:96, :], E3[64:96, :], E4[64:96, :], ALU.add, r=[t_E3, t_E4], w=[t_krst])
                self.dma(d_krot[64:96, cs], krst[64:96, :], r=[t_krst], w=[t_dkrot[blk]])
                shift(5, E3[:], t_E3)
                self.act(sgT[:], E3[:], AF.Sigmoid, r=[t_E3], w=[t_sgT])
                for fc in range(4):
                    shift(6 + fc, Ev[:, fc, :], t_Ev[fc])
                if l == 0 or "novres" in dbg:
                    self.dma(vfirst[:, :, cs].rearrange("f p s -> p f s"), Ev[:], r=t_Ev, w=[t_vfirst[blk]])
                else:
                    for fc in range(4):
                        self.act(Evb[:, fc, :], Ev[:, fc, :], AF.Copy, r=[t_Ev[fc]], w=[t_Evb])
                    pb, tpb = bank()
                    for fc in range(4):
                        self.mm(pb[0:32, :], vr1[:, fc, :], Evb[:, fc, :], fc == 0, fc == 3, r=[t_vr, t_Evb], w=[tpb])
                    self.act(vv1[:], pb[0:32, :], AF.Copy, r=[tpb], w=[t_vv1])
                    for fc in range(4):
                        pb, tpb = bank()
                        self.mm(pb[:], vr2[0:32, fc * 128:(fc + 1) * 128], vv1[0:32, :], True, True, r=[t_vr, t_vv1], w=[tpb])
                        x1 = Et["Ex1"]; self.act(x1[:], pb[:], AF.Sigmoid, r=[tpb, t_vecs], w=[tE["Ex1"]], bias=V(101 + fc, 102 + fc))
                        self.dma(vf[:], vfirst[fc, :, cs], r=[t_vfirst[blk]], w=[t_vf])
                        self.tt(vf[:], vf[:], Ev[:, fc, :], ALU.subtract, r=[t_vf, t_Ev[fc]], w=[t_vf])
                        self.tt(vf[:], vf[:], x1[:], ALU.mult, r=[t_vf, tE["Ex1"]], w=[t_vf])
                        self.tt(Ev[:, fc, :], Ev[:, fc, :], vf[:], ALU.add, r=[t_Ev[fc], t_vf], w=[t_Ev[fc]])
                def fcchain(fc, sb_):
                    Er, Ek, Ea, Eld, Ekap, Ekm, Eb, EL, Ex1, Ex2 = [Ets[sb_][n] for n in names]
                    tr_, tk_, ta_, tld, tkap, tkm, tb_, tL, tx1, tx2 = [tEs[sb_][n] for n in names]
                    fs = slice(fc * 128, (fc + 1) * 128)
                    sg_ = stg[:, sb_]; tsg = t_stg[sb_]
                    shift(10 + 2 * fc, Er[:], tr_)
                    shift(11 + 2 * fc, Ek[:], tk_)
                    pb, tpb = bank()
                    self.mm(pb[:], wlora[0:64, fs], wdT[0:64, :], True, True, r=[t_wl, t_wdT], w=[tpb])
                    yield
                    self.act(Ex1[:], pb[:], AF.Exp, r=[tpb, t_dvec], w=[tx1], scale=-1.0, bias=dvec[:, l, 18 + fc:19 + fc])
                    yield
                    self.act(Ex1[:], Ex1[:], AF.Ln, r=[tx1, t_cst], w=[tx1], bias=c_one)
                    yield
                    self.act(Ex1[:], Ex1[:], AF.Exp, r=[tx1, t_cst], w=[tx1], scale=-1.0, bias=c_mhalf)
                    yield
                    self.ts(Eld[:], Ex1[:], -1.0, None, ALU.mult, r=[tx1], w=[tld])
                    yield
                    pb, tpb = bank()
                    self.mm(pb[:], wlora[0:64, 512 + fc * 128:512 + (fc + 1) * 128], adT[0:64, :], True, True, r=[t_wl, t_adT], w=[tpb])
                    yield
                    self.act(Ea[:], pb[:], AF.Sigmoid, r=[tpb, t_vecs], w=[ta_], bias=V(77 + fc, 78 + fc))
                    yield
                    self.ts(Ekap[:], Ek[:], V(81 + fc, 82 + fc), None, ALU.mult, r=[tk_, t_vecs], w=[tkap])
                    yield
                    sq1 = stg[:, sb_, 5, :]
                    self.act(sq1, Ekap[:], AF.Square, r=[tkap], w=[tsg])
                    yield
                    pb, tpb = bank()
                    self.mm(pb[:], bones_b, sq1, True, True, r=[t_cstb, tsg], w=[tpb])
                    yield
                    self.act(Ex1[:], pb[:], AF.Sqrt, r=[tpb], w=[tx1])
                    yield
                    self.ts(Ex1[:], Ex1[:], 1e-12, None, ALU.max, r=[tx1], w=[tx1])
                    yield
                    self.recip(Ex1[:], Ex1[:], r=[tx1], w=[tx1])
                    yield
                    self.tt(Ekap[:], Ekap[:], Ex1[:], ALU.mult, r=[tkap, tx1], w=[tkap])
                    yield
                    self.ts(Ekm[:], Ea[:], -1.0, V(85 + fc, 86 + fc), ALU.add, ALU.mult, r=[ta_, t_vecs], w=[tkm])
                    yield
                    self.stt(Ekm[:], Ekm[:], 1.0, Ek[:], ALU.add, ALU.mult, r=[tkm, tk_], w=[tkm])
                    yield
                    self.tt(Eb[:], Ekap[:], Ea[:], ALU.mult, r=[tkap, ta_], w=[tb_])
                    yield
                    self.stt(Ex1[:], Er[:], V(89 + fc, 90 + fc), Ekm[:], ALU.mult, ALU.mult, r=[tr_, t_vecs, tkm], w=[tx1])
                    yield
                    self.act(sq1, Ex1[:], AF.Copy, r=[tx1], w=[tsg])
                    yield
                    pb, tpb = bank()
                    self.mm(pb[:], bones_b, sq1, True, True, r=[t_cstb, tsg], w=[tpb])
                    yield
                    self.tt(Ex2[:], pb[:], Ev[:, fc, :], ALU.mult, r=[tpb, t_Ev[fc]], w=[tx2])
                    yield
                    self.dma(d_bonus[fc, :, cs], Ex2[:], r=[tx2], w=[t_dbonus[blk]])
                    yield
                    for q in range(4):
                        qs = slice(q * 128, (q + 1) * 128)
                        self.K.op("dve", lambda e, qs=qs, EL=EL, Eld=Eld: e.tensor_tensor_scan(EL[:, qs], ones_f, Eld[:, qs], 0.0, ALU.mult, ALU.add),
                                  [t_cst, tld], [tL])
                    gq = gcs[:, fc, :]
                    self.act(gq, EL[:].rearrange("p (q t) -> p q t", t=128)[:, :, 127], AF.Exp, r=[tL], w=[t_gcs[fc]])
                    yield
                    self.dma(d_gC[fc, :, blk * 4:(blk + 1) * 4], gq, r=[t_gcs[fc]], w=[t_dgC[blk]])
                    yield
                    self.tt(Ex1[:], EL[:], Eld[:], ALU.subtract, r=[tL, tld], w=[tx1])
                    yield
                    self.act(Ex1[:], Ex1[:], AF.Exp, r=[tx1], w=[tx1])
                    yield
                    self.tt(sg_[:, 0, :], Ekap[:], Ex1[:], ALU.mult, r=[tkap, tx1], w=[tsg])
                    yield
                    self.act(Ex1[:], EL[:], AF.Exp, r=[tL], w=[tx1])
                    yield
                    self.tt(sg_[:, 1, :], Er[:], Ex1[:], ALU.mult, r=[tr_, tx1], w=[tsg])
                    yield
                    self.act(Ex1[:], EL[:], AF.Exp, r=[tL], w=[tx1], scale=-1.0)
                    yield
                    self.tt(sg_[:, 2, :], Ekm[:], Ex1[:], ALU.mult, r=[tkm, tx1], w=[tsg])
                    yield
                    self.tt(sg_[:, 3, :], Eb[:], Ex1[:], ALU.mult, r=[tb_, tx1], w=[tsg])
                    yield
                    self.act(sg_[:, 4, :], Ev[:, fc, :], AF.Copy, r=[t_Ev[fc]], w=[tsg])
                    yield
                    self.dma(d_rw[:, fc, :, cs].rearrange("k p s -> p k s"), sg_[:, 0:5, :], r=[tsg], w=[t_drw[blk]])
                    pb, tpb = bank()
                    self.mm(pb[:], wlora[:, 1024 + fc * 128:1024 + (fc + 1) * 128], sgT[:], True, True, r=[t_wl, t_sgT], w=[tpb])
                    yield
                    self.act(sg_[:, 5, :], pb[:], AF.Copy, r=[tpb], w=[tsg])
                    yield
                    self.dma(d_gT[fc, :, cs], sg_[:, 5, :], r=[tsg], w=[t_dgT[blk]])
                    yield

                for f0 in (0, 2):
                    gens = [fcchain(f0, 0), fcchain(f0 + 1, 1)]
                    while gens:
                        for g in list(gens):
                            try: next(g)
                            except StopIteration: gens.remove(g)
            self.pop()
            if stop_after == f"1a{l}":
                break
            self.push()
            ld5 = [self.sb(f"ld5_{i}", [64, 5, 8, 128], BF16) for i in range(3)]; t_ld5 = [Trk() for _ in range(3)]
            tm_s = [self.sb(f"tm{i}", [128, 4, 8, 64], BF16) for i in range(3)]; t_tm_s = [[Trk() for _ in range(4)] for _ in range(3)]
            gCall = self.sb("gCall", [64, 8, NT]); t_gCall = Trk()
            self.dma(gCall[:], d_gC.rearrange("f (hp j) n -> j (f hp) n", hp=2), r=t_dgC, w=[t_gCall])
            A1_s = [self.sb(f"A1{i}", [128, 8, 256], BF16) for i in range(3)]; t_A1_s = [Trk() for _ in range(3)]
            A2_s = [self.sb(f"A2{i}", [128, 8, 256], BF16) for i in range(3)]; t_A2_s = [Trk() for _ in range(3)]
            Lm_s = [self.sb(f"Lm{i}", [128, 8, 128], BF16) for i in range(3)]; t_Lm_s = [Trk() for _ in range(3)]
            Pp_s = [[self.sb(f"Pp{j}_{i}", [128, 8, 128], BF16) for i in range(2)] for j in range(3)]; t_Pp_s = [[Trk(), Trk()] for _ in range(3)]
            Qq_s = [[self.sb(f"Qq{j}_{i}", [128, 8, 128], BF16) for i in range(2)] for j in range(3)]; t_Qq_s = [[Trk(), Trk()] for _ in range(3)]
            Yf_s = [self.sb(f"Yf{i}", [128, 8, 128]) for i in range(3)]; t_Yf_s = [Trk() for _ in range(3)]
            Yb_s = [self.sb(f"Yb{i}", [128, 8, 128], BF16) for i in range(3)]; t_Yb_s = [Trk() for _ in range(3)]
            kapP_s = [self.sb(f"kapP{i}", [64, 8, 128], BF16) for i in range(3)]; t_kapP_s = [Trk() for _ in range(3)]
            LkV_s = [self.sb(f"LkV{i}", [128, 8, 64], BF16) for i in range(3)]; t_LkV_s = [Trk() for _ in range(3)]
            Uloc_s = [self.sb(f"Uloc{i}", [128, 8, 64]) for i in range(3)]; t_Uloc_s = [Trk() for _ in range(3)]
            Ub = self.sb("Ub", [128, 8, 64], BF16); t_Ub = Trk()
            Un = self.sb("Un", [128, 8, 64], BF16); t_Un = Trk()
            KV_s = [self.sb(f"KV{i}", [64, 8, 64]) for i in range(3)]; t_KV_s = [Trk() for _ in range(3)]
            Tst = self.sb("Tst", [64, 8, 64]); t_T = Trk()
            Tb = [self.sb(f"Tb{i}", [64, 8, 64], BF16) for i in range(2)]; t_Tb = [Trk(), Trk()]
            Ttmp = self.sb("Ttmp", [64, 8, 64]); t_Ttmp = Trk()
            Of = self.sb("Of", [128, 8, 64]); t_Of = Trk()
            On = self.sb("On", [128, 8, 64]); t_On = Trk()
            gst = self.sb("gst", [128, 8, 8]); t_gst = Trk()
            gmv = self.sb("gmv", [128, 8, 2]); t_gmv = Trk()
            bon_s = [self.sb(f"bon{i}", [128, 4, 128]) for i in range(3)]; t_bon_s = [Trk() for _ in range(3)]
            gTt_s = [self.sb(f"gTt{i}", [128, 4, 128], BF16) for i in range(3)]; t_gTt_s = [Trk() for _ in range(3)]
            R1 = self.sb("R1", [128, 4, 128]); t_R1 = Trk()
            rot = self.sb("rot", [128, 4, 128], BF16); t_rot = Trk()
            self.memset(Tst[:], 0.0, w=[t_T])
            self.memset(Tb[0][:], 0.0, w=[t_Tb[0]])
            mask_ui = cstb[:, 128:384].rearrange("p (o n) -> p o n", o=1)
            mask_sl = m_sl_b.rearrange("p (o n) -> p o n", o=1)
            identb3 = ident_f.rearrange("p (o n) -> p o n", o=1)
            def bfv(pb):
                return pb[:].bitcast(BF16)
            def chunk(c):
                st = c % 3
                A1 = A1_s[st]; t_A1 = t_A1_s[st]
                A2 = A2_s[st]; t_A2 = t_A2_s[st]
                Lm = Lm_s[st]; t_Lm = t_Lm_s[st]
                Yf = Yf_s[st]; t_Yf = t_Yf_s[st]
                Yb = Yb_s[st]; t_Yb = t_Yb_s[st]
                kapP = kapP_s[st]; t_kapP = t_kapP_s[st]
                LkV = LkV_s[st]; t_LkV = t_LkV_s[st]
                Uloc = Uloc_s[st]; t_Uloc = t_Uloc_s[st]
                KV = KV_s[st]; t_KV = t_KV_s[st]
                bon = bon_s[st]; t_bon = t_bon_s[st]
                gTt = gTt_s[st]; t_gTt = t_gTt_s[st]
                tm = tm_s[st]; t_tm = t_tm_s[st]
                Pp = Pp_s[st]; t_Pp = t_Pp_s[st]
                Qq = Qq_s[st]; t_Qq = t_Qq_s[st]
                blk = c // 4; cs = slice(c * 128, (c + 1) * 128)
                lb = ld5[c % 3]; tlb = t_ld5[c % 3]
                for k5 in range(5):
                    self.dma(lb[:, k5, :, :], d_rw[k5].rearrange("f (hp j) s -> j (f hp) s", hp=2)[:, :, cs],
                             r=[t_drw[blk]], w=[tlb])
                self.dma(bon[:], d_bonus.rearrange("f p s -> p f s")[:, :, cs], r=[t_dbonus[blk]], w=[t_bon])
                self.dma(gTt[:], d_gT.rearrange("f p s -> p f s")[:, :, cs], r=[t_dgT[blk]], w=[t_gTt])
                for ti, k5 in enumerate((0, 2, 3, 4)):
                    pb, tpb = bank()
                    pv = bfv(pb)
                    for h in range(8):
                        self.tr(pv[:, h * 64:(h + 1) * 64], lb[0:64, k5, h, :], ident_b[0:64, 0:64], r=[tlb, t_cstb], w=[tpb])
                    self.cp(tm[:, ti, :, :], pv[:, 0:512].rearrange("p (h i) -> p h i", h=8), r=[tpb], w=[t_tm[ti]])
                yield 0
                for hp in range(4):
                    for which, (Adst, tA, k5) in enumerate(((A1, t_A1, 2), (A2, t_A2, 3))):
                        pb, tpb = bank()
                        for hh in range(2):
                            h = 2 * hp + hh
                            self.mm(pb[:, hh * 256:(hh + 1) * 256], lb[0:64, k5, h, :], lb[0:64, 0:2, h, :], True, True, r=[tlb], w=[tpb])
                        self.tt(Adst[:, 2 * hp:2 * hp + 2, :], pb[:].rearrange("p (h n) -> p h n", h=2),
                                mask_ui.broadcast_to([128, 2, 256]), ALU.mult, r=[tpb, t_cstb], w=[tA])
                for half in range(2):
                    pb, tpb = bank()
                    for hh in range(4):
                        h = 4 * half + hh
                        self.mm(pb[:, hh * 128:(hh + 1) * 128], lb[0:64, 0, h, :], lb[0:64, 3, h, :], True, True, r=[tlb], w=[tpb])
                    self.tt(Lm[:, 4 * half:4 * half + 4, :], pb[:].rearrange("p (h n) -> p h n", h=4),
                            mask_sl.broadcast_to([128, 4, 128]), ALU.mult, r=[tpb, t_cstb], w=[t_Lm])
                yield 0
                Nm = A2[:, :, 0:128]
                self.tt(Yf[:], identb3.broadcast_to([128, 8, 128]), Nm, ALU.subtract, r=[t_cst, t_A2], w=[t_Yf])
                self.act(Yb[:], Yf[:], AF.Copy, r=[t_Yf], w=[t_Yb])
                Qc, tQc, Pc, tPc = Lm, t_Lm, Nm, t_A2
                for k in range(6):
                    yield 0
                    Qn, tQn = Qq[k % 2], t_Qq[k % 2]
                    Pn, tPn = Pp[k % 2], t_Pp[k % 2]
                    for half in range(2):
                        pb, tpb = bank()
                        for hh in range(4):
                            h = 4 * half + hh
                            self.mm(pb[:, hh * 128:(hh + 1) * 128], Pc[:, h, :], Qc[:, h, :], True, True, r=[tPc, tQc], w=[tpb])
                        self.act(Qn[:, 4 * half:4 * half + 4, :], pb[:].rearrange("p (h n) -> p h n", h=4), AF.Copy, r=[tpb], w=[tQn])
                    if k < 5:
                        for half in range(2):
                            pb, tpb = bank()
                            for hh in range(4):
                                h = 4 * half + hh
                                self.mm(pb[:, hh * 128:(hh + 1) * 128], Qc[:, h, :], Pc[:, h, :], True, True, r=[tPc, tQc], w=[tpb])
                            self.cp(Pn[:, 4 * half:4 * half + 4, :], pb[:].rearrange("p (h n) -> p h n", h=4), r=[tpb], w=[tPn])
                    yield 0
                    for half in range(2):
                        pb, tpb = bank()
                        for hh in range(4):
                            h = 4 * half + hh
                            self.mm(pb[:, hh * 128:(hh + 1) * 128], Qn[:, h, :], Yb[:, h, :], True, True, r=[tQn, t_Yb], w=[tpb])
                        self.tt(Yf[:, 4 * half:4 * half + 4, :], Yf[:, 4 * half:4 * half + 4, :],
                                pb[:].rearrange("p (h n) -> p h n", h=4), ALU.add, r=[tpb, t_Yf], w=[t_Yf])
                    self.act(Yb[:], Yf[:], AF.Copy, r=[t_Yf], w=[t_Yb])
                    Qc, tQc, Pc, tPc = Qn, tQn, Pn, tPn
                yield 0
                for half in range(2):
                    pb, tpb = bank()
                    for hh in range(4):
                        h = 4 * half + hh
                        self.mm(pb[0:64, hh * 128:(hh + 1) * 128], tm[:, 0, h, :], Yb[:, h, :], True, True, r=[t_tm[0], t_Yb], w=[tpb])
                    self.act(kapP[:, 4 * half:4 * half + 4, :], pb[0:64, :].rearrange("p (h n) -> p h n", h=4), AF.Copy, r=[tpb], w=[t_kapP])
                pb, tpb = bank()
                for h in range(8):
                    self.mm(pb[0:64, h * 64:(h + 1) * 64], tm[:, 1, h, :], tm[:, 3, h, :], True, True, r=[t_tm[1], t_tm[3]], w=[tpb])
                self.cp(KV[:], pb[0:64, :].rearrange("p (h n) -> p h n", h=8), r=[tpb], w=[t_KV])
                pb, tpb = bank()
                for h in range(8):
                    self.mm(pb[:, h * 64:(h + 1) * 64], A1[:, h, 0:128], tm[:, 3, h, :], True, True, r=[t_A1, t_tm[3]], w=[tpb])
                self.act(LkV[:], pb[:].rearrange("p (h n) -> p h n", h=8), AF.Copy, r=[tpb], w=[t_LkV])
                pb, tpb = bank()
                for h in range(8):
                    self.mm(pb[:, h * 64:(h + 1) * 64], Yb[:, h, :], LkV[:, h, :], True, True, r=[t_Yb, t_LkV], w=[tpb])
                self.cp(Uloc[:], pb[:].rearrange("p (h n) -> p h n", h=8), r=[tpb], w=[t_Uloc])
                yield 1
                Tc, tTc = Tb[c % 2], t_Tb[c % 2]
                Tn_, tTn = Tb[(c + 1) % 2], t_Tb[(c + 1) % 2]
                pb, tpb = bank()
                for h in range(8):
                    self.mm(pb[:, h * 64:(h + 1) * 64], kapP[0:64, h, :], Tc[0:64, h, :], True, True, r=[t_kapP, tTc], w=[tpb])
                self.tt(Ub[:], pb[:].rearrange("p (h n) -> p h n", h=8), Uloc[:], ALU.add, r=[tpb, t_Uloc], w=[t_Ub])
                self.ts(Un[:], Ub[:], -1.0, None, ALU.mult, r=[t_Ub], w=[t_Un], eng="pool")
                self.tt(Ttmp[:], Tst[:], KV[:], ALU.add, r=[t_T, t_KV], w=[t_Ttmp])
                pb2, tpb2 = bank()
                for h in range(8):
                    self.mm(pb2[0:64, h * 64:(h + 1) * 64], tm[:, 2, h, :], Ub[:, h, :], True, True, r=[t_tm[2], t_Ub], w=[tpb2])
                self.tt(Ttmp[:], Ttmp[:], pb2[0:64, :].rearrange("p (h n) -> p h n", h=8), ALU.subtract, r=[t_Ttmp, tpb2], w=[t_Ttmp])
                self.tt(Tst[:], Ttmp[:], gCall[:, :, c:c + 1].broadcast_to([64, 8, 64]), ALU.mult, r=[t_Ttmp, t_gCall], w=[t_T])
                self.act(Tn_[:], Tst[:], AF.Copy, r=[t_T], w=[tTn])
                pb, tpb = bank()
                for h in range(8):
                    o_ = pb[:, h * 64:(h + 1) * 64]
                    self.mm(o_, lb[0:64, 1, h, :], Tc[0:64, h, :], True, False, r=[tlb, tTc], w=[tpb])
                    self.mm(o_, A1[:, h, 128:256], tm[:, 3, h, :], False, False, r=[t_A1, t_tm[3]], w=[tpb])
                    self.mm(o_, A2[:, h, 128:256], Un[:, h, :], False, True, r=[t_A2, t_Un], w=[tpb])
                self.act(Of[:], pb[:].rearrange("p (h n) -> p h n", h=8), AF.Copy, r=[tpb], w=[t_Of])
                for h in range(8):
                    self.K.op("dve", lambda e, h=h, gst=gst, Of=Of: e.bn_stats(gst[:, h, 0:6], Of[:, h, :]), [t_Of], [t_gst])
                for h in range(8):
                    self.K.op("dve", lambda e, h=h, gst=gst, gmv=gmv: e.bn_aggr(gmv[:, h, :], gst[:, h, 0:6]), [t_gst], [t_gmv])
                self.act(gst[:, :, 6], gmv[:, :, 1], AF.Sqrt, r=[t_gmv, t_cst], w=[t_gst], bias=c_gneps)
                self.recip(gst[:, :, 7], gst[:, :, 6], r=[t_gst], w=[t_gst])
                self.tt(On[:], Of[:], gmv[:, :, 0:1].broadcast_to([128, 8, 64]), ALU.subtract, r=[t_Of, t_gmv], w=[t_On])
                self.tt(On[:], On[:], gst[:, :, 7:8].broadcast_to([128, 8, 64]), ALU.mult, r=[t_On, t_gst], w=[t_On])
                pb, tpb = bank()
                for fc in range(4):
                    self.tr(pb[:, fc * 128:(fc + 1) * 128], On[:, 2 * fc:2 * fc + 2, :].rearrange("p h i -> p (h i)"), ident_f,
                            r=[t_On, t_cst], w=[tpb])
                for fc in range(4):
                    self.act(R1[:, fc, :], pb[:, fc * 128:(fc + 1) * 128], AF.Identity, r=[tpb, t_vecs], w=[t_R1],
                             scale=V(93 + fc, 94 + fc), bias=V(97 + fc, 98 + fc))
                self.tt(R1[:], R1[:], bon[:], ALU.add, r=[t_R1, t_bon], w=[t_R1])
                self.tt(rot[:], R1[:], gTt[:], ALU.mult, r=[t_R1, t_gTt], w=[t_rot])
                self.dma(d_ro.rearrange("f p s -> p f s")[:, :, cs], rot[:], r=[t_rot], w=[t_dro[c]], q="pool")
            def drain(g):
                for _ in g: pass
            for c0 in range(0, NT, 3):
                gs = [chunk(c0 + i) for i in range(min(3, NT - c0))]
                live = list(gs)
                while live:
                    for g in list(live):
                        if next(g) == 1: live.remove(g)
                for g in gs: drain(g)
            self.pop()
            if stop_after == f"1b{l}":
                break
            self.push()
            cosB = [self.sb(f"cosB{i}", [128, 512]) for i in range(2)]; sinB = [self.sb(f"sinB{i}", [128, 512]) for i in range(2)]
            t_ropeB = [Trk(), Trk()]
            qnT = self.sb("qnT", [128, 2, S], BF16); t_qnT = Trk()
            ckvT = self.sb("ckvT", [128, S], BF16); t_ckvT = Trk()
            kTb = [self.sb(f"kTb{i}", [128, S], BF16) for i in range(2)]; t_kT = [Trk(), Trk()]
            self.dma(qnT[:], d_qnT[0:2].rearrange("c p s -> p c s"), r=t_dqn, w=[t_qnT])
            self.dma(ckvT[:], d_qnT[2], r=t_dqn, w=[t_ckvT])
            for i in range(2):
                self.dma(kTb[i][64:96, :], d_krot[64:96, :], r=t_dkrot, w=[t_kT[i]])
            wuq = self.sb("wuq", [128, 2, 1536], BF16); wukv = self.sb("wukv", [128, 1024], BF16); wo = self.sb("wo", [128, 8, D], BF16)
            t_w2 = Trk()
            self.dma(wuq[:], w_uq_in[l], w=[t_w2], q="pool")
            self.dma(wukv[:], w_ukv_in[l], w=[t_w2], q="pool")
            self.dma(wo[:], w_o_in[l], w=[t_w2], q="pool")
            Vaug = self.sb("Vaug", [128, NT, 8, 65], BF16); t_V = [Trk() for _ in range(NT)]
            self.K.op("pool", lambda e, Vaug=Vaug: e.memset(Vaug[:], 1.0), (), t_V)
            lnB = [self.sb(f"lnB{i}", [128, D]) for i in range(2)]; t_lnB = [Trk(), Trk()]
            self.dma(lnB[0][:], rows_in[l:l + 1, 0:D].broadcast_to([128, D]), w=[t_lnB[0]])
            self.dma(lnB[1][:], rows_in[l:l + 1, D:2 * D].broadcast_to([128, D]), w=[t_lnB[1]])
            gtB = self.sb("gtB", [128, D]); t_gtB = Trk()
            dg = self.sb("dg", [128, 128]); t_dg = Trk()
            for half in range(2):
                pb, tpb = bank()
                for q in range(4):
                    c = half * 4 + q
                    self.ts(dg[:], ident_f, modT[:, l, 16 + c:17 + c], None, ALU.mult, r=[t_cst, t_mod], w=[t_dg])
                    self.mm(pb[:, q * 128:(q + 1) * 128], ones_f, dg[:], True, True, r=[t_cst, t_dg], w=[tpb])
                self.cp(gtB[:, half * 512:(half + 1) * 512], pb[:], r=[tpb], w=[t_gtB])
            qTh = [self.sb(f"qTh{i}", [128, 512], BF16) for i in range(2)]; t_qTh = [Trk(), Trk()]
            rq1 = self.sb("rq1", [128, 512]); rq2 = self.sb("rq2", [128, 512]); t_rq = Trk()
            PT = [self.sb(f"PT{i}", [128, 512], BF16) for i in range(6)]; t_PT = [Trk() for _ in range(6)]
            oTs = self.sb("oTs", [128, 512]); t_oTs = Trk()
            o_tm = self.sb("o_tm", [128, 4, 512]); t_otm = Trk()
            rec = self.sb("rec", [128, 8]); t_rec = Trk()
            junk = self.sb("junk", [128, 512]); t_junk = Trk()
            mixT = self.sb("mixT", [128, 8, 512], BF16); t_mixT = Trk()
            ymix = self.sb("ymix", [128, D]); t_ymix = Trk()
            SCALE = float(96 ** -0.5)
            ptc = [0]
            ptn = 0
            for QB in range(NB):
                cs = slice(QB * 512, (QB + 1) * 512)
                cosF_, sinF_, t_rope = cosB[QB % 2], sinB[QB % 2], t_ropeB[QB % 2]
                self.dma(cosF_[64:96, :], d_rope[0][:, cs], r=[t_drope], w=[t_rope])
                self.dma(sinF_[64:96, :], d_rope[1][:, cs], r=[t_drope], w=[t_rope])
                for j in range(4):
                    tt_ = QB * 4 + j
                    pb, tpb = bank()
                    self.mm(pb[:], ckvT[:, tt_ * 128:(tt_ + 1) * 128], wukv[:, 512:1024], True, True, r=[t_ckvT, t_w2], w=[tpb])
                    self.cp(Vaug[:, tt_, :, 0:64], pb[:].rearrange("p (h i) -> p h i", h=8), r=[tpb], w=[t_V[tt_]])
                def prep(h):
                    kb_ = kTb[h % 2]; tkb = t_kT[h % 2]
                    for kb in range(QB + 1):
                        pb, tpb = bank()
                        self.mm(pb[0:64, :], wukv[:, h * 64:(h + 1) * 64], ckvT[:, kb * 512:(kb + 1) * 512], True, True, r=[t_w2, t_ckvT], w=[tpb])
                        self.act(kb_[0:64, kb * 512:(kb + 1) * 512], pb[0:64, :], AF.Copy, r=[tpb], w=[tkb])
                    qh = qTh[h % 2]; tqh = t_qTh[h % 2]
                    pbA, tpA = bank()
                    for kc in range(2):
                        self.mm(pbA[0:96, :], wuq[:, kc, h * 96:(h + 1) * 96], qnT[:, kc, cs], kc == 0, kc == 1, r=[t_w2, t_qnT], w=[tpA])
                    pbB, tpB = bank()
                    for kc in range(2):
                        self.mm(pbB[0:96, :], wuq[:, kc, 768 + h * 96:768 + (h + 1) * 96], qnT[:, kc, cs], kc == 0, kc == 1, r=[t_w2, t_qnT], w=[tpB])
                    self.act(qh[0:64, :], pbA[0:64, :], AF.Copy, r=[tpA], w=[tqh])
                    self.tt(rq1[64:96, :], pbA[64:96, :], cosF_[64:96, :], ALU.mult, r=[tpA, t_rope], w=[t_rq])
                    self.tt(rq2[64:96, :], pbB[64:96, :], sinF_[64:96, :], ALU.mult, r=[tpB, t_rope], w=[t_rq])
                    self.tt(qh[64:96, :], rq1[64:96, :], rq2[64:96, :], ALU.add, r=[t_rq], w=[tqh])
                def attn(h):
                    kb_ = kTb[h % 2]; tkb = t_kT[h % 2]
                    qh = qTh[h % 2]; tqh = t_qTh[h % 2]
                    pbO, tpO = PS[7], t_PS[7]
                    nkt = QB * 4 + 4
                    def smm(kt):
                        j = kt - QB * 4
                        n0 = 0 if j < 0 else j * 128
                        N = 512 - n0
                        pbS, tpS = bank()
                        self.mm(pbS[:, 0:N], kb_[0:96, kt * 128:(kt + 1) * 128], qh[0:96, n0:512], True, True, r=[tkb, tqh], w=[tpS])
                        return pbS, tpS, j, n0, N
                    AH = 3
                    q_ = [smm(i) for i in range(min(AH, nkt))]
                    for kt in range(nkt):
                        pbS, tpS, j, n0, N = q_.pop(0)
                        if kt + AH < nkt: q_.append(smm(kt + AH))
                        pt = PT[ptc[0] % 6]; tpt = t_PT[ptc[0] % 6]; ptc[0] += 1
                        self.act(pt[:, 0:N], pbS[:, 0:N], AF.Exp, r=[tpS], w=[tpt], scale=SCALE)
                        if j >= 0:
                            self.tt(pt[:, 0:128], pt[:, 0:128], m_iu_b, ALU.mult, r=[tpt, t_cstb], w=[tpt])
                        self.mm(pbO[0:65, n0:512], Vaug[:, kt, h, :], pt[:, 0:N], kt == 0, kt == nkt - 1, r=[t_V[kt], tpt], w=[tpO])
                    self.act(oTs[0:65, :], pbO[0:65, :], AF.Copy, r=[tpO], w=[t_oTs])
                    pbt, tpt_ = bank()
                    for j in range(4):
                        self.tr(pbt[:, j * 65:(j + 1) * 65], oTs[0:65, j * 128:(j + 1) * 128], ident_f[0:65, 0:65], r=[t_oTs, t_cst], w=[tpt_])
                    pv = pbt[:, 0:260].rearrange("p (j n) -> p j n", j=4)
                    self.recip(rec[:, 0:4], pv[:, :, 64], r=[tpt_], w=[t_rec])
                    self.tt(o_tm[:, :, h * 64:(h + 1) * 64], pv[:, :, 0:64], rec[:, 0:4].rearrange("p (j o) -> p j o", o=1).broadcast_to([128, 4, 64]),
                            ALU.mult, r=[tpt_, t_rec], w=[t_otm])
                prep(0)
                for h in range(8):
                    if h + 1 < 8: prep(h + 1)
                    attn(h)
                self.dma(mixT[:, 4:8, :], d_ro.rearrange("f p s -> p f s")[:, :, cs], r=t_dro[QB * 4:QB * 4 + 4], w=[t_mixT])
                for j in range(4):
                    self.act(junk[:], o_tm[:, j, :], AF.Square, r=[t_otm], w=[t_junk, t_rec], accum=rec[:, 4:5])
                    self.act(rec[:, 5:6], rec[:, 4:5], AF.Sqrt, r=[t_rec, t_cst], w=[t_rec], scale=1.0 / 512, bias=c_rmseps)
                    self.recip(rec[:, 6:7], rec[:, 5:6], r=[t_rec], w=[t_rec])
                    self.ts(o_tm[:, j, :], o_tm[:, j, :], rec[:, 6:7], None, ALU.mult, r=[t_otm, t_rec], w=[t_otm])
                    pb, tpb = bank()
                    for c in range(4):
                        self.tr(pb[:, c * 128:(c + 1) * 128], o_tm[:, j, c * 128:(c + 1) * 128], ident_f, r=[t_otm, t_cst], w=[tpb])
                    for c in range(4):
                        self.act(mixT[:, c, j * 128:(j + 1) * 128], pb[:, c * 128:(c + 1) * 128], AF.Copy, r=[tpb, t_vecs], w=[t_mixT],
                                 scale=V(69 + c, 70 + c))
                if f"mixT{l}" in dbg:
                    if QB == 0: self.dbg_mix = self.dout(f"dbg_mixT{l}", [128, 8, S], BF16)
                    self.dma(self.dbg_mix[:, :, cs], mixT[:], r=[t_mixT])
                for j in range(4):
                    tt_ = QB * 4 + j; bi = tt_ % 2
                    self.dma(xt[bi][:], xres[tt_ * 128:(tt_ + 1) * 128, :], r=[t_xres[tt_]], w=[t_xt[bi]])
                    for half in range(2):
                        pb, tpb = bank()
                        for c in range(8):
                            self.mm(pb[:], mixT[:, c, j * 128:(j + 1) * 128], wo[:, c, half * 512:(half + 1) * 512], c == 0, c == 7,
                                    r=[t_mixT, t_w2], w=[tpb])
                        self.tt(ymix[:, half * 512:(half + 1) * 512], pb[:], gtB[:, half * 512:(half + 1) * 512], ALU.mult, r=[tpb, t_gtB], w=[t_ymix])
                    self.stt(xt[bi][:], xt[bi][:], ALPHA, ymix[:], ALU.mult, ALU.add, r=[t_xt[bi], t_ymix], w=[t_xt[bi]])
                    layernorm(xt[bi][:], t_xt[bi], lnB[0][:], lnB[1][:], t_lnB[0], t_lnB[1])
                    self.dma(xmid[tt_ * 128:(tt_ + 1) * 128, :], xt[bi][:], r=[t_xt[bi]], w=[t_xmid[tt_]], q="pool")
            self.pop()
            if "pt_at2" in dbg:
                tst = self.sb("tst", [128, 64]); t_tst = Trk()
                self.memset(tst[:], 3.0, w=[t_tst], eng="pool")
                o = self.dout("dbg_pt2", [128, 64]); self.dma(o[:, :], tst[:], r=[t_tst])
            if stop_after == f"2{l}":
                break
            self.push()
            NBLK = 4 * NT + NE; NSLOT = NBLK * 128; BIG = 1000000.0
            w1rows = self.ins.get("exp_w1"); w2rows = self.ins.get("exp_w2")
            if w1rows is None: w1rows = self.din("exp_w1", [L * NE * 128, 8 * 2 * D])
            if w2rows is None: w2rows = self.din("exp_w2", [L * NE * 128, 8 * D])
            b1rows = b1v_in
            d_hs = self.scr_hs; d_ys = self.scr_ys
            t_hs = [Trk() for _ in range(NBLK)]; t_ys = [Trk() for _ in range(NBLK)]
            lnB = [self.sb(f"ln2B{i}", [128, D]) for i in range(2)]; t_lnB = [Trk(), Trk()]
            self.dma(lnB[0][:], rows_in[l:l + 1, 2 * D:3 * D].broadcast_to([128, D]), w=[t_lnB[0]])
            self.dma(lnB[1][:], rows_in[l:l + 1, 3 * D:4 * D].broadcast_to([128, D]), w=[t_lnB[1]])
            b2all = self.sb("b2all", [NE, D], BF16); t_rw = Trk()
            self.dma(b2all[:], exp_b2_in[l], w=[t_rw], q="pool")
            gates = self.sb("gates", [128, NT, NE]); t_gates = [Trk() for _ in range(NT)]
            gk = self.sb("gk", [128, NT, 4]); t_gk = [Trk() for _ in range(NT)]
            slotidx = self.sb("slotidx", [128, NT, 4], I32); t_slotidx = [Trk() for _ in range(NT)]
            widx = self.sb("widx", [128, NBLK], I32); t_widx = Trk()
            gtB = self.sb("gt2B", [128, D]); t_gtB = Trk()
            self.push()
            rbB = self.sb("rbB", [128, NE]); t_rbB = Trk()
            self.dma(rbB[:], rows_in[l:l + 1, 4 * D:4 * D + NE].broadcast_to([128, NE]), w=[t_rbB])
            rwf = self.sb("rwf", [128, 8, NE]); h2f_s = [self.sb(f"h2f{i}", [128, 8, 128]) for i in range(4)]; t_h2f_s = [Trk() for _ in range(4)]
            xr_s = [self.sb(f"xr{i}", [128, D]) for i in range(4)]; t_xr_s = [Trk() for _ in range(4)]
            self.dma(rwf[:], router_w_in[l], w=[t_rw])
            dg = self.sb("dg2", [128, 128]); t_dg = Trk()
            def bcast_tile(name, col0, tl=None, ttr=None):
                if tl is None:
                    tl = self.sb(name, [128, D]); ttr = Trk()
                for half in range(2):
                    pb, tpb = bank()
                    for q in range(4):
                        c = half * 4 + q
                        self.ts(dg[:], ident_f, modT[:, l, col0 + c:col0 + c + 1], None, ALU.mult, r=[t_cst, t_mod], w=[t_dg])
                        self.mm(pb[:, q * 128:(q + 1) * 128], ones_f, dg[:], True, True, r=[t_cst, t_dg], w=[tpb])
                    self.cp(tl[:, half * 512:(half + 1) * 512], pb[:], r=[tpb], w=[ttr])
                return tl, ttr
            bcast_tile("gt2B", 40, gtB, t_gtB)
            scB, t_scB = bcast_tile("sc2B", 32)
            shB, t_shB = bcast_tile("sh2B", 24)
            lgs = self.sb("lgs", [128, NT, NE]); t_lgs = [Trk() for _ in range(NT)]
            m8s = self.sb("m8s", [128, NT, 8]); t_m8s = [Trk() for _ in range(NT)]
            maskb = self.sb("maskb", [128, NT, NE], BF16); t_maskb = [Trk() for _ in range(NT)]
            lg_s = [self.sb(f"lg{i}", [128, NE]) for i in range(4)]; m8_s = [self.sb(f"m8{i}", [128, 16]) for i in range(4)]; t_lg_s = [Trk() for _ in range(4)]; t_m8_s = [Trk() for _ in range(4)]
            msk_s = [self.sb(f"msk{i}", [128, NE]) for i in range(4)]; t_msk_s = [Trk() for _ in range(4)]
            def route_tile(tt_):
                st = tt_ % 4
                h2f, t_h2f, lg, t_lg, m8, t_m8, msk, t_msk = h2f_s[st], t_h2f_s[st], lg_s[st], t_lg_s[st], m8_s[st], t_m8_s[st], msk_s[st], t_msk_s[st]
                xq, t_xq = xr_s[st], t_xr_s[st]
                self.dma(xq[:], xmid[tt_ * 128:(tt_ + 1) * 128, :], r=[t_xmid[tt_]], w=[t_xq])
                yield
                for half in range(2):
                    pb, tpb = bank()
                    for q in range(4):
                        c = half * 4 + q
                        self.tr(pb[:, q * 128:(q + 1) * 128], xq[:, c * 128:(c + 1) * 128], ident_f, r=[t_xq, t_cst], w=[tpb])
                    for q in range(4):
                        c = half * 4 + q
                        self.ts(h2f[:, c, :], pb[:, q * 128:(q + 1) * 128], modT[:, l, 32 + c:33 + c], modT[:, l, 24 + c:25 + c], ALU.mult, ALU.add,
                                r=[tpb, t_mod], w=[t_h2f])
                pb, tpb = bank()
                for kc in range(8):
                    self.mm(pb[:, 0:NE], h2f[:, kc, :], rwf[:, kc, :], kc == 0, kc == 7, r=[t_h2f, t_rw], w=[tpb])
                self.tt(lgs[:, tt_, :], pb[:, 0:NE], rbB[:], ALU.add, r=[tpb, t_rbB], w=[t_lgs[tt_]])
                yield
                self.K.op("dve", lambda e, m8s=m8s, lgs=lgs, tt_=tt_: e.max(out=m8s[:, tt_, :], in_=lgs[:, tt_, :]), [t_lgs[tt_]], [t_m8s[tt_]])
                yield
                self.ts(msk[:], lgs[:, tt_, :], m8s[:, tt_, 3:4], None, ALU.is_ge, r=[t_lgs[tt_], t_m8s[tt_]], w=[t_msk])
                yield
                self.cp(maskb[:, tt_, :], msk[:], r=[t_msk], w=[t_maskb[tt_]])
                yield
                self.ts(m8[:, 8:9], m8s[:, tt_, 0:1], -1.0, None, ALU.mult, r=[t_m8s[tt_]], w=[t_m8])
                yield
                self.act(lg[:], lgs[:, tt_, :], AF.Exp, r=[t_lgs[tt_], t_m8], w=[t_lg], bias=m8[:, 8:9])
                yield
                self.tt(lg[:], lg[:], msk[:], ALU.mult, r=[t_lg, t_msk], w=[t_lg])
                yield
                self.K.op("dve", lambda e, m8=m8, lg=lg: e.reduce_sum(m8[:, 9:10], lg[:], AX.X), [t_lg], [t_m8])
                yield
                self.recip(m8[:, 10:11], m8[:, 9:10], r=[t_m8], w=[t_m8])
                yield
                self.ts(gates[:, tt_, :], lg[:], m8[:, 10:11], None, ALU.mult, r=[t_lg, t_m8], w=[t_gates[tt_]])
                yield
            def rr(gens):
                gens = list(gens)
                while gens:
                    for g in list(gens):
                        try: next(g)
                        except StopIteration: gens.remove(g)
            for t0 in range(0, NT, 4):
                rr([route_tile(t0 + i) for i in range(min(4, NT - t0))])
            if f"gates{l}" in dbg:
                dbg_out(f"dbg_gates{l}", [128, NT, NE], gates[:], t_gates)
            rt = self.sb("rt", [128, 8, NE]); t_rt = Trk()
            rti = self.sb("rti", [128, NE], I32); t_rti = Trk()
            pb, tpb = bank()
            for tt_ in range(NT):
                self.mm(pb[:, 0:NE], ones_b, maskb[:, tt_, :], tt_ == 0, tt_ == NT - 1, r=[t_cstb, t_maskb[tt_]], w=[tpb])
            self.ts(rt[:, 1, :], pb[:, 0:NE], 127.0, 1.0 / 128, ALU.add, ALU.mult, r=[tpb], w=[t_rt])
            self.ts(rt[:, 1, :], rt[:, 1, :], -0.49609375, None, ALU.add, r=[t_rt], w=[t_rt])
            self.cp(rti[:], rt[:, 1, :], r=[t_rt], w=[t_rti])
            self.cp(rt[:, 2, :], rti[:], r=[t_rti], w=[t_rt])
            self.ts(rt[:, 3, :], rt[:, 2, :], 128.0, None, ALU.mult, r=[t_rt], w=[t_rt])
            self.K.op("dve", lambda e, rt=rt: e.tensor_tensor_scan(rt[:, 4, :], ones_f[:, 0:NE], rt[:, 3, :], 0.0, ALU.mult, ALU.add), [t_rt, t_cst], [t_rt])
            self.tt(rt[:, 5, :], rt[:, 4, :], rt[:, 3, :], ALU.subtract, r=[t_rt], w=[t_rt])
            bx = self.sb("bx", [128, 6, NBLK]); t_bx = Trk()
            pcol = self.sb("pcol", [128, 1]); t_pcol = Trk()
            b128 = cst[:, 1024:1024 + NBLK]
            self.memset(bx[:, 0, :], 0.0, w=[t_bx])
            for e_ in range(NE):
                self.stt(bx[:, 0, :], b128, rt[:, 4, e_:e_ + 1], bx[:, 0, :], ALU.is_ge, ALU.add, r=[t_cst, t_rt, t_bx], w=[t_bx])
            self.ts(bx[:, 1, :], bx[:, 0, :], 31.5, None, ALU.is_lt, r=[t_bx], w=[t_bx])
            self.memset(bx[:, 2, 0:1], 1.0, w=[t_bx])
            self.tt(bx[:, 2, 1:NBLK], bx[:, 0, 1:NBLK], bx[:, 0, 0:NBLK - 1], ALU.not_equal, r=[t_bx], w=[t_bx])
            self.memset(bx[:, 2, NBLK // 2:NBLK // 2 + 1], 1.0, w=[t_bx])
            self.tt(bx[:, 2, :], bx[:, 2, :], bx[:, 1, :], ALU.mult, r=[t_bx], w=[t_bx])
            self.ts(pcol[:], cst[:, 776:777], float(l * NE * 128) - BIG, None, ALU.add, r=[t_cst], w=[t_pcol])
            self.ts(bx[:, 3, :], bx[:, 0, :], 128.0, pcol[:, 0:1], ALU.mult, ALU.add, r=[t_bx, t_pcol], w=[t_bx])
            self.tt(bx[:, 3, :], bx[:, 3, :], bx[:, 2, :], ALU.mult, r=[t_bx], w=[t_bx])
            self.ts(bx[:, 3, :], bx[:, 3, :], BIG, None, ALU.add, r=[t_bx], w=[t_bx])
            self.cp(widx[:], bx[:, 3, :], r=[t_bx], w=[t_widx])
            if f"route{l}" in dbg:
                dbg_out(f"dbg_rt{l}", [128, 8, NE], rt[:], [t_rt]); dbg_out(f"dbg_bx{l}", [128, 6, NBLK], bx[:], [t_bx])
                dbg_out(f"dbg_widx{l}", [128, NBLK], widx[:], [t_widx], I32)
            slotf_s = [self.sb(f"slotf{i}", [128, NE]) for i in range(2)]; t_slotf_s = [Trk(), Trk()]
            oh_s = [self.sb(f"oh{i}", [128, NE]) for i in range(2)]; t_oh_s = [Trk(), Trk()]
            tmp32_s = [self.sb(f"tmp32{i}", [128, NE]) for i in range(2)]; t_tmp32_s = [Trk(), Trk()]
            sk_s = [self.sb(f"sk{i}", [128, 4]) for i in range(2)]; t_sk_s = [Trk(), Trk()]
            h2a_s = [self.sb(f"h2a{i}", [128, D]) for i in range(2)]; t_h2a_s = [Trk(), Trk()]
            h2tm = [self.sb(f"h2tm{i}", [128, D], BF16) for i in range(2)]; t_h2tm = [Trk(), Trk()]
            def slot_tile(tt_):
                bi = tt_ % 2
                slotf, t_slotf, oh, t_oh, tmp32, t_tmp32, sk, t_sk, h2a, t_h2a = [x[bi] for x in (slotf_s, t_slotf_s, oh_s, t_oh_s, tmp32_s, t_tmp32_s, sk_s, t_sk_s, h2a_s, t_h2a_s)]
                xq, t_xq = xr_s[bi], t_xr_s[bi]
                pb, tpb = bank()
                for u in range(tt_):
                    self.mm(pb[:, 0:NE], ones_b, maskb[:, u, :], u == 0, False, r=[t_cstb, t_maskb[u]], w=[tpb])
                    yield
                self.mm(pb[:, 0:NE], m_su_b, maskb[:, tt_, :], tt_ == 0, True, r=[t_cstb, t_maskb[tt_]], w=[tpb])
                yield
                self.tt(slotf[:], pb[:, 0:NE], rt[:, 5, :], ALU.add, r=[tpb, t_rt], w=[t_slotf])
                yield
                for k in range(4):
                    self.ts(oh[:], lgs[:, tt_, :], m8s[:, tt_, k:k + 1], None, ALU.is_equal, r=[t_lgs[tt_], t_m8s[tt_]], w=[t_oh])
                    yield
                    self.tt(tmp32[:], oh[:], slotf[:], ALU.mult, r=[t_oh, t_slotf], w=[t_tmp32])
                    yield
                    self.K.op("dve", lambda e, sk=sk, tmp32=tmp32, k=k: e.reduce_sum(sk[:, k:k + 1], tmp32[:], AX.X), [t_tmp32], [t_sk])
                    self.tt(tmp32[:], oh[:], gates[:, tt_, :], ALU.mult, r=[t_oh, t_gates[tt_]], w=[t_tmp32])
                    yield
                    self.K.op("dve", lambda e, gk=gk, tmp32=tmp32, k=k, tt_=tt_: e.reduce_sum(gk[:, tt_, k:k + 1], tmp32[:], AX.X), [t_tmp32], [t_gk[tt_]])
                self.cp(slotidx[:, tt_, :], sk[:], r=[t_sk], w=[t_slotidx[tt_]])
                yield
                self.dma(xq[:], xmid[tt_ * 128:(tt_ + 1) * 128, :], r=[t_xmid[tt_]], w=[t_xq])
                yield
                self.tt(h2a[:], xq[:], scB[:], ALU.mult, r=[t_xq, t_scB], w=[t_h2a])
                yield
                hb = h2tm[bi]; thb = t_h2tm[bi]
                self.tt(hb[:], h2a[:], shB[:], ALU.add, r=[t_h2a, t_shB], w=[thb])
                yield
                for k in range(4):
                    self.K.op("pool", lambda e, hb=hb, slotidx=slotidx, tt_=tt_, k=k: e.indirect_dma_start(
                        out=d_hs[:, :], out_offset=bass.IndirectOffsetOnAxis(ap=slotidx[:, tt_, k:k + 1], axis=0), in_=hb[:, :], in_offset=None,
                        bounds_check=self.breg(e, NSLOT - 1), oob_is_err=False), [thb, t_slotidx[tt_]], t_hs, dma=True)
            for t0 in range(0, NT, 2):
                rr([slot_tile(t0 + i) for i in range(min(2, NT - t0))])
            if f"route{l}" in dbg:
                dbg_out(f"dbg_slotidx{l}", [128, NT, 4], slotidx[:], t_slotidx, I32); dbg_out(f"dbg_gk{l}", [128, NT, 4], gk[:], t_gk)
            self.pop()
            self.push()
            W1s = [self.sb(f"W1_{i}", [128, 8, 2 * D], BF16) for i in range(2)]; t_W1s = [Trk(), Trk()]
            W2s = [self.sb(f"W2_{i}", [128, 8, D], BF16) for i in range(2)]; t_W2s = [Trk(), Trk()]
            b1ts = [self.sb(f"b1t{i}", [128, 16]) for i in range(2)]; t_b1s = [Trk(), Trk()]
            xs = [self.sb(f"xs{i}", [128, D], BF16) for i in range(2)]; t_xs = [Trk(), Trk()]
            xsT = [self.sb(f"xsT{i}", [128, 8, 128], BF16) for i in range(2)]; t_xsT = [Trk(), Trk()]
            uT = [self.sb(f"uT{i}", [128, 8, 128], BF16) for i in range(2)]; t_uT = [Trk(), Trk()]
            g1s = [self.sb(f"g1_{i}", [128, 4, 128]) for i in range(2)]; sgs = [self.sb(f"sg_{i}", [128, 4, 128]) for i in range(2)]
            l1s = [self.sb(f"l1_{i}", [128, 4, 128]) for i in range(2)]
            t_g1s = [Trk(), Trk()]; t_sgs = [Trk(), Trk()]; t_l1s = [Trk(), Trk()]
            yo = [self.sb(f"yo{i}", [128, D]) for i in range(2)]; t_yo = [Trk(), Trk()]
            order = []
            for i in range(NBLK // 2):
                order += [(i, 0), (NBLK // 2 + i, 1)]
            hnc = [0]
            def stG(seq):
                b, sid = order[seq]; bi = seq % 2
                W1, W2, b1t = W1s[sid], W2s[sid], b1ts[sid]; t_W1, t_W2, t_b1 = t_W1s[sid], t_W2s[sid], t_b1s[sid]
                self.K.op("pool", lambda e, W1=W1, widx=widx, b=b: e.indirect_dma_start(
                    out=W1[:].rearrange("p k n -> p (k n)"), out_offset=None, in_=w1rows[:, :],
                    in_offset=bass.IndirectOffsetOnAxis(ap=widx[:, b:b + 1], axis=0), bounds_check=self.breg(e, L * NE * 128 - 1), oob_is_err=False),
                    [t_widx], [t_W1], dma=True)
                self.K.op("pool", lambda e, W2=W2, widx=widx, b=b: e.indirect_dma_start(
                    out=W2[:].rearrange("p k n -> p (k n)"), out_offset=None, in_=w2rows[:, :],
                    in_offset=bass.IndirectOffsetOnAxis(ap=widx[:, b:b + 1], axis=0), bounds_check=self.breg(e, L * NE * 128 - 1), oob_is_err=False),
                    [t_widx], [t_W2], dma=True)
                self.K.op("pool", lambda e, b1t=b1t, widx=widx, b=b: e.indirect_dma_start(
                    out=b1t[:, :], out_offset=None, in_=b1rows[:, :],
                    in_offset=bass.IndirectOffsetOnAxis(ap=widx[:, b:b + 1], axis=0), bounds_check=self.breg(e, L * NE * 128 - 1), oob_is_err=False),
                    [t_widx], [t_b1], dma=True)
            def stT(seq):
                b, sid = order[seq]; bi = seq % 2
                W1, W2, b1t = W1s[sid], W2s[sid], b1ts[sid]; t_W1, t_W2, t_b1 = t_W1s[sid], t_W2s[sid], t_b1s[sid]
                self.dma(xs[bi][:], d_hs[b * 128:(b + 1) * 128, :], r=[t_hs[b]], w=[t_xs[bi]])
                pb, tpb = bank()
                pv = pb[:].bitcast(BF16)
                for c in range(8):
                    self.tr(pv[:, c * 128:(c + 1) * 128], xs[bi][:, c * 128:(c + 1) * 128], ident_b, r=[t_xs[bi], t_cstb], w=[tpb])
                self.act(xsT[bi][:], pv[:, 0:1024].rearrange("p (c n) -> p c n", c=8), AF.Copy, r=[tpb], w=[t_xsT[bi]])
            def stM1(seq):
                b, sid = order[seq]; bi = seq % 2
                W1, W2, b1t = W1s[sid], W2s[sid], b1ts[sid]; t_W1, t_W2, t_b1 = t_W1s[sid], t_W2s[sid], t_b1s[sid]
                for half in range(2):
                    gi = hnc[0] % 2; hnc[0] += 1
                    pG, tpG = bank(); pL, tpL = bank()
                    for q in range(4):
                        m = half * 4 + q
                        for kc in range(8):
                            self.mm(pG[:, q * 128:(q + 1) * 128], W1[:, kc, m * 128:(m + 1) * 128], xsT[bi][:, kc, :], kc == 0, kc == 7, r=[t_W1, t_xsT[bi]], w=[tpG])
                    for q in range(4):
                        m = half * 4 + q
                        for kc in range(8):
                            self.mm(pL[:, q * 128:(q + 1) * 128], W1[:, kc, D + m * 128:D + (m + 1) * 128], xsT[bi][:, kc, :], kc == 0, kc == 7, r=[t_W1, t_xsT[bi]], w=[tpL])
                    g1_, sg_, l1_ = g1s[gi], sgs[gi], l1s[gi]
                    bg = b1t[:, half * 4:half * 4 + 4].rearrange("p (q o) -> p q o", o=1).broadcast_to([128, 4, 128])
                    bl = b1t[:, 8 + half * 4:8 + half * 4 + 4].rearrange("p (q o) -> p q o", o=1).broadcast_to([128, 4, 128])
                    self.tt(g1_[:], pG[:].rearrange("p (q n) -> p q n", q=4), bg, ALU.add, r=[tpG, t_b1], w=[t_g1s[gi]])
                    self.ts(g1_[:], g1_[:], 7.0, None, ALU.min, r=[t_g1s[gi]], w=[t_g1s[gi]])
                    self.act(sg_[:], g1_[:], AF.Sigmoid, r=[t_g1s[gi]], w=[t_sgs[gi]], scale=1.702)
                    self.tt(l1_[:], pL[:].rearrange("p (q n) -> p q n", q=4), bl, ALU.add, r=[tpL, t_b1], w=[t_l1s[gi]])
                    self.ts(l1_[:], l1_[:], 7.0, -7.0, ALU.min, ALU.max, r=[t_l1s[gi]], w=[t_l1s[gi]])
                    self.tt(g1_[:], g1_[:], sg_[:], ALU.mult, r=[t_g1s[gi], t_sgs[gi]], w=[t_g1s[gi]])
                    self.stt(uT[bi][:, half * 4:half * 4 + 4, :], l1_[:], 1.0, g1_[:], ALU.add, ALU.mult, r=[t_l1s[gi], t_g1s[gi]], w=[t_uT[bi]])
            def stM2(seq):
                b, sid = order[seq]; bi = seq % 2
                W1, W2, b1t = W1s[sid], W2s[sid], b1ts[sid]; t_W1, t_W2, t_b1 = t_W1s[sid], t_W2s[sid], t_b1s[sid]
                y_ = yo[bi]; ty = t_yo[bi]
                for half in range(2):
                    pb, tpb = bank()
                    for m in range(8):
                        self.mm(pb[:], uT[bi][:, m, :], W2[:, m, half * 512:(half + 1) * 512], m == 0, m == 7, r=[t_uT[bi], t_W2], w=[tpb])
                    self.act(y_[:, half * 512:(half + 1) * 512], pb[:], AF.Copy, r=[tpb], w=[ty])
                self.dma(d_ys[b * 128:(b + 1) * 128, :], y_[:], r=[ty], w=[t_ys[b]])
            nseq = len(order)
            stG(0); stG(1)
            stT(0); stT(1)
            stM1(0)
            for i in range(nseq):
                if i + 2 < nseq: stT(i + 2)
                if i + 1 < nseq: stM1(i + 1)
                stM2(i)
                if i + 2 < nseq: stG(i + 2)
            self.pop()
            gT32 = self.sb("gT32", [NE, 128], BF16); t_gT32 = Trk()
            ya = self.sb("ya", [128, D]); t_ya = Trk()
            yg = [self.sb(f"yg{i}", [128, D]) for i in range(8)]; t_yg = [Trk() for _ in range(8)]
            def gath(tt_):
                for k in range(4):
                    yg_, tyg = yg[(tt_ % 2) * 4 + k], t_yg[(tt_ % 2) * 4 + k]
                    self.K.op("pool", lambda e, yg_=yg_, slotidx=slotidx, tt_=tt_, k=k: e.indirect_dma_start(
                        out=yg_[:, :], out_offset=None, in_=d_ys[:, :], in_offset=bass.IndirectOffsetOnAxis(ap=slotidx[:, tt_, k:k + 1], axis=0),
                        bounds_check=self.breg(e, NSLOT - 1), oob_is_err=False), t_ys + [t_slotidx[tt_]], [tyg], dma=True)
            gath(0)
            for tt_ in range(NT):
                bi = tt_ % 2
                pbg, tpg = bank()
                self.tr(pbg[0:NE, 0:128], gates[:, tt_, :], ident_f, r=[t_gates[tt_], t_cst], w=[tpg])
                self.act(gT32[:], pbg[0:NE, 0:128], AF.Copy, r=[tpg], w=[t_gT32])
                self.dma(xt[bi][:], xmid[tt_ * 128:(tt_ + 1) * 128, :], r=[t_xmid[tt_]], w=[t_xt[bi]])
                for half in range(2):
                    hs = slice(half * 512, (half + 1) * 512)
                    pb, tpb = bank()
                    self.mm(pb[:], gT32[0:NE, :], b2all[0:NE, hs], True, True, r=[t_gT32, t_rw], w=[tpb])
                    self.cp(ya[:, hs], pb[:], r=[tpb], w=[t_ya])
                for k in range(4):
                    yg_, tyg = yg[(tt_ % 2) * 4 + k], t_yg[(tt_ % 2) * 4 + k]
                    self.stt(ya[:], yg_[:], gk[:, tt_, k:k + 1], ya[:], ALU.mult, ALU.add, r=[tyg, t_gk[tt_], t_ya], w=[t_ya])
                if tt_ + 1 < NT: gath(tt_ + 1)
                if f"ffn{l}" in dbg:
                    if tt_ == 0: self.dbg_ffn = self.dout(f"dbg_ffn{l}", [S, D])
                    self.dma(self.dbg_ffn[tt_ * 128:(tt_ + 1) * 128, :], ya[:], r=[t_ya])
                self.tt(ya[:], ya[:], gtB[:], ALU.mult, r=[t_ya, t_gtB], w=[t_ya])
                self.stt(xt[bi][:], xt[bi][:], ALPHA, ya[:], ALU.mult, ALU.add, r=[t_xt[bi], t_ya], w=[t_xt[bi]])
                layernorm(xt[bi][:], t_xt[bi], lnB[0][:], lnB[1][:], t_lnB[0], t_lnB[1])
                if l == L - 1:
                    self.dma(y_out[tt_ * 128:(tt_ + 1) * 128, :], xt[bi][:], r=[t_xt[bi]])
                else:
                    self.dma(xres[tt_ * 128:(tt_ + 1) * 128, :], xt[bi][:], r=[t_xt[bi]], w=[t_xres[tt_]])
            self.pop()
            if "pooltest" in dbg:
                if "pt2" in dbg:
                    self.memset(cst[:, 900:1000], 3.0, w=[t_cst], eng="pool")
                else:
                    self.dma(cst[:], consts_in[:, :], w=[t_cst], q="pool")
                o = self.dout("dbg_pt", [128, 1024]); self.dma(o[:, :], cst[:], r=[t_cst])
            if stop_after == f"3{l}":
                break

        self.scr = dict(xres=xres, xmid=xmid, d_qnT=d_qnT, d_krot=d_krot, d_rw=d_rw, d_gC=d_gC, d_bonus=d_bonus, d_gT=d_gT,
                        d_ro=d_ro, vfirst=vfirst)
        for name in dbg:
            if name.startswith("scr:"):
                k_ = name[4:]; a = self.scr[k_]
                o = self.dout("dbg_" + k_, list(a.shape), a.dtype if hasattr(a, "dtype") else F32)
                self.K.barrier()
                self.K.op("sp", lambda e, o=o, a=a: e.dma_start(out=o, in_=a), (), (), dma=True)
        return self.finish()

    def finish(self):
        self.K.emit(self.nc)
        return self.nc


def _perm_w_in():
    MLA_IN = 416
    cols = -np.ones(NCH * 128, np.int64)
    cols[0:256] = np.arange(0, 256)
    cols[256:384] = np.arange(256, 384)
    r0 = MLA_IN; k0 = r0 + 512; v0 = k0 + 512; wd0 = v0 + 512; ad0 = wd0 + 64; gd0 = ad0 + 64
    kr = np.arange(384, 416)
    krs = kr.reshape(16, 2)[:, ::-1].reshape(32)
    cols[384:448] = np.arange(wd0, wd0 + 64); cols[448:480] = kr
    cols[512:576] = np.arange(ad0, ad0 + 64); cols[576:608] = krs
    cols[640:768] = np.arange(gd0, gd0 + 128)
    cols[768:1280] = np.arange(v0, v0 + 512)
    for fc in range(4):
        cols[(10 + 2 * fc) * 128:(11 + 2 * fc) * 128] = np.arange(r0 + fc * 128, r0 + (fc + 1) * 128)
        cols[(11 + 2 * fc) * 128:(12 + 2 * fc) * 128] = np.arange(k0 + fc * 128, k0 + (fc + 1) * 128)
    return cols

def _take_cols(w, cols):
    out = np.zeros(w.shape[:-1] + (len(cols),), w.dtype)
    m = cols >= 0
    out[..., m] = w[..., cols[m]]
    return out

def _kc(w):
    K, N = w.shape[-2:]
    return np.ascontiguousarray(np.moveaxis(w.reshape(w.shape[:-2] + (K // 128, 128, N)), -3, -2))

def _fm(v, n):
    return np.ascontiguousarray(v.reshape(v.shape[0], n, 128).transpose(0, 2, 1))

def _consts():
    c = np.zeros((128, 1280), np.float32)
    i = np.arange(128)
    c[:, 776] = i
    c[:, 1024:1024 + 256] = 128.0 * np.arange(256)[None, :]
    c[:, 0:128] = np.eye(128)
    c[:, 128:256] = (i[None, :] > i[:, None])
    c[:, 256:384] = (i[None, :] >= i[:, None])
    c[:, 384:512] = (i[None, :] < i[:, None])
    c[:, 512:640] = (i[None, :] // 64 == i[:, None] // 64)
    c[:, 640:768] = 1.0
    fi = np.clip((i - 64) // 2, 0, 15)
    inv_freq = (10000.0 ** (-np.arange(0, 32, 2, dtype=np.float32) / 32)).astype(np.float32)
    c[:, 768] = inv_freq[fi] / np.float32(2 * np.pi)
    c[:, 769] = np.where(i % 2 == 0, -1.0, 1.0)
    c[:, 770] = RMS_EPS; c[:, 771] = 1.0; c[:, 772] = -0.5; c[:, 773] = GN_EPS; c[:, 774] = 0.0; c[:, 775] = LN_EPS
    return c

def prep_shared(inp):
    f = lambda a: np.ascontiguousarray(a, dtype=np.float32)
    cols = _perm_w_in()
    sh = {}
    sh["consts"] = _consts()
    sh["embrow"] = f(np.stack([inp["emb_ln_g"], inp["emb_ln_b"]]))
    sh["ada_w"] = _kc(f(inp["ada_w"]))
    vecs = np.zeros((L, 128, 128), np.float32)
    vecs[:, :, 0:48] = _fm(f(inp["ada_b"]), 48)
    mu_full = np.zeros((L, 2208), np.float32); mu_full[:, 416:] = inp["rwkv_mu"]
    mu_p = _take_cols(mu_full, cols)
    mu_p[:, 0:384] = 0; mu_p[:, 448:512] = 0; mu_p[:, 576:640] = 0
    vecs[:, :, 48:66] = _fm(mu_p, NCH)
    vecs[:, :, 66:68] = _fm(f(inp["q_norm_g"]), 2)
    vecs[:, :, 68:69] = _fm(f(inp["kv_norm_g"]), 1)
    vecs[:, :, 69:73] = _fm(f(inp["mla_out_g"]), 4)
    vecs[:, :, 73:77] = _fm(f(inp["rwkv_w0"]), 4)
    vecs[:, :, 77:81] = _fm(f(inp["rwkv_a0"]), 4)
    vecs[:, :, 81:85] = _fm(f(inp["rwkv_k_k"]), 4)
    vecs[:, :, 85:89] = _fm(f(inp["rwkv_k_a"]), 4)
    vecs[:, :, 89:93] = _fm(f(inp["rwkv_r_k"]).reshape(L, 512), 4)
    vecs[:, :, 93:97] = _fm(f(inp["rwkv_gn_g"]), 4)
    vecs[:, :, 97:101] = _fm(f(inp["rwkv_gn_b"]), 4)
    vecs[1:, :, 101:105] = _fm(f(inp["vres_v0"]), 4)
    sh["vecs"] = vecs
    sh["w_in"] = _kc(_take_cols(f(inp["w_in"]), cols))
    wuq = f(inp["w_uq"])
    sw = np.arange(768).reshape(8, 96).copy()
    sw[:, 64:96] = sw[:, 64:96].reshape(8, 16, 2)[:, :, ::-1].reshape(8, 32)
    sh["w_uq"] = _kc(np.concatenate([wuq, wuq[:, :, sw.reshape(-1)]], axis=-1))
    sh["w_ukv"] = f(np.concatenate([inp["w_uk"], inp["w_uv"]], axis=-1))
    wl = np.zeros((L, 128, 1536), np.float32)
    wl[:, 0:64, 0:512] = inp["rwkv_w2"]; wl[:, 0:64, 512:1024] = inp["rwkv_a2"]; wl[:, :, 1024:1536] = inp["rwkv_g2"]
    sh["w_lora"] = wl
    sh["vres1"] = _kc(f(inp["vres_v1"][0])); sh["vres2"] = f(inp["vres_v2"][0])
    sh["w_o"] = _kc(f(inp["w_o"]))
    sh["rows"] = f(np.concatenate([inp["ln1_g"], inp["ln1_b"], inp["ln2_g"], inp["ln2_b"], inp["router_b"]], axis=-1))
    sh["router_w"] = _kc(f(inp["router_w"]))
    gl = np.concatenate([np.arange(0, 2 * D, 2), np.arange(1, 2 * D, 2)])
    sh["_gl"] = gl
    sh["b1v"] = np.ascontiguousarray(f(inp["exp_b1"])[:, :, gl].reshape(L, NE, 16, 128).transpose(0, 1, 3, 2)).reshape(L * NE * 128, 16)
    sh["exp_b2"] = f(inp["exp_b2"])
    return sh

def prep_big(inp, sh):
    gl = sh["_gl"]
    w1 = np.asarray(inp["exp_w1"], dtype=np.float32)
    sh["exp_w1"] = np.ascontiguousarray(np.moveaxis(w1[..., gl].reshape(L, NE, 8, 128, 2 * D), 2, 3)).reshape(L * NE * 128, 8 * 2 * D)
    w2 = np.asarray(inp["exp_w2"], dtype=np.float32)
    sh["exp_w2"] = np.ascontiguousarray(np.moveaxis(w2.reshape(L, NE, 8, 128, D), 2, 3)).reshape(L * NE * 128, 8 * D)

def prep_core(inp, sh, b, S, names):
    f = lambda a: np.ascontiguousarray(a, dtype=np.float32)
    d = {k: v for k, v in sh.items() if k in names}
    d["x"] = f(inp["x"][b, :S]); d["cT"] = f(inp["c"][b].reshape(8, 128).T)
    d["pos"] = np.ascontiguousarray(inp["positions"][b, :S].reshape(1, S).astype(np.int32))
    return d


def kernel(**inputs):
    S = 4096
    bld = B(S)
    nc = bld.build()
    names = set(bld.ins.keys())
    sh = prep_shared(inputs)
    if "exp_w1" in names: prep_big(inputs, sh)
    per = [prep_core(inputs, sh, b, S, names) for b in range(8)]
    res = run_bass_kernel_spmd(nc, per, core_ids=list(range(8)))
    return np.stack([r["y"] for r in res.results], axis=0)
```
